# Optimizing a Trainium2 kernel written in Bass

```python
import jax, jax.numpy as jnp
from jax import lax
import numpy as np

D_MODEL = 1024
BATCH = 8
SEQ = 4096
DEPTH = 1

CHUNK = 64
PLE_DIM = 256
GLA_HEADS = 4
GLA_DK = 128
GLA_DV = 256
GLA_GATE_RANK = 16
GLA_GATE_TAU = 16.0
GLA_QK = GLA_HEADS * GLA_DK
GLA_V = GLA_HEADS * GLA_DV
ATT_HEADS = 16
ATT_DH = 64
ATT_W = ATT_HEADS * ATT_DH
ATT_PAST_CHUNKS = 8
REL_CLIP = 256
N_EXPERTS = 32
TOP_K = 4
D_EXPERT = D_MODEL
SWIGLU_ALPHA = 1.702
SWIGLU_LIMIT = 7.0
EPS = 1e-6
IN_SPLITS = (GLA_QK, GLA_QK, GLA_V, GLA_GATE_RANK, GLA_V, ATT_W, ATT_W, ATT_W, D_MODEL, D_MODEL)
D_IN = 2 * GLA_QK + 2 * GLA_V + GLA_GATE_RANK + 3 * ATT_W + 2 * D_MODEL

kernel_name = 'hybrid_gla_bandattn_moe_ple_block'


def rmsnorm(x, g):
    xf = x.astype(jnp.float32)
    y = xf * lax.rsqrt(jnp.mean(xf * xf, axis=-1, keepdims=True) + EPS)
    return (y * g.astype(jnp.float32)).astype(x.dtype)


def gla_chunked(q, k, v, g_log):
    B, T, H, dk = q.shape
    dv = v.shape[-1]
    n = T // CHUNK

    def chunks(a):
        return a.astype(jnp.float32).reshape(B, n, CHUNK, H, a.shape[-1]).transpose(1, 0, 3, 2, 4)

    qc = chunks(q) * (dk ** -0.5)
    kc, vc, gc = chunks(k), chunks(v), chunks(g_log)
    causal = jnp.tril(jnp.ones((CHUNK, CHUNK), dtype=bool))[:, :, None]

    def step(S, inp):
        qi, ki, vi, gi = inp
        b = jnp.cumsum(gi, axis=2)
        diff = b[:, :, :, None, :] - b[:, :, None, :, :]
        decay = jnp.exp(jnp.where(causal, diff, -jnp.inf))
        scores = jnp.einsum('bhtsd,bhsd->bhts', qi[:, :, :, None, :] * decay, ki)
        o = (jnp.einsum('bhts,bhsv->bhtv', scores, vi)
             + jnp.einsum('bhtd,bhdv->bhtv', qi * jnp.exp(b), S))
        b_last = b[:, :, -1:, :]
        S = (jnp.exp(b_last[:, :, 0, :, None]) * S
             + jnp.einsum('bhsd,bhsv->bhdv', ki * jnp.exp(b_last - b), vi))
        return S, o

    S0 = jnp.zeros((B, H, dk, dv), jnp.float32)
    _, o = lax.scan(step, S0, (qc, kc, vc, gc))
    return o.transpose(1, 0, 3, 2, 4).reshape(B, T, H, dv)


def chunk_band_attention(q, k, v, rel_bias):
    B, T, H, dh = q.shape
    n = T // CHUNK
    pad = ATT_PAST_CHUNKS * CHUNK
    band = pad + CHUNK
    kp = jnp.pad(k, ((0, 0), (pad, 0), (0, 0), (0, 0)))
    vp = jnp.pad(v, ((0, 0), (pad, 0), (0, 0), (0, 0)))
    qc = q.reshape(B, n, CHUNK, H, dh).transpose(1, 0, 2, 3, 4)
    q_pos = jnp.arange(CHUNK) + pad
    k_pos = jnp.arange(band)
    rel = jnp.clip(q_pos[:, None] - k_pos[None, :], -REL_CLIP, REL_CLIP) + REL_CLIP
    bias = rel_bias.astype(jnp.float32)[:, rel]
    scale = dh ** -0.5

    def one_chunk(args):
        c, qi = args
        start = c * CHUNK
        ki = lax.dynamic_slice_in_dim(kp, start, band, axis=1)
        vi = lax.dynamic_slice_in_dim(vp, start, band, axis=1)
        s = jnp.einsum('bqhd,bkhd->bhqk', qi, ki).astype(jnp.float32) * scale + bias
        valid = (start + k_pos) >= pad
        s = jnp.where(valid[None, None, None, :], s, -jnp.inf)
        w = jax.nn.softmax(s, axis=-1)
        return jnp.einsum('bhqk,bkhd->bqhd', w.astype(vi.dtype), vi)

    o = lax.map(one_chunk, (jnp.arange(n), qc))
    return o.transpose(1, 0, 2, 3, 4).reshape(B, T, H * dh)


def clamped_swiglu(h):
    x_glu = jnp.minimum(h[..., ::2], SWIGLU_LIMIT)
    x_lin = jnp.clip(h[..., 1::2], -SWIGLU_LIMIT, SWIGLU_LIMIT)
    return x_glu * jax.nn.sigmoid(SWIGLU_ALPHA * x_glu) * (x_lin + 1.0)


def moe_ffn(x, w_router, b_router, w1, b1, w2, b2):
    logits = (x @ w_router + b_router).astype(jnp.float32)
    top_vals, top_idx = lax.top_k(logits, TOP_K)
    top_w = jax.nn.softmax(top_vals, axis=-1)
    gates = jnp.einsum('nk,nke->ne', top_w, jax.nn.one_hot(top_idx, N_EXPERTS, dtype=jnp.float32))
    out = jnp.zeros(x.shape, jnp.float32)
    for e in range(N_EXPERTS):
        h = x @ w1[e] + b1[e]
        y = clamped_swiglu(h) @ w2[e] + b2[e]
        out = out + gates[:, e:e + 1] * y.astype(jnp.float32)
    return out.astype(x.dtype)


def setup_inputs(seed: int = 0) -> dict:
    key = jax.random.key(seed)
    ks = jax.random.split(key, 20)

    def nrm(k, shape, scale):
        return jax.random.normal(k, shape, jnp.float32) * scale

    def gain(k, shape):
        return 1.0 + nrm(k, shape, 0.05)

    D, E, F = D_MODEL, N_EXPERTS, D_EXPERT
    return {
        'x': nrm(ks[0], (BATCH, SEQ, D), 1.0),
        'p': nrm(ks[1], (DEPTH, BATCH, SEQ, PLE_DIM), 1.0),
        'ln_mix': gain(ks[2], (DEPTH, D)),
        'w_in': nrm(ks[3], (DEPTH, D, D_IN), D ** -0.5),
        'w_gk': nrm(ks[4], (DEPTH, GLA_GATE_RANK, GLA_QK), GLA_GATE_RANK ** -0.5),
        'b_gk': nrm(ks[5], (DEPTH, GLA_QK), 0.1),
        'gla_norm': gain(ks[6], (DEPTH, GLA_DV)),
        'rel_bias': nrm(ks[7], (DEPTH, ATT_HEADS, 2 * REL_CLIP + 1), 0.1),
        'w_out': nrm(ks[8], (DEPTH, D, D), D ** -0.5),
        'ln_moe': gain(ks[9], (DEPTH, D)),
        'w_router': nrm(ks[10], (DEPTH, D, E), D ** -0.5),
        'b_router': nrm(ks[11], (DEPTH, E), 0.01),
        'w1': nrm(ks[12], (DEPTH, E, D, 2 * F), D ** -0.5),
        'b1': nrm(ks[13], (DEPTH, E, 2 * F), 0.01),
        'w2': nrm(ks[14], (DEPTH, E, F, D), F ** -0.5),
        'b2': nrm(ks[15], (DEPTH, E, D), 0.01),
        'ln_ple': gain(ks[16], (DEPTH, D)),
        'w_ple_gate': nrm(ks[17], (DEPTH, D, D), D ** -0.5),
        'w_ple_proj': nrm(ks[18], (DEPTH, PLE_DIM, D), PLE_DIM ** -0.5),
        'ln_final': gain(ks[19], (D,)),
    }


def reference(x, p, ln_mix, w_in, w_gk, b_gk, gla_norm, rel_bias, w_out, ln_moe, w_router,
              b_router, w1, b1, w2, b2, ln_ple, w_ple_gate, w_ple_proj, ln_final):
    B, T, D = x.shape
    splits = np.cumsum(IN_SPLITS)[:-1].tolist()
    for i in range(DEPTH):
        xn = rmsnorm(x, ln_mix[i])
        z = xn @ w_in[i]
        q_g, k_g, v_g, gk_low, r_g, q_a, k_a, v_a, gt_a, gt_b = jnp.split(z, splits, axis=-1)

        g_log = jax.nn.log_sigmoid((gk_low @ w_gk[i] + b_gk[i]).astype(jnp.float32)) / GLA_GATE_TAU
        o_g = gla_chunked(q_g.reshape(B, T, GLA_HEADS, GLA_DK),
                          k_g.reshape(B, T, GLA_HEADS, GLA_DK),
                          v_g.reshape(B, T, GLA_HEADS, GLA_DV),
                          g_log.reshape(B, T, GLA_HEADS, GLA_DK))
        o_g = rmsnorm(o_g, gla_norm[i]) * jax.nn.silu(r_g.reshape(B, T, GLA_HEADS, GLA_DV).astype(jnp.float32))
        y_a = o_g.reshape(B, T, GLA_V).astype(x.dtype)

        y_b = chunk_band_attention(q_a.reshape(B, T, ATT_HEADS, ATT_DH),
                                   k_a.reshape(B, T, ATT_HEADS, ATT_DH),
                                   v_a.reshape(B, T, ATT_HEADS, ATT_DH),
                                   rel_bias[i])

        h = jax.nn.sigmoid(gt_a) * y_a + jax.nn.sigmoid(gt_b) * y_b
        x = x + h @ w_out[i]

        xm = rmsnorm(x, ln_moe[i]).reshape(B * T, D)
        x = x + moe_ffn(xm, w_router[i], b_router[i], w1[i], b1[i], w2[i], b2[i]).reshape(B, T, D)

        ple_gate = jax.nn.sigmoid(rmsnorm(x, ln_ple[i]) @ w_ple_gate[i])
        x = x + ple_gate * (p[i] @ w_ple_proj[i])
    return rmsnorm(x, ln_final)
```

```python
import numpy as np
from contextlib import ExitStack
import concourse.bass as bass
import concourse.mybir as mybir
from concourse.bass_utils import run_bass_kernel_spmd

F32 = mybir.dt.float32
BF16 = mybir.dt.bfloat16
I32 = mybir.dt.int32
U32 = mybir.dt.uint32
AF = mybir.ActivationFunctionType
ALU = mybir.AluOpType
AX = mybir.AxisListType


class Buf:
    __slots__ = ("name", "last_w", "readers")

    def __init__(self, name):
        self.name = name
        self.last_w = None
        self.readers = []


class DSem:
    def __init__(self, prog):
        self.prog = prog
        self.count = 0
        self.handle = prog.new_sem()
        prog.dsems.append(self)


class Op:
    __slots__ = ("eng", "fn", "cdeps", "ddeps", "is_dma", "dsem", "needs_inc", "inc_val", "dma_val")


class Prog:
    ENGS = ("pe", "act", "dve", "pool", "sp")

    def __init__(self, nc, same_engine_sync=True):
        self.nc = nc
        self.es = ExitStack()
        self.q = {e: [] for e in self.ENGS}
        self.esem = {}
        self.same_engine_sync = same_engine_sync
        self.nsem = 0
        self.dsems = []
        self.pending = {}
        for e in self.ENGS:
            self.esem[e] = self.new_sem()

    def new_sem(self):
        self.nsem += 1
        return self.es.enter_context(self.nc.semaphore("s%d" % self.nsem))

    def op(self, eng, fn, reads=(), writes=(), dsem=None, is_dma=False):
        o = Op()
        o.eng = eng
        o.fn = fn
        o.is_dma = is_dma
        o.dsem = dsem
        o.needs_inc = False
        o.inc_val = None
        o.dma_val = None
        cdeps = set()
        ddeps = {}

        def add_dep(p):
            if p is None or p is o:
                return
            if p.is_dma:
                ds = p.dsem
                ddeps[id(ds)] = (ds, ds.count)
            else:
                if p.eng == eng and not is_dma and (eng == "pe" or not self.same_engine_sync):
                    return
                cdeps.add(p)

        pend = self.pending.pop(eng, None)
        if pend is not None:
            for p in pend[0]:
                if p.eng != eng or self.same_engine_sync:
                    if not (p.eng == eng and eng == "pe"):
                        cdeps.add(p)
            for ds, v in pend[1]:
                ddeps[id(ds)] = (ds, v)
        for b in reads:
            add_dep(b.last_w)
        for b in writes:
            add_dep(b.last_w)
            for r in b.readers:
                add_dep(r)
        for b in reads:
            b.readers.append(o)
        for b in writes:
            b.last_w = o
            b.readers = []
        for p in cdeps:
            p.needs_inc = True
        o.cdeps = cdeps
        o.ddeps = list(ddeps.values())
        if is_dma:
            dsem.count += 16
            o.dma_val = dsem.count
        self.q[eng].append(o)
        return o

    def barrier(self):
        last = []
        for e in self.ENGS:
            for o in reversed(self.q[e]):
                if not o.is_dma:
                    last.append(o)
                    break
        dsv = [(ds, ds.count) for ds in self.dsems if ds.count > 0]
        for e in self.ENGS:
            self.pending[e] = (last, dsv)

    def dma(self, eng, out, in_, reads=(), writes=(), dsem=None):
        return self.op(eng, lambda e: e.dma_start(out=out, in_=in_), reads=reads, writes=writes,
                       dsem=dsem, is_dma=True)

    def emit(self, final_waits=()):
        nc = self.nc
        for e in self.ENGS:
            c = 0
            for o in self.q[e]:
                if o.needs_inc and not o.is_dma:
                    c += 1
                    o.inc_val = c
        engmap = {"pe": "tensor", "act": "scalar", "dve": "vector", "pool": "gpsimd", "sp": "sync"}
        with nc.Block() as block:
            for e in self.ENGS:
                ops = self.q[e]
                esem = self.esem
                fw = final_waits if e == "sp" else ()

                def body(eng, ops=ops, e=e, fw=fw):
                    waited = {}
                    for o in ops:
                        waits = []
                        for p in o.cdeps:
                            waits.append((esem[p.eng], p.inc_val))
                        for ds, v in o.ddeps:
                            waits.append((ds.handle, v))
                        for h, v in waits:
                            k = id(h)
                            if waited.get(k, 0) >= v:
                                continue
                            waited[k] = v
                            eng.wait_ge(h, v)
                        ins = o.fn(eng)
                        if o.is_dma:
                            ins.then_inc(o.dsem.handle, 16)
                        elif o.needs_inc:
                            ins.then_inc(esem[e], 1)
                    for ds in fw:
                        eng.wait_ge(ds.handle, ds.count)

                getattr(block, engmap[e])(body)
        self.es.close()


D = 1024
DIN = 8208
EPS = 1e-6
NEG = -30000.0
DEFER_TAIL = True
QK_AHEAD = True
C_QG, C_KG, C_VG, C_GK, C_RG, C_QA, C_KA, C_VA, C_GA, C_GB = 0, 512, 1024, 2048, 2064, 3088, 4112, 5136, 6160, 7184


def build_program(T, dbg=False, stop_after=99, fill=0.0):
    NB = T // 128
    NSB = T // 512
    CAP = T
    NT = (4 * T) // 512 + 31
    NROWS = 32 * CAP + NT * 512
    nc = bass.Bass("TRN2", target_bir_lowering=False)

    def din(name, shape, dt=F32):
        return nc.dram_tensor(name, shape, dt, kind="ExternalInput").ap()

    def dscr(name, shape, dt, out=False):
        return nc.dram_tensor(name, shape, dt, kind=("ExternalOutput" if out else "Internal")).ap()

    x_d = din("x", [T, D])
    p_d = din("p", [T, 256])
    w_in = din("w_in", [D, DIN])
    wgk_d = din("wgk_aug", [17, 512])
    biasT_d = din("biasT", [128, 16, 5, 128])
    w_out_d = din("w_out", [D, D])
    w_router_d = din("w_router", [D, 32])
    w1_d = din("w1", [32 * 128, 8 * 2048])
    b1_d = din("b1", [32 * 128, 16])
    w2_d = din("w2", [32 * 128, 8 * D])
    b2_d = din("b2", [32, D])
    w_pg_d = din("w_pg", [D, D])
    w_pp_d = din("w_pp", [256, D])
    lnmixT_d = din("lnmixT", [128, 8])
    lnpleT_d = din("lnpleT", [128, 8])
    lnmoe_d = din("lnmoe_b", [128, D])
    lnfin_d = din("lnfin_b", [128, D])
    gnorm_d = din("gnorm_b", [128, 256])
    brout_d = din("brout_b", [128, 32])
    cA_d = din("cA", [128, 128 * 6 + 512 + 32 + 8 + 4 + 1 + 64])
    maskadd_d = din("maskadd", [128, 5, 128])

    out_d = nc.dram_tensor("out", [T, D], F32, kind="ExternalOutput").ap()
    ha_d = dscr("ha_s", [T, D], BF16)
    x1_d = dscr("x1_s", [T, D], F32, out=dbg)
    xs_d = dscr("xs_s", [NT * 512, D], BF16)
    ys_d = dscr("ys_s", [NT * 512, D], F32)
    xm_d = dscr("xm_s", [T, D], BF16)
    LOGCAP = CAP.bit_length() - 1
    assert (1 << LOGCAP) == CAP
    dbg_d = {}
    if dbg:
        dbg_d["ha"] = dscr("dbg_ha", [T, D], F32, out=True)
        dbg_d["h"] = dscr("dbg_h", [T, D], F32, out=True)
        dbg_d["lg"] = dscr("dbg_lg", [T, 32], F32, out=True)
        dbg_d["x2"] = dscr("dbg_x2", [T, D], F32, out=True)

    P = Prog(nc)
    es = ExitStack()
    dumped = set()

    def dump(name, ap, b, shape, dt=F32):
        if not dbg or name in dumped:
            return
        dumped.add(name)
        dd = nc.dram_tensor("dmp_" + name, list(shape), dt, kind="ExternalOutput").ap()
        P.dma('sp', dd, ap, reads=[b], dsem=DSem(P))

    def sb(name, shape, dt, nb=1, stack=None):
        t = (stack or es).enter_context(nc.sbuf_tensor("sb_" + name, shape, dt))
        bufs = [Buf("%s_%d" % (name, i)) for i in range(nb)]
        return t, (bufs[0] if nb == 1 else bufs)

    def I(eng, method, reads, writes, *a, **k):
        return P.op(eng, lambda e: getattr(e, method)(*a, **k), reads=reads, writes=writes)

    ps = []
    psb = []
    for b in range(8):
        ps.append(es.enter_context(nc.psum_tensor("ps%d" % b, [128, 512], F32)))
        psb.append(Buf("ps%d" % b))

    def psbf(b):
        return ps[b][:].bitcast(BF16)

    NCA = 128 * 6 + 512 + 32 + 8 + 4 + 1 + 64
    cA, cAb = sb("cA", [128, NCA], F32)
    o = 0
    identf = cA[:, o:o + 128]; o += 128
    triS = cA[:, o:o + 128]; o += 128
    c_tristrict = cA[:, o:o + 128]; o += 128
    c_ones = cA[:, o:o + 128]; o += 128
    c_ident2 = cA[:, o:o + 128]; o += 128
    c_spare = cA[:, o:o + 128]; o += 128
    maskA4 = cA[:, o:o + 512]; o += 512
    eoff = cA[:, o:o + 32]; o += 32
    pk8 = cA[:, o:o + 8]; o += 8
    pc4 = cA[:, o:o + 4]; o += 4
    pidx = cA[:, o:o + 1]; o += 1
    tvals = cA[:, o:o + 64]; o += 64
    ds_c = DSem(P)
    P.dma('sp', cA[:], cA_d, writes=[cAb], dsem=ds_c)
    cB, cBb = sb("cB", [128, 3 * 128], BF16)
    ident = cB[:, 0:128]
    tristrict = cB[:, 128:256]
    ones_bf = cB[:, 256:384]
    lnmixT, lnmixTb = sb("lnmixT", [128, 8], F32)
    lnpleT, lnpleTb = sb("lnpleT", [128, 8], F32)
    P.dma('sp', lnmixT[:], lnmixT_d, writes=[lnmixTb], dsem=ds_c)
    P.dma('sp', lnpleT[:], lnpleT_d, writes=[lnpleTb], dsem=ds_c)
    gnorm, gnormb = sb("gnorm", [128, 256], F32)
    P.dma('sp', gnorm[:], gnorm_d, writes=[gnormb], dsem=ds_c)
    wgk, wgkb = sb("wgk", [17, 512], F32)
    P.dma('sp', wgk[:], wgk_d, writes=[wgkb], dsem=ds_c)
    I('dve', 'tensor_copy', [cAb], [cBb], out=cB[:, 0:128], in_=c_ident2)
    I('dve', 'tensor_copy', [cAb], [cBb], out=cB[:, 128:384], in_=cA[:, 256:512])
    lnb, lnbb = sb("lnb", [128, 8, 128], F32)
    for kc in range(8):
        I('dve', 'tensor_scalar', [cAb, lnmixTb], [lnbb], out=lnb[:, kc, :], in0=c_ones, scalar1=lnmixT[:, kc:kc + 1],
          scalar2=None, op0=ALU.mult)

    ss, ssb = sb("ss", [128, 8], F32)
    junk, junkb = sb("junk", [128, 1024], F32)
    slots_i, slots_ib = sb("slots_i", [128, NB, 4], I32)
    gate4, gate4b = sb("gate4", [128, NB, 4], F32)
    cm, cmb = sb("cm", [128, 32], F32)
    cmbf, cmbfb = sb("cmbf", [128, 32], BF16)
    epsc, epscb = sb("epsc", [128, 1], F32)
    zt, ztb = sb("zt", [128, D], BF16)
    I('pool', 'memset', [], [ztb], zt[:], fill)
    ds_z = DSem(P)
    zfill_next = [0]

    def zero_fill(n):
        for _ in range(n):
            t_ = zfill_next[0]
            if t_ >= NT:
                return
            zfill_next[0] += 1
            P.dma('sp', xs_d[t_ * 512:(t_ + 1) * 512, :].rearrange("(c p) d -> p c d", p=128), zt[:].unsqueeze(1).to_broadcast([128, 4, D]), reads=[ztb], dsem=ds_z)
    esAB = ExitStack()
    xbuf = [sb("xb%d" % i, [128, D], F32, stack=esAB) for i in range(2)]
    dsx = [DSem(P) for _ in range(2)]
    xn, xnb = sb("xn", [128, D], BF16, stack=esAB)
    xnT, xnTb = sb("xnT", [128, 8, 512], BF16, stack=esAB)
    NRING = 4
    wring = [sb("wr%d" % i, [128, 8, 512], BF16, stack=esAB) for i in range(NRING)]
    dsw = [DSem(P) for _ in range(NRING)]
    wcount = [0]

    def rms_rstd(src_ap, srcb, n, col):
        I('act', 'activation', [srcb], [junkb], out=junk[:, 0:n], in_=src_ap, func=AF.Square)
        I('dve', 'tensor_reduce', [junkb], [ssb], out=ss[:, col:col + 1], in_=junk[:, 0:n], axis=AX.X, op=ALU.add)
        I('act', 'activation', [ssb], [ssb], out=ss[:, col + 1:col + 2], in_=ss[:, col:col + 1], func=AF.Ln,
          scale=1.0 / n, bias=epsc[:, 0:1])
        I('act', 'activation', [ssb], [ssb], out=ss[:, col + 2:col + 3], in_=ss[:, col + 1:col + 2], func=AF.Exp,
          scale=-0.5)
        return ss[:, col + 2:col + 3]

    I('dve', 'memset', [], [epscb], epsc[:], EPS)

    def load_x(i):
        xt, xtb = xbuf[i % 2]
        P.dma('sp', xt[:], x_d[i * 128:(i + 1) * 128, :], writes=[xtb], dsem=dsx[i % 2])

    def load_norm_A(i, j, dma=True):
        xt, xtb = xbuf[i % 2]
        if dma:
            load_x(i)
        r = rms_rstd(xt[:], xtb, D, 0)
        I('dve', 'tensor_scalar', [xtb, ssb], [xnb], out=xn[:], in0=xt[:], scalar1=r, scalar2=None, op0=ALU.mult)

    def load_norm_T(i, j, lnbt, lnbtb):
        load_norm_A(i, j)
        load_norm_B(j, lnbt, lnbtb)

    def load_norm_B(j, lnbt, lnbtb):
        pT = psbf(2)
        for kc in range(8):
            I('pe', 'transpose', [xnb, cBb], [psb[2]], out=pT[:, kc * 128:(kc + 1) * 128],
              in_=xn[:, kc * 128:(kc + 1) * 128], identity=ident)
        I('dve', 'tensor_tensor', [psb[2], lnbtb], [xnTb], out=xnT[:, :, j * 128:(j + 1) * 128],
          in0=pT.rearrange("p (a b) -> p a b", a=8), in1=lnbt[:], op=ALU.mult)

    w_in_v = w_in.rearrange("(kc p) c -> p kc c", p=128)

    def load_wchunk(c0, ncols):
        k = wcount[0] % NRING
        wcount[0] += 1
        wt, wtb = wring[k]
        P.dma('pool', wt[:, :, 0:ncols], w_in_v[:, :, c0:c0 + ncols], writes=[wtb], dsem=dsw[k])
        return wt, wtb

    pbank = [0]

    def proj_fm(wt, wtb, sub, nsub, evac):
        for q in range(nsub):
            b = pbank[0] % 2
            pbank[0] += 1
            for kc in range(8):
                I('pe', 'matmul', [wtb, xnTb], [psb[b]], ps[b][0:sub, :], lhsT=wt[:, kc, q * sub:(q + 1) * sub],
                  rhs=xnT[:, kc, :], start=(kc == 0), stop=(kc == 7))
            evac(q, ps[b], psb[b])

    def proj_tm(wt, wtb, evac):
        for j in range(4):
            b = pbank[0] % 2
            pbank[0] += 1
            for kc in range(8):
                I('pe', 'matmul', [wtb, xnTb], [psb[b]], ps[b][:, :], lhsT=xnT[:, kc, j * 128:(j + 1) * 128],
                  rhs=wt[:, kc, :], start=(kc == 0), stop=(kc == 7))
            evac(j, ps[b], psb[b])

    esG = ExitStack()
    qTg, qTgb = sb("qTg", [128, 4, 512], BF16, stack=esG)
    kTg, kTgb = sb("kTg", [128, 4, 512], BF16, stack=esG)
    gka, gkab = sb("gka", [32, 512], F32, stack=esG)
    vg, vgb = sb("vg", [128, 4, D], BF16, stack=esG)
    rs, rsb = sb("rs", [128, 4, D], BF16, stack=esG)
    sgt, sgtb = sb("sgt", [128, 512], BF16, stack=esG)
    Sst, Sstb = sb("Sst", [128, 4, 256], F32, stack=esG)
    Sbf, Sbfb = sb("Sbf", [128, 4, 256], BF16, stack=esG)
    e1t, e1tb = sb("e1t", [128, 512], F32, stack=esG)
    lt, ltb = sb("lt", [128, 512], F32, stack=esG)
    E1s = [sb("E1_%d" % i, [128, 512], F32, stack=esG) for i in range(2)]
    E3, E3b = sb("E3", [128, 512], F32, stack=esG)
    qtls = [sb("qtl%d" % i, [128, 512], BF16, stack=esG) for i in range(2)]
    ktl, ktlb = sb("ktl", [128, 512], BF16, stack=esG)
    khT, khTb = sb("khT", [128, 512], BF16, stack=esG)
    khs = [sb("kh%d" % i, [128, 512], BF16, stack=esG) for i in range(2)]
    ATs = [sb("AT%d" % i, [128, 512], BF16, stack=esG) for i in range(2)]
    yg, ygb = sb("yg", [128, D], F32, stack=esG)
    ss4, ss4b = sb("ss4", [128, 12], F32, stack=esG)
    hab = [sb("ha%d" % i, [128, D], BF16, stack=esG) for i in range(2)]
    dsha = [DSem(P) for _ in range(2)]
    I('pool', 'memset', [], [gkab], gka[:], 1.0)
    I('pool', 'memset', [], [Sstb], Sst[:], 0.0)
    I('pool', 'memset', [], [Sbfb], Sbf[:], 0.0)

    WG, _ = sb("WG", [128, 8, 4112], BF16, stack=esG)
    wg_chunks = {}
    for (c0, ncols, o0) in ((C_QG, 512, 0), (C_KG, 512, 512), (C_GK, 16, 2048), (C_VG, 512, 1024), (C_VG + 512, 512, 1536),
                            (C_RG, 512, 2064), (C_RG + 512, 512, 2576), (C_GA, 512, 3088), (C_GA + 512, 512, 3600)):
        bb = Buf("wg%d" % c0)
        P.dma('pool', WG[:, :, o0:o0 + ncols], w_in_v[:, :, c0:c0 + ncols], writes=[bb], dsem=DSem(P))
        wg_chunks[c0] = (WG[:, :, o0:o0 + ncols], bb)

    def load_wchunk_g(c0, ncols):
        return wg_chunks[c0]

    for j in range(4):
        load_norm_T(j, j, lnb, lnbb)
    for s in range(NSB):
        dump("xnT", xnT[:], xnTb, [128, 8, 512], BF16)
        wt, wtb = load_wchunk_g(C_QG, 512)
        proj_fm(wt, wtb, 128, 4, lambda q, pt, ptb: I('act', 'activation', [ptb], [qTgb], out=qTg[:, q, :], in_=pt[:, :], func=AF.Copy))
        wt, wtb = load_wchunk_g(C_KG, 512)
        proj_fm(wt, wtb, 128, 4, lambda q, pt, ptb: I('dve', 'tensor_copy', [ptb], [kTgb], out=kTg[:, q, :], in_=pt[:, :]))
        wt, wtb = load_wchunk_g(C_GK, 16)
        proj_fm(wt, wtb, 16, 1, lambda q, pt, ptb: I('dve', 'tensor_copy', [ptb], [gkab], out=gka[0:16, :], in_=pt[0:16, :]))
        for hf in range(2):
            wt, wtb = load_wchunk_g(C_VG + hf * 512, 512)
            proj_tm(wt, wtb, lambda j, pt, ptb, hf=hf: I('act', 'activation', [ptb], [vgb], out=vg[:, j, hf * 512:(hf + 1) * 512], in_=pt[:, :], func=AF.Copy))
        for hf in range(2):
            wt, wtb = load_wchunk_g(C_RG + hf * 512, 512)
            proj_tm(wt, wtb, lambda j, pt, ptb, hf=hf: I('act', 'activation', [ptb], [rsb], out=rs[:, j, hf * 512:(hf + 1) * 512], in_=pt[:, :], func=AF.Silu))
        for hf in range(2):
            wt, wtb = load_wchunk_g(C_GA + hf * 512, 512)

            def ev(j, pt, ptb, hf=hf):
                I('act', 'activation', [ptb], [sgtb], out=sgt[:], in_=pt[:, :], func=AF.Sigmoid)
                I('dve', 'tensor_tensor', [sgtb, rsb], [rsb], out=rs[:, j, hf * 512:(hf + 1) * 512],
                  in0=rs[:, j, hf * 512:(hf + 1) * 512], in1=sgt[:], op=ALU.mult)
            proj_tm(wt, wtb, ev)
        dump("qTg", qTg[:], qTgb, [128, 4, 512], BF16)
        dump("kTg", kTg[:], kTgb, [128, 4, 512], BF16)
        dump("gka", gka[:], gkab, [32, 512])
        dump("vg", vg[:], vgb, [128, 4, D], BF16)
        dump("rs", rs[:], rsb, [128, 4, D], BF16)
        def gla_front(j, s=s):
            tok = slice(j * 128, (j + 1) * 128)
            E1, E1b = E1s[j % 2]
            qtl, qtlb = qtls[j % 2]
            kh, khb = khs[j % 2]
            AT, ATb = ATs[j % 2]
            I('pe', 'matmul', [gkab, wgkb], [psb[3]], ps[3][:, :], lhsT=gka[0:17, tok], rhs=wgk[0:17, :], start=True, stop=True)
            I('act', 'activation', [psb[3]], [e1tb], out=e1t[:], in_=ps[3][:, :], func=AF.Exp, scale=-1.0)
            I('act', 'activation', [e1tb], [ltb], out=lt[:], in_=e1t[:], func=AF.Ln, bias=1.0)
            for h in range(4):
                I('pe', 'matmul', [ltb, cAb], [psb[4]], ps[4][:, h * 128:(h + 1) * 128], lhsT=lt[:, h * 128:(h + 1) * 128],
                  rhs=triS, start=True, stop=True)
            I('act', 'activation', [psb[4]], [E1b], out=E1[:], in_=ps[4][:, :], func=AF.Exp)
            I('act', 'activation', [psb[4]], [E3b], out=E3[:], in_=ps[4][:, :], func=AF.Exp, scale=-1.0)
            I('dve', 'scalar_tensor_tensor', [E1b, qTgb], [qtlb], out=qtl[:].rearrange("p (h t) -> p h t", h=4), in0=E1[:].rearrange("p (h t) -> p h t", h=4),
              scalar=128.0 ** -0.5, in1=qTg[:, :, tok], op0=ALU.mult, op1=ALU.mult)
            I('dve', 'tensor_tensor', [E3b, kTgb], [ktlb], out=ktl[:].rearrange("p (h t) -> p h t", h=4), in0=E3[:].rearrange("p (h t) -> p h t", h=4),
              in1=kTg[:, :, tok], op=ALU.mult)
            for h in range(4):
                I('dve', 'tensor_scalar', [ktlb, E1b], [khTb], out=khT[:, h * 128:(h + 1) * 128], in0=ktl[:, h * 128:(h + 1) * 128],
                  scalar1=E1[:, h * 128 + 127:h * 128 + 128], scalar2=None, op0=ALU.mult)
            pT = psbf(2)
            for h in range(4):
                I('pe', 'transpose', [khTb, cBb], [psb[2]], out=pT[:, h * 128:(h + 1) * 128], in_=khT[:, h * 128:(h + 1) * 128], identity=ident)
            I('act', 'activation', [psb[2]], [khb], out=kh[:], in_=pT[:, 0:512], func=AF.Copy)
            for h in range(4):
                I('pe', 'matmul', [ktlb, qtlb], [psb[3]], ps[3][:, h * 128:(h + 1) * 128], lhsT=ktl[:, h * 128:(h + 1) * 128],
                  rhs=qtl[:, h * 128:(h + 1) * 128], start=True, stop=True)
            I('dve', 'tensor_tensor', [psb[3], cAb], [ATb], out=AT[:], in0=ps[3][:, :], in1=maskA4, op=ALU.mult)

        def gla_back(j, s=s):
            i = 4 * s + j
            E1, E1b = E1s[j % 2]
            qtl, qtlb = qtls[j % 2]
            kh, khb = khs[j % 2]
            AT, ATb = ATs[j % 2]
            for h in range(4):
                bk = 5 + h // 2
                oc = slice((h % 2) * 256, (h % 2) * 256 + 256)
                I('pe', 'matmul', [ATb, vgb], [psb[bk]], ps[bk][:, oc], lhsT=AT[:, h * 128:(h + 1) * 128], rhs=vg[:, j, h * 256:(h + 1) * 256],
                  start=True, stop=False)
                I('pe', 'matmul', [qtlb, Sbfb], [psb[bk]], ps[bk][:, oc], lhsT=qtl[:, h * 128:(h + 1) * 128], rhs=Sbf[:, h, :],
                  start=False, stop=True)
            for hh in range(2):
                for h2 in range(2):
                    h = hh * 2 + h2
                    I('pe', 'matmul', [khb, vgb], [psb[7]], ps[7][:, h2 * 256:(h2 + 1) * 256], lhsT=kh[:, h * 128:(h + 1) * 128],
                      rhs=vg[:, j, h * 256:(h + 1) * 256], start=True, stop=True)
                for h2 in range(2):
                    h = hh * 2 + h2
                    I('dve', 'scalar_tensor_tensor', [Sstb, E1b, psb[7]], [Sstb], out=Sst[:, h, :], in0=Sst[:, h, :],
                      scalar=E1[:, h * 128 + 127:h * 128 + 128], in1=ps[7][:, h2 * 256:(h2 + 1) * 256], op0=ALU.mult, op1=ALU.add)
            I('pool', 'tensor_copy', [Sstb], [Sbfb], out=Sbf[:], in_=Sst[:])
            I('act', 'activation', [psb[5]], [junkb], out=junk[:, 0:512], in_=ps[5][:, :], func=AF.Square)
            I('act', 'activation', [psb[6]], [junkb], out=junk[:, 512:1024], in_=ps[6][:, :], func=AF.Square)
            I('dve', 'tensor_reduce', [junkb], [ss4b], out=ss4[:, 0:4], in_=junk[:].rearrange("p (h d) -> p h d", h=4), axis=AX.X, op=ALU.add)
            I('act', 'activation', [ss4b], [ss4b], out=ss4[:, 4:8], in_=ss4[:, 0:4], func=AF.Ln, scale=1.0 / 256, bias=epsc[:, 0:1])
            I('act', 'activation', [ss4b], [ss4b], out=ss4[:, 8:12], in_=ss4[:, 4:8], func=AF.Exp, scale=-0.5)
            for h in range(4):
                bk = 5 + h // 2
                oc = slice((h % 2) * 256, (h % 2) * 256 + 256)
                I('dve', 'scalar_tensor_tensor', [psb[bk], ss4b, gnormb], [ygb], out=yg[:, h * 256:(h + 1) * 256], in0=ps[bk][:, oc],
                  scalar=ss4[:, 8 + h:9 + h], in1=gnorm[:], op0=ALU.mult, op1=ALU.mult)
            hat, hatb = hab[i % 2]
            I('dve', 'tensor_tensor', [ygb, rsb], [hatb], out=hat[:], in0=yg[:], in1=rs[:, j, :], op=ALU.mult)
            P.dma('sp', ha_d[i * 128:(i + 1) * 128, :], hat[:], reads=[hatb], dsem=dsha[i % 2])
            if dbg:
                I('dve', 'tensor_copy', [hatb, ygb], [ygb], out=yg[:], in_=hat[:])
                P.dma('sp', dbg_d["ha"][i * 128:(i + 1) * 128, :], yg[:], reads=[ygb], dsem=dsha[i % 2])

        gla_front(0)
        for j in range(4):
            zero_fill((NT + 4 * NSB - 1) // (4 * NSB))
            if s + 1 < NSB:
                load_x(4 * (s + 1) + j)
            if j + 1 < 4:
                gla_front(j + 1)
            gla_back(j)
            if s + 1 < NSB:
                if j > 0:
                    load_norm_B(j - 1, lnb, lnbb)
                load_norm_A(4 * (s + 1) + j, j, dma=False)
        if s + 1 < NSB:
            load_norm_B(3, lnb, lnbb)
    zero_fill(NT)
    P.barrier()
    esG.close()
    if stop_after <= 1:
        P.emit(final_waits=[d for d in P.dsems if d.count > 0])
        es.close()
        return nc

    I('pool', 'memset', [], [cmb], cm[:], 0.0)
    I('pool', 'memset', [], [cmbfb], cmbf[:], 0.0)

    esB = ExitStack()
    qTa, qTab = sb("qTa", [128, 8, 512], BF16, stack=esB)
    kTa = [sb("kTa%d" % i, [128, 8, 512], BF16, stack=esB) for i in range(2)]
    vaug = [sb("vaug%d" % i, [128, 4, 16, 65], BF16, stack=esB) for i in range(2)]
    sgb, sgbb = sb("sgb", [128, 4, D], BF16, stack=esB)
    BMT, BMTb = sb("BMT", [128, 16, 5, 128], BF16, stack=esB)
    Wout, Woutb = sb("Wout", [128, 8, D], BF16, stack=esB)
    lnmoe, lnmoeb = sb("lnmoe", [128, D], F32, stack=esB)
    brout, broutb = sb("brout", [128, 32], F32, stack=esB)
    wr32, wr32b = sb("wr32", [128, 8, 32], F32, stack=esB)
    madd, maddb = sb("madd", [128, 5, 128], F32, stack=esB)
    bstg = [sb("bstg%d" % i, [128, 5, 128], F32, stack=esB) for i in range(2)]
    dsbs = [DSem(P) for _ in range(2)]
    PT = [sb("PT%d" % i, [128, 640], BF16, stack=esB) for i in range(2)]
    yb, ybb = sb("yb", [128, D], F32, stack=esB)
    rden, rdenb = sb("rden", [128, 4], F32, stack=esB)
    hld = [sb("hld%d" % i, [128, D], BF16, stack=esB) for i in range(2)]
    dshl = [DSem(P) for _ in range(2)]
    hm, hmb = sb("hm", [128, D], BF16, stack=esB)
    hT, hTb = sb("hT", [128, 8, 128], BF16, stack=esB)
    xres, xresb = sb("xres", [128, D], F32, stack=esB)
    dsxr = DSem(P)
    x1t, x1tb = sb("x1t", [128, D], F32, stack=esB)
    dsx1 = DSem(P)
    xm32, xm32b = sb("xm32", [128, D], F32, stack=esB)
    xmb = [sb("xmb%d" % i, [128, D], BF16, stack=esB) for i in range(2)]
    dsxm = [DSem(P) for _ in range(2)]
    xmT32, xmT32b = sb("xmT32", [128, 8, 128], F32, stack=esB)
    lg, lgb = sb("lg", [128, 32], F32, stack=esB)
    m8, m8b = sb("m8", [128, 8], F32, stack=esB)
    mask, maskb = sb("mask", [128, 32], F32, stack=esB)
    maskbf, maskbfb = sb("maskbf", [128, 32], BF16, stack=esB)
    ex, exb = sb("ex", [128, 32], F32, stack=esB)
    gates, gatesb = sb("gates", [128, 32], F32, stack=esB)
    slotf, slotfb = sb("slotf", [128, 32], F32, stack=esB)
    oh4, oh4b = sb("oh4", [128, 4, 32], F32, stack=esB)
    i8, i8b = sb("i8", [128, 8], U32, stack=esB)
    i8f, i8fb = sb("i8f", [128, 8], F32, stack=esB)
    jk32, jk32b = sb("jk32", [128, 32], F32, stack=esB)
    sm, smb = sb("sm", [128, 8], F32, stack=esB)
    slot4f, slot4fb = sb("slot4f", [128, 4], F32, stack=esB)
    YB = (5, 2)
    pending_tail = []
    _dbgs = []

    def dbgsem():
        if len(_dbgs) < 6:
            _dbgs.append(DSem(P))
            return _dbgs[-1]
        return _dbgs[len(_dbgs) % 6 - 1]
    ds_b = DSem(P)
    P.dma('sp', lnmoe[:], lnmoe_d, writes=[lnmoeb], dsem=ds_b)
    P.dma('sp', brout[:], brout_d, writes=[broutb], dsem=ds_b)
    P.dma('sp', wr32[:], w_router_d.rearrange("(kc p) e -> p kc e", p=128), writes=[wr32b], dsem=ds_b)
    P.dma('sp', madd[:], maskadd_d, writes=[maddb], dsem=ds_b)
    P.dma('pool', Wout[:], w_out_d.rearrange("(kc p) c -> p kc c", p=128), writes=[Woutb], dsem=ds_b)
    for h in range(16):
        st, stb = bstg[h % 2]
        P.dma('sp', st[:], biasT_d[:, h], writes=[stb], dsem=dsbs[h % 2])
        I('dve', 'tensor_tensor', [stb, maddb], [BMTb], out=BMT[:, h], in0=st[:], in1=madd[:], op=ALU.add)
    for i2 in range(2):
        I('pool', 'memset', [], [vaug[i2][1]], vaug[i2][0][:], 1.0)

    for j in range(4):
        load_norm_T(j, j, lnb, lnbb)
    for s in range(NSB):
        kT_t, kT_b = kTa[s % 2]
        va_t, va_b = vaug[s % 2]
        def pop_tail():
            if pending_tail:
                pending_tail.pop(0)()

        for hf in range(2):
            wt, wtb = load_wchunk(C_QA + hf * 512, 512)
            proj_fm(wt, wtb, 128, 4, lambda q, pt, ptb, hf=hf: I('act', 'activation', [ptb], [qTab], out=qTa[:, hf * 4 + q, :], in_=pt[:, :], func=AF.Copy, scale=0.125))
            pop_tail()
        for hf in range(2):
            wt, wtb = load_wchunk(C_KA + hf * 512, 512)
            proj_fm(wt, wtb, 128, 4, lambda q, pt, ptb, hf=hf: I('dve', 'tensor_copy', [ptb], [kT_b], out=kT_t[:, hf * 4 + q, :], in_=pt[:, :]))
            pop_tail()
        for hf in range(2):
            wt, wtb = load_wchunk(C_VA + hf * 512, 512)
            proj_tm(wt, wtb, lambda j, pt, ptb, hf=hf: I('act', 'activation', [ptb], [va_b], out=va_t[:, j, hf * 8:(hf + 1) * 8, 0:64],
                                                           in_=pt[:, :].rearrange("p (h d) -> p h d", h=8), func=AF.Copy))
            pop_tail()
        for hf in range(2):
            wt, wtb = load_wchunk(C_GB + hf * 512, 512)
            proj_tm(wt, wtb, lambda j, pt, ptb, hf=hf: I('act', 'activation', [ptb], [sgbb], out=sgb[:, j, hf * 512:(hf + 1) * 512], in_=pt[:, :], func=AF.Sigmoid))
            pop_tail()
        while pending_tail:
            pending_tail.pop(0)()

        for j in range(4):
            i = 4 * s + j
            tok = slice(j * 128, (j + 1) * 128)
            hl_t, hl_b = hld[i % 2]
            P.dma('sp', hl_t[:], ha_d[i * 128:(i + 1) * 128, :], writes=[hl_b], dsem=dshl[i % 2])
            if s + 1 < NSB:
                load_x(4 * (s + 1) + j)
            kbis = [kbi for kbi in range(5) if i - 4 + kbi >= 0]
            lo = kbis[0]

            def QK(h, i=i, tok=tok, kbis=kbis):
                hp, hh = h // 2, h % 2
                r0 = hh * 64
                xb_ = 3 + hh
                for kbi in kbis:
                    kb = i - 4 + kbi
                    kslot_t, kslot_b = kTa[(kb // 4) % 2]
                    koff = (kb % 4) * 128
                    if kbi < 4:
                        reg = ps[xb_][:, kbi * 128:(kbi + 1) * 128]
                        regb = psb[xb_]
                    else:
                        reg = ps[YB[hh]][:, 0:128]
                        regb = psb[YB[hh]]
                    I('pe', 'matmul', [cBb, BMTb], [regb], reg, lhsT=ident, rhs=BMT[:, h, kbi, :], start=True, stop=False)
                    I('pe', 'matmul', [kslot_b, qTab], [regb], reg, lhsT=kslot_t[r0:r0 + 64, hp, koff:koff + 128],
                      rhs=qTa[r0:r0 + 64, hp, tok], start=False, stop=True)

            def EXPPV(h, i=i, kbis=kbis, lo=lo):
                hh = h % 2
                xb_ = 3 + hh
                pt_t, pt_b = PT[hh]
                if lo < 4:
                    I('act', 'activation', [psb[xb_]], [pt_b], out=pt_t[:, lo * 128:512], in_=ps[xb_][:, lo * 128:512], func=AF.Exp)
                I('act', 'activation', [psb[YB[hh]]], [pt_b], out=pt_t[:, 512:640], in_=ps[YB[hh]][:, 0:128], func=AF.Exp)
                hq = h % 4
                for kbi in kbis:
                    kb = i - 4 + kbi
                    vslot_t, vslot_b = vaug[(kb // 4) % 2]
                    I('pe', 'matmul', [pt_b, vslot_b], [psb[6]], ps[6][:, hq * 65:(hq + 1) * 65], lhsT=pt_t[:, kbi * 128:(kbi + 1) * 128],
                      rhs=vslot_t[:, kb % 4, h, :], start=(kbi == kbis[0]), stop=(kbi == kbis[-1]))
                if hq == 3:
                    o3 = ps[6][:, 0:260].rearrange("p (h d) -> p h d", h=4)
                    I('dve', 'reciprocal', [psb[6]], [rdenb], out=rden[:], in_=o3[:, :, 64])
                    for q4 in range(4):
                        hh4 = h - 3 + q4
                        I('dve', 'tensor_scalar', [psb[6], rdenb], [ybb], out=yb[:, hh4 * 64:(hh4 + 1) * 64], in0=ps[6][:, q4 * 65:q4 * 65 + 64],
                          scalar1=rden[:, q4:q4 + 1], scalar2=None, op0=ALU.mult)

            if QK_AHEAD:
                QK(0)
            for h in range(16):
                if QK_AHEAD:
                    if h + 1 < 16:
                        QK(h + 1)
                else:
                    QK(h)
                EXPPV(h)
                if pending_tail and h in (1, 3, 5, 8, 11, 14):
                    pending_tail.pop(0)()
            while pending_tail:
                pending_tail.pop(0)()
            I('dve', 'tensor_tensor', [ybb, sgbb], [ybb], out=yb[:], in0=yb[:], in1=sgb[:, j, :], op=ALU.mult)
            I('dve', 'tensor_tensor', [ybb, hl_b], [hmb], out=hm[:], in0=yb[:], in1=hl_t[:], op=ALU.add)
            if dbg:
                I('dve', 'tensor_tensor', [ybb, hl_b], [ybb], out=yb[:], in0=yb[:], in1=hl_t[:], op=ALU.add)
                P.dma('sp', dbg_d["h"][i * 128:(i + 1) * 128, :], yb[:], reads=[ybb], dsem=dbgsem())

            def T1(i=i):
                pT = psbf(2)
                for kc in range(8):
                    I('pe', 'transpose', [hmb, cBb], [psb[2]], out=pT[:, kc * 128:(kc + 1) * 128], in_=hm[:, kc * 128:(kc + 1) * 128], identity=ident)
                I('act', 'activation', [psb[2]], [hTb], out=hT[:], in_=pT.rearrange("p (a b) -> p a b", a=8), func=AF.Copy)
                P.dma('sp', xres[:], x_d[i * 128:(i + 1) * 128, :], writes=[xresb], dsem=dsxr)

            def T2(i=i):
                for hf in range(2):
                    for kc in range(8):
                        I('pe', 'matmul', [hTb, Woutb], [psb[hf]], ps[hf][:, :], lhsT=hT[:, kc, :], rhs=Wout[:, kc, hf * 512:(hf + 1) * 512],
                          start=(kc == 0), stop=(kc == 7))
                    I('dve', 'tensor_tensor', [psb[hf], xresb], [x1tb], out=x1t[:, hf * 512:(hf + 1) * 512], in0=ps[hf][:, :],
                      in1=xres[:, hf * 512:(hf + 1) * 512], op=ALU.add)
                P.dma('sp', x1_d[i * 128:(i + 1) * 128, :], x1t[:], reads=[x1tb], dsem=dsx1)
                r = rms_rstd(x1t[:], x1tb, D, 0)
                I('dve', 'scalar_tensor_tensor', [x1tb, ssb, lnmoeb], [xm32b], out=xm32[:], in0=x1t[:], scalar=r, in1=lnmoe[:], op0=ALU.mult, op1=ALU.mult)
                xmb_t, xmb_b = xmb[i % 2]
                I('act', 'activation', [xm32b], [xmb_b], out=xmb_t[:], in_=xm32[:], func=AF.Copy)
                P.dma('sp', xm_d[i * 128:(i + 1) * 128, :], xmb_t[:], reads=[xmb_b], dsem=dsxm[i % 2])

            def T3(i=i):
                for kc in range(8):
                    bk = kc // 4
                    I('pe', 'transpose', [xm32b, cAb], [psb[bk]], out=ps[bk][:, (kc % 4) * 128:(kc % 4 + 1) * 128], in_=xm32[:, kc * 128:(kc + 1) * 128], identity=identf)
                I('act', 'activation', [psb[0]], [xmT32b], out=xmT32[:, 0:4, :], in_=ps[0][:, :].rearrange("p (a b) -> p a b", a=4), func=AF.Copy)
                I('dve', 'tensor_copy', [psb[1]], [xmT32b], out=xmT32[:, 4:8, :], in_=ps[1][:, :].rearrange("p (a b) -> p a b", a=4))

            def T4(i=i):
                for kc in range(8):
                    I('pe', 'matmul', [xmT32b, wr32b], [psb[7]], ps[7][:, 0:32], lhsT=xmT32[:, kc, :], rhs=wr32[:, kc, :], start=(kc == 0), stop=(kc == 7))
                I('dve', 'tensor_tensor', [psb[7], broutb], [lgb], out=lg[:], in0=ps[7][:, 0:32], in1=brout[:], op=ALU.add)
                if dbg:
                    P.dma('sp', dbg_d["lg"][i * 128:(i + 1) * 128, :], lg[:], reads=[lgb], dsem=dbgsem())
                I('dve', 'max', [lgb], [m8b], out=m8[:], in_=lg[:])
                I('dve', 'max_index', [lgb, m8b], [i8b], out=i8[:], in_max=m8[:], in_values=lg[:])
                I('dve', 'tensor_copy', [i8b], [i8fb], out=i8f[:], in_=i8[:])
                for k in range(4):
                    I('dve', 'tensor_scalar', [cAb, i8fb], [oh4b], out=oh4[:, k, :], in0=tvals[:, 0:32], scalar1=i8f[:, k:k + 1], scalar2=None, op0=ALU.is_equal)
                I('dve', 'tensor_tensor', [oh4b], [maskb], out=mask[:], in0=oh4[:, 0, :], in1=oh4[:, 1, :], op=ALU.add)
                I('dve', 'tensor_tensor', [oh4b, maskb], [maskb], out=mask[:], in0=mask[:], in1=oh4[:, 2, :], op=ALU.add)
                I('dve', 'tensor_tensor', [oh4b, maskb], [maskb], out=mask[:], in0=mask[:], in1=oh4[:, 3, :], op=ALU.add)
                I('dve', 'tensor_copy', [maskb], [maskbfb], out=maskbf[:], in_=mask[:])

            def T5(i=i):
                I('pe', 'matmul', [cBb, maskbfb], [psb[7]], ps[7][:, 64:96], lhsT=tristrict, rhs=maskbf[:], start=True, stop=False)
                I('pe', 'matmul', [cBb, cmbfb], [psb[7]], ps[7][:, 64:96], lhsT=ones_bf, rhs=cmbf[:], start=False, stop=True)
                I('dve', 'tensor_scalar', [m8b], [smb], out=sm[:, 0:1], in0=m8[:, 0:1], scalar1=-1.0, scalar2=None, op0=ALU.mult)
                I('act', 'activation', [lgb, smb], [exb], out=ex[:], in_=lg[:], func=AF.Exp, bias=sm[:, 0:1], scale=1.0)
                I('dve', 'tensor_tensor', [exb, maskb], [exb], out=ex[:], in0=ex[:], in1=mask[:], op=ALU.mult)
                I('dve', 'tensor_reduce', [exb], [smb], out=sm[:, 1:2], in_=ex[:], axis=AX.X, op=ALU.add)
                I('dve', 'reciprocal', [smb], [smb], out=sm[:, 2:3], in_=sm[:, 1:2])
                I('dve', 'tensor_scalar', [exb, smb], [gatesb], out=gates[:], in0=ex[:], scalar1=sm[:, 2:3], scalar2=None, op0=ALU.mult)
                I('dve', 'tensor_tensor', [psb[7], cAb], [slotfb], out=slotf[:], in0=ps[7][:, 64:96], in1=eoff, op=ALU.add)
                I('dve', 'tensor_tensor', [cmb, maskb], [cmb], out=cm[:], in0=cm[:], in1=mask[:], op=ALU.add)
                I('dve', 'tensor_copy', [cmb], [cmbfb], out=cmbf[:], in_=cm[:])
                for k in range(4):
                    I('dve', 'tensor_tensor', [oh4b, slotfb], [jk32b], out=jk32[:], in0=oh4[:, k, :], in1=slotf[:], op=ALU.mult)
                    I('dve', 'tensor_reduce', [jk32b], [slot4fb], out=slot4f[:, k:k + 1], in_=jk32[:], axis=AX.X, op=ALU.add)
                    I('dve', 'tensor_tensor', [oh4b, gatesb], [jk32b], out=jk32[:], in0=oh4[:, k, :], in1=gates[:], op=ALU.mult)
                    I('dve', 'tensor_reduce', [jk32b], [gate4b], out=gate4[:, i, k:k + 1], in_=jk32[:], axis=AX.X, op=ALU.add)
                I('dve', 'tensor_copy', [slot4fb], [slots_ib], out=slots_i[:, i, :], in_=slot4f[:])

            if s + 1 < NSB:
                load_norm_A(4 * (s + 1) + j, j, dma=False)
                pending_tail.append(lambda j=j: load_norm_B(j, lnb, lnbb))
            pending_tail.extend([T1, T2, T3, T4, T5])
            if not DEFER_TAIL or (j == 3 and s + 1 == NSB):
                while pending_tail:
                    pending_tail.pop(0)()
            elif j == 3:
                pending_tail.pop(0)()
    P.barrier()
    esB.close()
    esAB.close()

    esD = ExitStack()
    NK = NB * 4
    cnt, cntb_ = sb("cnt", [128, 32], F32, stack=esD)
    cnti, cntib = sb("cnti", [128, 32], I32, stack=esD)
    ntl, ntlb = sb("ntl", [128, 32], F32, stack=esD)
    cinc, cincb = sb("cinc", [128, 32], F32, stack=esD)
    offs, offsb = sb("offs", [128, 32], F32, stack=esD)
    s_e, s_eb = sb("s_e", [128, NK], I32, stack=esD)
    s_p, s_pb = sb("s_p", [128, NK], I32, stack=esD)
    f_e, f_eb = sb("f_e", [128, NK], F32, stack=esD)
    f_p, f_pb = sb("f_p", [128, NK], F32, stack=esD)
    f_t, f_tb = sb("f_t", [128, NK], F32, stack=esD)
    xml = [sb("xml%d" % i, [128, D], BF16, stack=esD) for i in range(4)]
    dsxl = [DSem(P) for _ in range(4)]
    I('pe', 'matmul', [cBb, cmbfb], [psb[7]], ps[7][:, 0:32], lhsT=ones_bf, rhs=cmbf[:], start=True, stop=True)
    I('dve', 'tensor_scalar', [psb[7]], [cntb_], out=cnt[:], in0=ps[7][:, 0:32], scalar1=511.0, scalar2=None, op0=ALU.add)
    I('dve', 'tensor_copy', [cntb_], [cntib], out=cnti[:], in_=cnt[:])
    I('dve', 'tensor_single_scalar', [cntib], [cntib], out=cnti[:], in_=cnti[:], scalar=9, op=ALU.arith_shift_right)
    I('dve', 'tensor_copy', [cntib], [ntlb], out=ntl[:], in_=cnti[:])
    I('dve', 'tensor_tensor_scan', [ntlb, cAb], [cincb], out=cinc[:], data0=c_ones[:, 0:32], data1=ntl[:], initial=0.0, op0=ALU.mult, op1=ALU.add)
    I('dve', 'tensor_tensor', [cincb, ntlb], [offsb], out=offs[:], in0=cinc[:], in1=ntl[:], op=ALU.subtract)
    I('dve', 'tensor_scalar', [offsb], [offsb], out=offs[:], in0=offs[:], scalar1=512.0, scalar2=None, op0=ALU.mult)
    sl2 = slots_i[:].rearrange("p a b -> p (a b)")
    I('dve', 'tensor_single_scalar', [slots_ib], [s_eb], out=s_e[:], in_=sl2, scalar=LOGCAP, op=ALU.arith_shift_right)
    I('dve', 'tensor_single_scalar', [slots_ib], [s_pb], out=s_p[:], in_=sl2, scalar=CAP - 1, op=ALU.bitwise_and)
    I('dve', 'tensor_copy', [s_eb], [f_eb], out=f_e[:], in_=s_e[:])
    I('dve', 'tensor_copy', [s_pb], [f_pb], out=f_p[:], in_=s_p[:])
    for e_ in range(32):
        I('dve', 'tensor_scalar', [f_eb, offsb], [f_tb], out=f_t[:], in0=f_e[:], scalar1=float(e_), scalar2=offs[:, e_:e_ + 1], op0=ALU.is_equal, op1=ALU.mult)
        I('dve', 'tensor_tensor', [f_tb, f_pb], [f_pb], out=f_p[:], in0=f_p[:], in1=f_t[:], op=ALU.add)
    I('dve', 'tensor_copy', [f_pb], [slots_ib], out=sl2, in_=f_p[:])
    for i in range(NB):
        xl_t, xl_b = xml[i % 4]
        P.dma('sp', xl_t[:], xm_d[i * 128:(i + 1) * 128, :], writes=[xl_b], dsem=dsxl[i % 4])
        for k in range(4):
            P.op('pool', lambda e, k=k, i=i, xl_t=xl_t: e.indirect_dma_start(
                out=xs_d, out_offset=bass.IndirectOffsetOnAxis(ap=slots_i[:, i, k:k + 1], axis=0), in_=xl_t[:], in_offset=None),
                reads=[xl_b, slots_ib], writes=[], dsem=dsxl[i % 4], is_dma=True)
    P.barrier()
    if stop_after <= 2:
        if dbg:
            dump("slots", slots_i[:], slots_ib, [128, NB, 4], I32)
            dump("gate4", gate4[:], gate4b, [128, NB, 4])
            dump("cm", cm[:], cmb, [128, 32])
        P.emit(final_waits=[d for d in P.dsems if d.count > 0])
        es.close()
        return nc

    esE = ExitStack()
    cmp_, cmpb = sb("cmp", [128, 32], F32, stack=esE)
    ET, ETb = sb("ET", [128, NT], F32, stack=esE)
    CEX, CEXb = sb("CEX", [128, NT], F32, stack=esE)
    JT, JTb = sb("JT", [128, NT], F32, stack=esE)
    EC, ECb = sb("EC", [128, NT], F32, stack=esE)
    BASE, BASEb = sb("BASE", [128, NT], F32, stack=esE)
    idxf, idxfb = sb("idxf", [128, NT, 12], F32, stack=esE)
    idxi, idxib = sb("idxi", [128, NT, 12], I32, stack=esE)
    OH, OHb = sb("OH", [32, NT], F32, stack=esE)
    ones512, ones512b = sb("ones512", [32, 512], F32, stack=esE)
    ohb, ohbb = sb("ohb", [32, 512], BF16, stack=esE)
    b1T = [sb("b1T%d" % i, [128, 16], F32, stack=esE) for i in range(2)]
    dsB1 = [DSem(P) for _ in range(2)]
    xsT2 = [sb("xsT%d" % i, [128, 8, 512], BF16, stack=esE) for i in range(2)]
    b2n, b2nb = sb("b2n", [32, D], BF16, stack=esE)
    W1t = [sb("W1t%d" % i, [128, 8, 2048], BF16, stack=esE) for i in range(2)]
    W2t = [sb("W2t%d" % i, [128, 8, D], BF16, stack=esE) for i in range(2)]
    xst = [sb("xst%d" % i, [128, 4, D], BF16, stack=esE) for i in range(2)]
    dsW1 = [DSem(P) for _ in range(2)]
    dsW2 = [DSem(P) for _ in range(2)]
    dsXs = [DSem(P) for _ in range(2)]
    actT, actTb = sb("actT", [128, 8, 512], BF16, stack=esE)
    gbuf = [sb("gb%d" % i, [128, 512], F32, stack=esE) for i in range(2)]
    sgbuf = [sb("sgb%d" % i, [128, 512], F32, stack=esE) for i in range(2)]
    lbuf = [sb("lb%d" % i, [128, 512], F32, stack=esE) for i in range(2)]
    yst = [sb("yst%d" % i, [128, D], F32, stack=esE) for i in range(2)]
    dsYs = [DSem(P) for _ in range(2)]
    ds_e = DSem(P)
    P.dma('pool', b2n[:], b2_d, writes=[b2nb], dsem=ds_e)
    I('dve', 'memset', [], [ones512b], ones512[:], 1.0)
    for i2 in range(2):
        I('dve', 'memset', [], [b1T[i2][1]], b1T[i2][0][:], 0.0)
    for t in range(NT):
        I('dve', 'tensor_scalar', [cincb], [cmpb], out=cmp_[:], in0=cinc[:], scalar1=float(t), scalar2=None, op0=ALU.is_le)
        I('dve', 'tensor_reduce', [cmpb], [ETb], out=ET[:, t:t + 1], in_=cmp_[:], axis=AX.X, op=ALU.add)
    I('dve', 'tensor_scalar', [ETb], [ECb], out=EC[:], in0=ET[:], scalar1=31.0, scalar2=128.0, op0=ALU.min, op1=ALU.mult)
    for t in range(NT):
        I('dve', 'tensor_scalar', [cAb, ECb], [idxfb], out=idxf[:, t, 4:12], in0=pidx.to_broadcast([128, 8]), scalar1=EC[:, t:t + 1], scalar2=None, op0=ALU.add)
        I('dve', 'tensor_scalar', [cAb, ECb], [idxfb], out=idxf[:, t, 0:4], in0=pc4, scalar1=0.0, scalar2=None, op0=ALU.add)
    I('dve', 'tensor_copy', [idxfb], [idxib], out=idxi[:], in_=idxf[:])
    I('dve', 'tensor_scalar', [ETb, cAb], [OHb], out=OH[:], in0=ET[0:32, :], scalar1=pidx[0:32, 0:1], scalar2=None, op0=ALU.is_equal)
    if dbg:
        dump("ET", ET[:], ETb, [128, NT])
        dump("idxi", idxi[:], idxib, [128, NT, 12], I32)
        dump("ntl", ntl[:], ntlb, [128, 32])

    def issue_gathers(t):
        k = t % 2
        P.dma('sp', xst[k][0][:], xs_d[t * 512:(t + 1) * 512, :].rearrange("(c p) d -> p c d", p=128), writes=[xst[k][1]], dsem=dsXs[k])
        P.op('pool', lambda e, t=t, k=k: e.indirect_dma_start(
            out=b1T[k][0][:], out_offset=None, in_=b1_d, in_offset=bass.IndirectOffsetOnAxis(ap=idxi[:, t, 4:5], axis=0)),
            reads=[idxib], writes=[b1T[k][1]], dsem=dsB1[k], is_dma=True)
        P.op('pool', lambda e, t=t, k=k: e.indirect_dma_start(
            out=W1t[k][0][:].rearrange("p a b -> p (a b)"), out_offset=None, in_=w1_d, in_offset=bass.IndirectOffsetOnAxis(ap=idxi[:, t, 4:5], axis=0)),
            reads=[idxib], writes=[W1t[k][1]], dsem=dsW1[k], is_dma=True)
        P.op('pool', lambda e, t=t, k=k: e.indirect_dma_start(
            out=W2t[k][0][:].rearrange("p a b -> p (a b)"), out_offset=None, in_=w2_d, in_offset=bass.IndirectOffsetOnAxis(ap=idxi[:, t, 4:5], axis=0)),
            reads=[idxib], writes=[W2t[k][1]], dsem=dsW2[k], is_dma=True)

    def do_transposes(t):
        k = t % 2
        xs_t, xs_b = xst[k]
        xsT, xsTb = xsT2[k]
        for c in range(4):
            bk = 6 + (c % 2)
            pT = psbf(bk)
            for kc in range(8):
                I('pe', 'transpose', [xs_b, cBb], [psb[bk]], out=pT[:, kc * 128:(kc + 1) * 128], in_=xs_t[:, c, kc:D:8], identity=ident)
            if c % 2 == 0:
                I('act', 'activation', [psb[bk]], [xsTb], out=xsT[:, :, c * 128:(c + 1) * 128], in_=pT.rearrange("p (a b) -> p a b", a=8), func=AF.Copy)
            else:
                I('dve', 'tensor_copy', [psb[bk]], [xsTb], out=xsT[:, :, c * 128:(c + 1) * 128], in_=pT.rearrange("p (a b) -> p a b", a=8))

    issue_gathers(0)
    do_transposes(0)
    ysn = 0
    for t in range(NT):
        k = t % 2
        if t + 1 < NT:
            issue_gathers(t + 1)
        w1_t, w1_b = W1t[k]
        w2_t, w2_b = W2t[k]
        b1_t, b1_b = b1T[k]
        xsT, xsTb = xsT2[k]
        I('dve', 'tensor_scalar', [ones512b, OHb], [ohbb], out=ohb[:], in0=ones512[:], scalar1=OH[:, t:t + 1], scalar2=None, op0=ALU.mult)
        I('dve', 'tensor_scalar', [b1_b], [b1_b], out=b1_t[:, 1:16:2], in0=b1_t[:, 1:16:2], scalar1=1.0, scalar2=None, op0=ALU.add)
        for fj in range(8):
            bA = (fj % 2) * 2
            bB = bA + 1
            for (bk, off) in ((bA, 0), (bB, 1)):
                for kc in range(8):
                    I('pe', 'matmul', [w1_b, xsTb], [psb[bk]], ps[bk][:, :], lhsT=w1_t[:, kc, 2 * fj + off:2048:16], rhs=xsT[:, kc, :],
                      start=(kc == 0), stop=(kc == 7))
            g_t, g_b = gbuf[fj % 2]
            s_t, s_b = sgbuf[fj % 2]
            l_t, l_b = lbuf[fj % 2]
            I('dve', 'tensor_scalar', [psb[bA], b1_b], [g_b], out=g_t[:], in0=ps[bA][:, :], scalar1=b1_t[:, 2 * fj:2 * fj + 1], scalar2=7.0, op0=ALU.add, op1=ALU.min)
            I('act', 'activation', [g_b], [s_b], out=s_t[:], in_=g_t[:], func=AF.Sigmoid, scale=1.702)
            I('act', 'activation', [psb[bB], b1_b], [l_b], out=l_t[:], in_=ps[bB][:, :], func=AF.Identity, bias=b1_t[:, 2 * fj + 1:2 * fj + 2], scale=1.0)
            I('dve', 'tensor_scalar', [l_b], [l_b], out=l_t[:], in0=l_t[:], scalar1=8.0, scalar2=-6.0, op0=ALU.min, op1=ALU.max)
            I('dve', 'tensor_tensor', [l_b, g_b], [l_b], out=l_t[:], in0=l_t[:], in1=g_t[:], op=ALU.mult)
            I('dve', 'tensor_tensor', [l_b, s_b], [actTb], out=actT[:, fj, :], in0=l_t[:], in1=s_t[:], op=ALU.mult)
        if t + 1 < NT:
            do_transposes(t + 1)
        for c in range(4):
            ys_t, ys_b = yst[ysn % 2]
            dsy = dsYs[ysn % 2]
            ysn += 1
            for hf in range(2):
                bk = 4 + hf
                for fj in range(8):
                    I('pe', 'matmul', [actTb, w2_b], [psb[bk]], ps[bk][:, :], lhsT=actT[:, fj, c * 128:(c + 1) * 128], rhs=w2_t[:, fj, hf * 512:(hf + 1) * 512],
                      start=(fj == 0), stop=False)
                I('pe', 'matmul', [ohbb, b2nb], [psb[bk]], ps[bk][:, :], lhsT=ohb[0:32, 0:128], rhs=b2n[0:32, hf * 512:(hf + 1) * 512], start=False, stop=True)
                if hf == 0:
                    I('act', 'activation', [psb[bk]], [ys_b], out=ys_t[:, 0:512], in_=ps[bk][:, :], func=AF.Copy)
                else:
                    I('dve', 'tensor_copy', [psb[bk]], [ys_b], out=ys_t[:, 512:1024], in_=ps[bk][:, :])
            P.dma('sp', ys_d[t * 512 + c * 128:t * 512 + (c + 1) * 128, :], ys_t[:], reads=[ys_b], dsem=dsy)
    P.barrier()
    esE.close()
    esD.close()
    if stop_after <= 3 and False:
        pass

    esC = ExitStack()
    Wpg, Wpgb = sb("Wpg", [128, 8, D], BF16, stack=esC)
    Wpp, Wppb = sb("Wpp", [128, 2, D], BF16, stack=esC)
    lnfin, lnfinb = sb("lnfin", [128, D], F32, stack=esC)
    lnbp, lnbpb = sb("lnbp", [128, 8, 128], F32, stack=esC)
    ds_p = DSem(P)
    P.dma('pool', Wpg[:], w_pg_d.rearrange("(kc p) c -> p kc c", p=128), writes=[Wpgb], dsem=ds_p)
    P.dma('pool', Wpp[:], w_pp_d.rearrange("(kc p) c -> p kc c", p=128), writes=[Wppb], dsem=ds_p)
    P.dma('sp', lnfin[:], lnfin_d, writes=[lnfinb], dsem=ds_p)
    for kc in range(8):
        I('dve', 'tensor_scalar', [cAb, lnpleTb], [lnbpb], out=lnbp[:, kc, :], in0=c_ones, scalar1=lnpleT[:, kc:kc + 1], scalar2=None, op0=ALU.mult)
    NP3 = 3
    x1l = [sb("x1l%d" % i, [128, D], F32, stack=esC) for i in range(NP3)]
    dsx1l = [DSem(P) for _ in range(NP3)]
    yk = [[sb("yk%d_%d" % (i, k), [128, D], F32, stack=esC) for k in range(4)] for i in range(NP3)]
    dsyk = [DSem(P) for _ in range(NP3)]
    pb32 = [sb("pb32_%d" % i, [128, 256], F32, stack=esC) for i in range(NP3)]
    dspb = [DSem(P) for _ in range(NP3)]
    pbl = [sb("pbl%d" % i, [128, 256], BF16, stack=esC) for i in range(2)]
    xp, xpb = sb("xp", [128, D], BF16, stack=esC)
    xpTs = [sb("xpT%d" % i, [128, 8, 128], BF16, stack=esC) for i in range(2)]
    pTs = [sb("pTt%d" % i, [128, 2, 128], BF16, stack=esC) for i in range(2)]
    sgp, sgpb = sb("sgp", [128, 512], F32, stack=esC)
    x3, x3b = sb("x3", [128, D], F32, stack=esC)
    ot = [sb("ot%d" % i, [128, D], F32, stack=esC) for i in range(2)]
    dso = [DSem(P) for _ in range(2)]

    nhalf, nhalfb = sb("nhalf", [128, 1], F32, stack=esC)
    I('dve', 'memset', [], [nhalfb], nhalf[:], -0.5)

    def rms_rstd_pow(src_ap, srcb, n, col):
        I('act', 'activation', [srcb], [junkb], out=junk[:, 0:n], in_=src_ap, func=AF.Square)
        I('dve', 'tensor_reduce', [junkb], [ssb], out=ss[:, col:col + 1], in_=junk[:, 0:n], axis=AX.X, op=ALU.add)
        I('dve', 'tensor_scalar', [ssb], [ssb], out=ss[:, col + 1:col + 2], in0=ss[:, col:col + 1], scalar1=1.0 / n, scalar2=EPS, op0=ALU.mult, op1=ALU.add)
        I('pool', 'tensor_tensor', [ssb, nhalfb], [ssb], out=ss[:, col + 2:col + 3], in0=ss[:, col + 1:col + 2], in1=nhalf[:], op=ALU.pow)
        return ss[:, col + 2:col + 3]

    def stageL(i):
        x1_t, x1_b = x1l[i % NP3]
        P.dma('sp', x1_t[:], x1_d[i * 128:(i + 1) * 128, :], writes=[x1_b], dsem=dsx1l[i % NP3])
        P.dma('sp', pb32[i % NP3][0][:], p_d[i * 128:(i + 1) * 128, :], writes=[pb32[i % NP3][1]], dsem=dspb[i % NP3])
        for k in range(4):
            P.op('pool', lambda e, k=k, i=i: e.indirect_dma_start(
                out=yk[i % NP3][k][0][:], out_offset=None, in_=ys_d, in_offset=bass.IndirectOffsetOnAxis(ap=slots_i[:, i, k:k + 1], axis=0)),
                reads=[slots_ib], writes=[yk[i % NP3][k][1]], dsem=dsyk[i % NP3], is_dma=True)

    def stageA(i):
        x1_t, x1_b = x1l[i % NP3]
        pb_t, pb_b = pbl[i % 2]
        I('act', 'activation', [pb32[i % NP3][1]], [pb_b], out=pb_t[:], in_=pb32[i % NP3][0][:], func=AF.Copy)
        for k in range(4):
            y_t, y_b = yk[i % NP3][k]
            I('dve', 'scalar_tensor_tensor', [y_b, gate4b, x1_b], [x1_b], out=x1_t[:], in0=y_t[:], scalar=gate4[:, i, k:k + 1], in1=x1_t[:], op0=ALU.mult, op1=ALU.add)
        if dbg:
            P.dma('sp', dbg_d["x2"][i * 128:(i + 1) * 128, :], x1_t[:], reads=[x1_b], dsem=DSem(P))
        r = rms_rstd_pow(x1_t[:], x1_b, D, 0)
        I('dve', 'tensor_scalar', [x1_b, ssb], [xpb], out=xp[:], in0=x1_t[:], scalar1=r, scalar2=None, op0=ALU.mult)
        xpT, xpTb = xpTs[i % 2]
        pT_, pTb_ = pTs[i % 2]
        pT = psbf(2)
        for kc in range(8):
            I('pe', 'transpose', [xpb, cBb], [psb[2]], out=pT[:, kc * 128:(kc + 1) * 128], in_=xp[:, kc * 128:(kc + 1) * 128], identity=ident)
        I('dve', 'tensor_tensor', [psb[2], lnbpb], [xpTb], out=xpT[:], in0=pT.rearrange("p (a b) -> p a b", a=8), in1=lnbp[:], op=ALU.mult)
        pT5 = psbf(5)
        for kc in range(2):
            I('pe', 'transpose', [pb_b, cBb], [psb[5]], out=pT5[:, kc * 128:(kc + 1) * 128], in_=pb_t[:, kc * 128:(kc + 1) * 128], identity=ident)
        I('act', 'activation', [psb[5]], [pTb_], out=pT_[:], in_=pT5[:, 0:256].rearrange("p (a b) -> p a b", a=2), func=AF.Copy)

    def stageB(i):
        x1_t, x1_b = x1l[i % NP3]
        xpT, xpTb = xpTs[i % 2]
        pT_, pTb_ = pTs[i % 2]
        for hf in range(2):
            for kc in range(8):
                I('pe', 'matmul', [xpTb, Wpgb], [psb[hf]], ps[hf][:, :], lhsT=xpT[:, kc, :], rhs=Wpg[:, kc, hf * 512:(hf + 1) * 512], start=(kc == 0), stop=(kc == 7))
            for kc in range(2):
                I('pe', 'matmul', [pTb_, Wppb], [psb[3 + hf]], ps[3 + hf][:, :], lhsT=pT_[:, kc, :], rhs=Wpp[:, kc, hf * 512:(hf + 1) * 512], start=(kc == 0), stop=(kc == 1))
            I('act', 'activation', [psb[hf]], [sgpb], out=sgp[:], in_=ps[hf][:, :], func=AF.Sigmoid)
            I('dve', 'tensor_tensor', [sgpb, psb[3 + hf]], [sgpb], out=sgp[:], in0=sgp[:], in1=ps[3 + hf][:, :], op=ALU.mult)
            I('dve', 'tensor_tensor', [sgpb, x1_b], [x3b], out=x3[:, hf * 512:(hf + 1) * 512], in0=sgp[:], in1=x1_t[:, hf * 512:(hf + 1) * 512], op=ALU.add)
        r = rms_rstd_pow(x3[:], x3b, D, 4)
        o_t, o_b = ot[i % 2]
        I('dve', 'scalar_tensor_tensor', [x3b, ssb, lnfinb], [o_b], out=o_t[:], in0=x3[:], scalar=r, in1=lnfin[:], op0=ALU.mult, op1=ALU.mult)
        P.dma('sp', out_d[i * 128:(i + 1) * 128, :], o_t[:], reads=[o_b], dsem=dso[i % 2])

    stageL(0)
    if NB > 1:
        stageL(1)
    stageA(0)
    for i in range(NB):
        if i + 2 < NB:
            stageL(i + 2)
        if i + 1 < NB:
            stageA(i + 1)
        stageB(i)
    P.emit(final_waits=[d for d in P.dsems if d.count > 0])
    esC.close()
    es.close()
    return nc


def make_consts(T):
    CAP = T
    s = np.arange(128)[:, None]
    t = np.arange(128)[None, :]
    ident = (s == t).astype(np.float32)
    triS = np.where(s <= t, -1.0 / 16.0, 0.0).astype(np.float32)
    tristrict = (s < t).astype(np.float32)
    ones = np.ones((128, 128), np.float32)
    maskA = (s <= t).astype(np.float32)
    maskA4 = np.tile(maskA, (1, 4))
    eoff = np.tile((np.arange(32) * CAP).astype(np.float32)[None, :], (128, 1))
    pk8 = (np.arange(8)[None, :] * 128 + np.arange(128)[:, None]).astype(np.float32)
    pc4 = (np.arange(4)[None, :] * 128 + np.arange(128)[:, None]).astype(np.float32)
    pidx = np.arange(128, dtype=np.float32)[:, None]
    tv = np.tile(np.arange(64, dtype=np.float32)[None, :], (128, 1))
    cA = np.concatenate([ident, triS, tristrict, ones, ident, ones, maskA4, eoff, pk8, pc4, pidx, tv], axis=1)
    maskadd = np.zeros((128, 5, 128), np.float32)
    maskadd[0:64, 0, 64:128] = NEG
    maskadd[64:128, 4, 0:64] = NEG
    return np.ascontiguousarray(cA.astype(np.float32)), maskadd


def make_shared(inputs, T):
    f = lambda a: np.ascontiguousarray(np.asarray(a, dtype=np.float32))
    cA, maskadd = make_consts(T)
    rel_bias = np.asarray(inputs["rel_bias"][0], np.float32)
    k = np.arange(128)[:, None, None]
    kb = np.arange(5)[None, :, None]
    q = np.arange(128)[None, None, :]
    rel = np.clip((512 + q) - (kb * 128 + k), -256, 256) + 256
    biasT = np.transpose(rel_bias[:, rel], (1, 0, 2, 3))
    sh = {
        "w_in": f(inputs["w_in"][0]),
        "wgk_aug": f(np.concatenate([inputs["w_gk"][0], inputs["b_gk"][0][None, :]], axis=0)),
        "biasT": f(biasT),
        "w_out": f(inputs["w_out"][0]),
        "w_router": f(inputs["w_router"][0]),
        "w1": f(np.asarray(inputs["w1"][0]).reshape(32 * 128, 8 * 2048)),
        "b1": f(np.asarray(inputs["b1"][0]).reshape(32 * 128, 16)),
        "w2": f(np.asarray(inputs["w2"][0]).reshape(32 * 128, 8 * D)),
        "b2": f(inputs["b2"][0]),
        "w_pg": f(inputs["w_ple_gate"][0]),
        "w_pp": f(inputs["w_ple_proj"][0]),
        "lnmixT": f(np.asarray(inputs["ln_mix"][0]).reshape(8, 128).T),
        "lnpleT": f(np.asarray(inputs["ln_ple"][0]).reshape(8, 128).T),
        "lnmoe_b": f(np.broadcast_to(np.asarray(inputs["ln_moe"][0])[None, :], (128, D))),
        "lnfin_b": f(np.broadcast_to(np.asarray(inputs["ln_final"])[None, :], (128, D))),
        "gnorm_b": f(np.broadcast_to(np.asarray(inputs["gla_norm"][0])[None, :], (128, 256))),
        "brout_b": f(np.broadcast_to(np.asarray(inputs["b_router"][0])[None, :], (128, 32))),
        "cA": cA,
        "maskadd": maskadd,
    }
    return sh


def kernel(**inputs):
    x = np.asarray(inputs["x"], np.float32)
    p = np.asarray(inputs["p"], np.float32)
    Bn, T, _ = x.shape
    nc = build_program(T)
    sh = make_shared(inputs, T)
    in_maps = []
    for c in range(Bn):
        m = dict(sh)
        m["x"] = np.ascontiguousarray(x[c])
        m["p"] = np.ascontiguousarray(p[0, c])
        in_maps.append(m)
    res = run_bass_kernel_spmd(nc, in_maps, core_ids=list(range(Bn)))
    return np.stack([np.asarray(r["out"], np.float32) for r in res.results], axis=0)
```

```python
import numpy as np
from contextlib import ExitStack
import concourse.bass as bass
import concourse.mybir as mybir
from concourse.bass_utils import run_bass_kernel_spmd

F32 = mybir.dt.float32
BF16 = mybir.dt.bfloat16
I32 = mybir.dt.int32
U32 = mybir.dt.uint32
AF = mybir.ActivationFunctionType
ALU = mybir.AluOpType
AX = mybir.AxisListType


class Buf:
    __slots__ = ("name", "last_w", "readers")

    def __init__(self, name):
        self.name = name
        self.last_w = None
        self.readers = []


class DSem:
    def __init__(self, prog):
        self.prog = prog
        self.count = 0
        self.handle = prog.new_sem()
        prog.dsems.append(self)


class Op:
    __slots__ = ("eng", "fn", "cdeps", "ddeps", "is_dma", "dsem", "needs_inc", "inc_val", "dma_val")


class Prog:
    ENGS = ("pe", "act", "dve", "pool", "sp")

    def __init__(self, nc, same_engine_sync=True):
        self.nc = nc
        self.es = ExitStack()
        self.q = {e: [] for e in self.ENGS}
        self.esem = {}
        self.same_engine_sync = same_engine_sync
        self.nsem = 0
        self.dsems = []
        self.pending = {}
        for e in self.ENGS:
            self.esem[e] = self.new_sem()

    def new_sem(self):
        self.nsem += 1
        return self.es.enter_context(self.nc.semaphore("s%d" % self.nsem))

    def op(self, eng, fn, reads=(), writes=(), dsem=None, is_dma=False):
        o = Op()
        o.eng = eng
        o.fn = fn
        o.is_dma = is_dma
        o.dsem = dsem
        o.needs_inc = False
        o.inc_val = None
        o.dma_val = None
        cdeps = set()
        ddeps = {}

        def add_dep(p):
            if p is None or p is o:
                return
            if p.is_dma:
                ds = p.dsem
                ddeps[id(ds)] = (ds, ds.count)
            else:
                if p.eng == eng and not is_dma and (eng == "pe" or not self.same_engine_sync):
                    return
                cdeps.add(p)

        pend = self.pending.pop(eng, None)
        if pend is not None:
            for p in pend[0]:
                if p.eng != eng or self.same_engine_sync:
                    if not (p.eng == eng and eng == "pe"):
                        cdeps.add(p)
            for ds, v in pend[1]:
                ddeps[id(ds)] = (ds, v)
        for b in reads:
            add_dep(b.last_w)
        for b in writes:
            add_dep(b.last_w)
            for r in b.readers:
                add_dep(r)
        for b in reads:
            b.readers.append(o)
        for b in writes:
            b.last_w = o
            b.readers = []
        for p in cdeps:
            p.needs_inc = True
        o.cdeps = cdeps
        o.ddeps = list(ddeps.values())
        if is_dma:
            dsem.count += 16
            o.dma_val = dsem.count
        self.q[eng].append(o)
        return o

    def barrier(self):
        last = []
        for e in self.ENGS:
            for o in reversed(self.q[e]):
                if not o.is_dma:
                    last.append(o)
                    break
        dsv = [(ds, ds.count) for ds in self.dsems if ds.count > 0]
        for e in self.ENGS:
            self.pending[e] = (last, dsv)

    def dma(self, eng, out, in_, reads=(), writes=(), dsem=None):
        return self.op(eng, lambda e: e.dma_start(out=out, in_=in_), reads=reads, writes=writes,
                       dsem=dsem, is_dma=True)

    def emit(self, final_waits=()):
        nc = self.nc
        for e in self.ENGS:
            c = 0
            for o in self.q[e]:
                if o.needs_inc and not o.is_dma:
                    c += 1
                    o.inc_val = c
        engmap = {"pe": "tensor", "act": "scalar", "dve": "vector", "pool": "gpsimd", "sp": "sync"}
        with nc.Block() as block:
            for e in self.ENGS:
                ops = self.q[e]
                esem = self.esem
                fw = final_waits if e == "sp" else ()

                def body(eng, ops=ops, e=e, fw=fw):
                    waited = {}
                    for o in ops:
                        waits = []
                        for p in o.cdeps:
                            waits.append((esem[p.eng], p.inc_val))
                        for ds, v in o.ddeps:
                            waits.append((ds.handle, v))
                        for h, v in waits:
                            k = id(h)
                            if waited.get(k, 0) >= v:
                                continue
                            waited[k] = v
                            eng.wait_ge(h, v)
                        ins = o.fn(eng)
                        if o.is_dma:
                            ins.then_inc(o.dsem.handle, 16)
                        elif o.needs_inc:
                            ins.then_inc(esem[e], 1)
                    for ds in fw:
                        eng.wait_ge(ds.handle, ds.count)

                getattr(block, engmap[e])(body)
        self.es.close()


D = 1024
DIN = 8208
EPS = 1e-6
NEG = -30000.0
DEFER_TAIL = True
QK_AHEAD = True
C_QG, C_KG, C_VG, C_GK, C_RG, C_QA, C_KA, C_VA, C_GA, C_GB = 0, 512, 1024, 2048, 2064, 3088, 4112, 5136, 6160, 7184


def build_program(T, dbg=False, stop_after=99, fill=0.0):
    NB = T // 128
    NSB = T // 512
    CAP = T
    NT = (4 * T) // 512 + 31
    NROWS = 32 * CAP + NT * 512
    nc = bass.Bass("TRN2", target_bir_lowering=False)

    def din(name, shape, dt=F32):
        return nc.dram_tensor(name, shape, dt, kind="ExternalInput").ap()

    def dscr(name, shape, dt, out=False):
        return nc.dram_tensor(name, shape, dt, kind=("ExternalOutput" if out else "Internal")).ap()

    x_d = din("x", [T, D])
    p_d = din("p", [T, 256])
    w_in = din("w_in", [D, DIN])
    wgk_d = din("wgk_aug", [17, 512])
    biasT_d = din("biasT", [128, 16, 5, 128])
    w_out_d = din("w_out", [D, D])
    w_router_d = din("w_router", [D, 32])
    w1_d = din("w1", [32 * 128, 8 * 2048])
    b1_d = din("b1", [32 * 128, 16])
    w2_d = din("w2", [32 * 128, 8 * D])
    b2_d = din("b2", [32, D])
    w_pg_d = din("w_pg", [D, D])
    w_pp_d = din("w_pp", [256, D])
    lnmixT_d = din("lnmixT", [128, 8])
    lnpleT_d = din("lnpleT", [128, 8])
    lnmoe_d = din("lnmoe_b", [128, D])
    lnfin_d = din("lnfin_b", [128, D])
    gnorm_d = din("gnorm_b", [128, 256])
    brout_d = din("brout_b", [128, 32])
    cA_d = din("cA", [128, 128 * 6 + 512 + 32 + 8 + 4 + 1 + 64])
    maskadd_d = din("maskadd", [128, 5, 128])

    out_d = nc.dram_tensor("out", [T, D], F32, kind="ExternalOutput").ap()
    ha_d = dscr("ha_s", [T, D], BF16)
    x1_d = dscr("x1_s", [T, D], F32, out=dbg)
    xs_d = dscr("xs_s", [NT * 512, D], BF16)
    ys_d = dscr("ys_s", [NT * 512, D], F32)
    xm_d = dscr("xm_s", [T, D], BF16)
    LOGCAP = CAP.bit_length() - 1
    assert (1 << LOGCAP) == CAP
    dbg_d = {}
    if dbg:
        dbg_d["ha"] = dscr("dbg_ha", [T, D], F32, out=True)
        dbg_d["h"] = dscr("dbg_h", [T, D], F32, out=True)
        dbg_d["lg"] = dscr("dbg_lg", [T, 32], F32, out=True)
        dbg_d["x2"] = dscr("dbg_x2", [T, D], F32, out=True)

    P = Prog(nc)
    es = ExitStack()
    dumped = set()

    def dump(name, ap, b, shape, dt=F32):
        if not dbg or name in dumped:
            return
        dumped.add(name)
        dd = nc.dram_tensor("dmp_" + name, list(shape), dt, kind="ExternalOutput").ap()
        P.dma('sp', dd, ap, reads=[b], dsem=DSem(P))

    def sb(name, shape, dt, nb=1, stack=None):
        t = (stack or es).enter_context(nc.sbuf_tensor("sb_" + name, shape, dt))
        bufs = [Buf("%s_%d" % (name, i)) for i in range(nb)]
        return t, (bufs[0] if nb == 1 else bufs)

    def I(eng, method, reads, writes, *a, **k):
        return P.op(eng, lambda e: getattr(e, method)(*a, **k), reads=reads, writes=writes)

    ps = []
    psb = []
    for b in range(8):
        ps.append(es.enter_context(nc.psum_tensor("ps%d" % b, [128, 512], F32)))
        psb.append(Buf("ps%d" % b))

    def psbf(b):
        return ps[b][:].bitcast(BF16)

    NCA = 128 * 6 + 512 + 32 + 8 + 4 + 1 + 64
    cA, cAb = sb("cA", [128, NCA], F32)
    o = 0
    identf = cA[:, o:o + 128]; o += 128
    triS = cA[:, o:o + 128]; o += 128
    c_tristrict = cA[:, o:o + 128]; o += 128
    c_ones = cA[:, o:o + 128]; o += 128
    c_ident2 = cA[:, o:o + 128]; o += 128
    c_spare = cA[:, o:o + 128]; o += 128
    maskA4 = cA[:, o:o + 512]; o += 512
    eoff = cA[:, o:o + 32]; o += 32
    pk8 = cA[:, o:o + 8]; o += 8
    pc4 = cA[:, o:o + 4]; o += 4
    pidx = cA[:, o:o + 1]; o += 1
    tvals = cA[:, o:o + 64]; o += 64
    ds_c = DSem(P)
    P.dma('sp', cA[:], cA_d, writes=[cAb], dsem=ds_c)
    cB, cBb = sb("cB", [128, 3 * 128], BF16)
    ident = cB[:, 0:128]
    tristrict = cB[:, 128:256]
    ones_bf = cB[:, 256:384]
    lnmixT, lnmixTb = sb("lnmixT", [128, 8], F32)
    lnpleT, lnpleTb = sb("lnpleT", [128, 8], F32)
    P.dma('sp', lnmixT[:], lnmixT_d, writes=[lnmixTb], dsem=ds_c)
    P.dma('sp', lnpleT[:], lnpleT_d, writes=[lnpleTb], dsem=ds_c)
    gnorm, gnormb = sb("gnorm", [128, 256], F32)
    P.dma('sp', gnorm[:], gnorm_d, writes=[gnormb], dsem=ds_c)
    wgk, wgkb = sb("wgk", [17, 512], F32)
    P.dma('sp', wgk[:], wgk_d, writes=[wgkb], dsem=ds_c)
    I('dve', 'tensor_copy', [cAb], [cBb], out=cB[:, 0:128], in_=c_ident2)
    I('dve', 'tensor_copy', [cAb], [cBb], out=cB[:, 128:384], in_=cA[:, 256:512])
    lnb, lnbb = sb("lnb", [128, 8, 128], F32)
    for kc in range(8):
        I('dve', 'tensor_scalar', [cAb, lnmixTb], [lnbb], out=lnb[:, kc, :], in0=c_ones, scalar1=lnmixT[:, kc:kc + 1],
          scalar2=None, op0=ALU.mult)

    ss, ssb = sb("ss", [128, 8], F32)
    junk, junkb = sb("junk", [128, 1024], F32)
    slots_i, slots_ib = sb("slots_i", [128, NB, 4], I32)
    gate4, gate4b = sb("gate4", [128, NB, 4], F32)
    cm, cmb = sb("cm", [128, 32], F32)
    cmbf, cmbfb = sb("cmbf", [128, 32], BF16)
    epsc, epscb = sb("epsc", [128, 1], F32)
    zt, ztb = sb("zt", [128, D], BF16)
    I('pool', 'memset', [], [ztb], zt[:], fill)
    ds_z = DSem(P)
    zfill_next = [0]

    def zero_fill(n):
        for _ in range(n):
            t_ = zfill_next[0]
            if t_ >= NT:
                return
            zfill_next[0] += 1
            P.dma('sp', xs_d[t_ * 512:(t_ + 1) * 512, :].rearrange("(c p) d -> p c d", p=128), zt[:].unsqueeze(1).to_broadcast([128, 4, D]), reads=[ztb], dsem=ds_z)
    esAB = ExitStack()
    xbuf = [sb("xb%d" % i, [128, D], F32, stack=esAB) for i in range(2)]
    dsx = [DSem(P) for _ in range(2)]
    xn, xnb = sb("xn", [128, D], BF16, stack=esAB)
    xnT, xnTb = sb("xnT", [128, 8, 512], BF16, stack=esAB)
    NRING = 4
    wring = [sb("wr%d" % i, [128, 8, 512], BF16, stack=esAB) for i in range(NRING)]
    dsw = [DSem(P) for _ in range(NRING)]
    wcount = [0]

    def rms_rstd(src_ap, srcb, n, col):
        I('act', 'activation', [srcb], [junkb], out=junk[:, 0:n], in_=src_ap, func=AF.Square)
        I('dve', 'tensor_reduce', [junkb], [ssb], out=ss[:, col:col + 1], in_=junk[:, 0:n], axis=AX.X, op=ALU.add)
        I('act', 'activation', [ssb], [ssb], out=ss[:, col + 1:col + 2], in_=ss[:, col:col + 1], func=AF.Ln,
          scale=1.0 / n, bias=epsc[:, 0:1])
        I('act', 'activation', [ssb], [ssb], out=ss[:, col + 2:col + 3], in_=ss[:, col + 1:col + 2], func=AF.Exp,
          scale=-0.5)
        return ss[:, col + 2:col + 3]

    I('dve', 'memset', [], [epscb], epsc[:], EPS)

    def load_x(i):
        xt, xtb = xbuf[i % 2]
        P.dma('sp', xt[:], x_d[i * 128:(i + 1) * 128, :], writes=[xtb], dsem=dsx[i % 2])

    def load_norm_A(i, j, dma=True):
        xt, xtb = xbuf[i % 2]
        if dma:
            load_x(i)
        r = rms_rstd(xt[:], xtb, D, 0)
        I('dve', 'tensor_scalar', [xtb, ssb], [xnb], out=xn[:], in0=xt[:], scalar1=r, scalar2=None, op0=ALU.mult)

    def load_norm_T(i, j, lnbt, lnbtb):
        load_norm_A(i, j)
        load_norm_B(j, lnbt, lnbtb)

    def load_norm_B(j, lnbt, lnbtb):
        pT = psbf(2)
        for kc in range(8):
            I('pe', 'transpose', [xnb, cBb], [psb[2]], out=pT[:, kc * 128:(kc + 1) * 128],
              in_=xn[:, kc * 128:(kc + 1) * 128], identity=ident)
        I('dve', 'tensor_tensor', [psb[2], lnbtb], [xnTb], out=xnT[:, :, j * 128:(j + 1) * 128],
          in0=pT.rearrange("p (a b) -> p a b", a=8), in1=lnbt[:], op=ALU.mult)

    w_in_v = w_in.rearrange("(kc p) c -> p kc c", p=128)

    def load_wchunk(c0, ncols):
        k = wcount[0] % NRING
        wcount[0] += 1
        wt, wtb = wring[k]
        P.dma('pool', wt[:, :, 0:ncols], w_in_v[:, :, c0:c0 + ncols], writes=[wtb], dsem=dsw[k])
        return wt, wtb

    pbank = [0]

    def proj_fm(wt, wtb, sub, nsub, evac):
        for q in range(nsub):
            b = pbank[0] % 2
            pbank[0] += 1
            for kc in range(8):
                I('pe', 'matmul', [wtb, xnTb], [psb[b]], ps[b][0:sub, :], lhsT=wt[:, kc, q * sub:(q + 1) * sub],
                  rhs=xnT[:, kc, :], start=(kc == 0), stop=(kc == 7))
            evac(q, ps[b], psb[b])

    def proj_tm(wt, wtb, evac):
        for j in range(4):
            b = pbank[0] % 2
            pbank[0] += 1
            for kc in range(8):
                I('pe', 'matmul', [wtb, xnTb], [psb[b]], ps[b][:, :], lhsT=xnT[:, kc, j * 128:(j + 1) * 128],
                  rhs=wt[:, kc, :], start=(kc == 0), stop=(kc == 7))
            evac(j, ps[b], psb[b])

    esG = ExitStack()
    qTg, qTgb = sb("qTg", [128, 4, 512], BF16, stack=esG)
    kTg, kTgb = sb("kTg", [128, 4, 512], BF16, stack=esG)
    gka, gkab = sb("gka", [32, 512], F32, stack=esG)
    vg, vgb = sb("vg", [128, 4, D], BF16, stack=esG)
    rs, rsb = sb("rs", [128, 4, D], BF16, stack=esG)
    sgt, sgtb = sb("sgt", [128, 512], BF16, stack=esG)
    Sst, Sstb = sb("Sst", [128, 4, 256], F32, stack=esG)
    Sbf, Sbfb = sb("Sbf", [128, 4, 256], BF16, stack=esG)
    e1t, e1tb = sb("e1t", [128, 512], F32, stack=esG)
    lt, ltb = sb("lt", [128, 512], F32, stack=esG)
    E1s = [sb("E1_%d" % i, [128, 512], F32, stack=esG) for i in range(2)]
    E3, E3b = sb("E3", [128, 512], F32, stack=esG)
    qtls = [sb("qtl%d" % i, [128, 512], BF16, stack=esG) for i in range(2)]
    ktl, ktlb = sb("ktl", [128, 512], BF16, stack=esG)
    khT, khTb = sb("khT", [128, 512], BF16, stack=esG)
    khs = [sb("kh%d" % i, [128, 512], BF16, stack=esG) for i in range(2)]
    ATs = [sb("AT%d" % i, [128, 512], BF16, stack=esG) for i in range(2)]
    yg, ygb = sb("yg", [128, D], F32, stack=esG)
    ss4, ss4b = sb("ss4", [128, 12], F32, stack=esG)
    hab = [sb("ha%d" % i, [128, D], BF16, stack=esG) for i in range(2)]
    dsha = [DSem(P) for _ in range(2)]
    I('pool', 'memset', [], [gkab], gka[:], 1.0)
    I('pool', 'memset', [], [Sstb], Sst[:], 0.0)
    I('pool', 'memset', [], [Sbfb], Sbf[:], 0.0)

    WG, _ = sb("WG", [128, 8, 4112], BF16, stack=esG)
    wg_chunks = {}
    for (c0, ncols, o0) in ((C_QG, 512, 0), (C_KG, 512, 512), (C_GK, 16, 2048), (C_VG, 512, 1024), (C_VG + 512, 512, 1536),
                            (C_RG, 512, 2064), (C_RG + 512, 512, 2576), (C_GA, 512, 3088), (C_GA + 512, 512, 3600)):
        bb = Buf("wg%d" % c0)
        P.dma('pool', WG[:, :, o0:o0 + ncols], w_in_v[:, :, c0:c0 + ncols], writes=[bb], dsem=DSem(P))
        wg_chunks[c0] = (WG[:, :, o0:o0 + ncols], bb)

    def load_wchunk_g(c0, ncols):
        return wg_chunks[c0]

    for j in range(4):
        load_norm_T(j, j, lnb, lnbb)
    for s in range(NSB):
        dump("xnT", xnT[:], xnTb, [128, 8, 512], BF16)
        wt, wtb = load_wchunk_g(C_QG, 512)
        proj_fm(wt, wtb, 128, 4, lambda q, pt, ptb: I('act', 'activation', [ptb], [qTgb], out=qTg[:, q, :], in_=pt[:, :], func=AF.Copy))
        wt, wtb = load_wchunk_g(C_KG, 512)
        proj_fm(wt, wtb, 128, 4, lambda q, pt, ptb: I('dve', 'tensor_copy', [ptb], [kTgb], out=kTg[:, q, :], in_=pt[:, :]))
        wt, wtb = load_wchunk_g(C_GK, 16)
        proj_fm(wt, wtb, 16, 1, lambda q, pt, ptb: I('dve', 'tensor_copy', [ptb], [gkab], out=gka[0:16, :], in_=pt[0:16, :]))
        for hf in range(2):
            wt, wtb = load_wchunk_g(C_VG + hf * 512, 512)
            proj_tm(wt, wtb, lambda j, pt, ptb, hf=hf: I('act', 'activation', [ptb], [vgb], out=vg[:, j, hf * 512:(hf + 1) * 512], in_=pt[:, :], func=AF.Copy))
        for hf in range(2):
            wt, wtb = load_wchunk_g(C_RG + hf * 512, 512)
            proj_tm(wt, wtb, lambda j, pt, ptb, hf=hf: I('act', 'activation', [ptb], [rsb], out=rs[:, j, hf * 512:(hf + 1) * 512], in_=pt[:, :], func=AF.Silu))
        for hf in range(2):
            wt, wtb = load_wchunk_g(C_GA + hf * 512, 512)

            def ev(j, pt, ptb, hf=hf):
                I('act', 'activation', [ptb], [sgtb], out=sgt[:], in_=pt[:, :], func=AF.Sigmoid)
                I('dve', 'tensor_tensor', [sgtb, rsb], [rsb], out=rs[:, j, hf * 512:(hf + 1) * 512],
                  in0=rs[:, j, hf * 512:(hf + 1) * 512], in1=sgt[:], op=ALU.mult)
            proj_tm(wt, wtb, ev)
        dump("qTg", qTg[:], qTgb, [128, 4, 512], BF16)
        dump("kTg", kTg[:], kTgb, [128, 4, 512], BF16)
        dump("gka", gka[:], gkab, [32, 512])
        dump("vg", vg[:], vgb, [128, 4, D], BF16)
        dump("rs", rs[:], rsb, [128, 4, D], BF16)
        def gla_front(j, s=s):
            tok = slice(j * 128, (j + 1) * 128)
            E1, E1b = E1s[j % 2]
            qtl, qtlb = qtls[j % 2]
            kh, khb = khs[j % 2]
            AT, ATb = ATs[j % 2]
            I('pe', 'matmul', [gkab, wgkb], [psb[3]], ps[3][:, :], lhsT=gka[0:17, tok], rhs=wgk[0:17, :], start=True, stop=True)
            I('act', 'activation', [psb[3]], [e1tb], out=e1t[:], in_=ps[3][:, :], func=AF.Exp, scale=-1.0)
            I('act', 'activation', [e1tb], [ltb], out=lt[:], in_=e1t[:], func=AF.Ln, bias=1.0)
            for h in range(4):
                I('pe', 'matmul', [ltb, cAb], [psb[4]], ps[4][:, h * 128:(h + 1) * 128], lhsT=lt[:, h * 128:(h + 1) * 128],
                  rhs=triS, start=True, stop=True)
            I('act', 'activation', [psb[4]], [E1b], out=E1[:], in_=ps[4][:, :], func=AF.Exp)
            I('act', 'activation', [psb[4]], [E3b], out=E3[:], in_=ps[4][:, :], func=AF.Exp, scale=-1.0)
            I('dve', 'scalar_tensor_tensor', [E1b, qTgb], [qtlb], out=qtl[:].rearrange("p (h t) -> p h t", h=4), in0=E1[:].rearrange("p (h t) -> p h t", h=4),
              scalar=128.0 ** -0.5, in1=qTg[:, :, tok], op0=ALU.mult, op1=ALU.mult)
            I('dve', 'tensor_tensor', [E3b, kTgb], [ktlb], out=ktl[:].rearrange("p (h t) -> p h t", h=4), in0=E3[:].rearrange("p (h t) -> p h t", h=4),
              in1=kTg[:, :, tok], op=ALU.mult)
            for h in range(4):
                I('dve', 'tensor_scalar', [ktlb, E1b], [khTb], out=khT[:, h * 128:(h + 1) * 128], in0=ktl[:, h * 128:(h + 1) * 128],
                  scalar1=E1[:, h * 128 + 127:h * 128 + 128], scalar2=None, op0=ALU.mult)
            pT = psbf(2)
            for h in range(4):
                I('pe', 'transpose', [khTb, cBb], [psb[2]], out=pT[:, h * 128:(h + 1) * 128], in_=khT[:, h * 128:(h + 1) * 128], identity=ident)
            I('act', 'activation', [psb[2]], [khb], out=kh[:], in_=pT[:, 0:512], func=AF.Copy)
            for h in range(4):
                I('pe', 'matmul', [ktlb, qtlb], [psb[3]], ps[3][:, h * 128:(h + 1) * 128], lhsT=ktl[:, h * 128:(h + 1) * 128],
                  rhs=qtl[:, h * 128:(h + 1) * 128], start=True, stop=True)
            I('dve', 'tensor_tensor', [psb[3], cAb], [ATb], out=AT[:], in0=ps[3][:, :], in1=maskA4, op=ALU.mult)

        def gla_back(j, s=s):
            i = 4 * s + j
            E1, E1b = E1s[j % 2]
            qtl, qtlb = qtls[j % 2]
            kh, khb = khs[j % 2]
            AT, ATb = ATs[j % 2]
            for h in range(4):
                bk = 5 + h // 2
                oc = slice((h % 2) * 256, (h % 2) * 256 + 256)
                I('pe', 'matmul', [ATb, vgb], [psb[bk]], ps[bk][:, oc], lhsT=AT[:, h * 128:(h + 1) * 128], rhs=vg[:, j, h * 256:(h + 1) * 256],
                  start=True, stop=False)
                I('pe', 'matmul', [qtlb, Sbfb], [psb[bk]], ps[bk][:, oc], lhsT=qtl[:, h * 128:(h + 1) * 128], rhs=Sbf[:, h, :],
                  start=False, stop=True)
            for hh in range(2):
                for h2 in range(2):
                    h = hh * 2 + h2
                    I('pe', 'matmul', [khb, vgb], [psb[7]], ps[7][:, h2 * 256:(h2 + 1) * 256], lhsT=kh[:, h * 128:(h + 1) * 128],
                      rhs=vg[:, j, h * 256:(h + 1) * 256], start=True, stop=True)
                for h2 in range(2):
                    h = hh * 2 + h2
                    I('dve', 'scalar_tensor_tensor', [Sstb, E1b, psb[7]], [Sstb], out=Sst[:, h, :], in0=Sst[:, h, :],
                      scalar=E1[:, h * 128 + 127:h * 128 + 128], in1=ps[7][:, h2 * 256:(h2 + 1) * 256], op0=ALU.mult, op1=ALU.add)
            I('pool', 'tensor_copy', [Sstb], [Sbfb], out=Sbf[:], in_=Sst[:])
            I('act', 'activation', [psb[5]], [junkb], out=junk[:, 0:512], in_=ps[5][:, :], func=AF.Square)
            I('act', 'activation', [psb[6]], [junkb], out=junk[:, 512:1024], in_=ps[6][:, :], func=AF.Square)
            I('dve', 'tensor_reduce', [junkb], [ss4b], out=ss4[:, 0:4], in_=junk[:].rearrange("p (h d) -> p h d", h=4), axis=AX.X, op=ALU.add)
            I('act', 'activation', [ss4b], [ss4b], out=ss4[:, 4:8], in_=ss4[:, 0:4], func=AF.Ln, scale=1.0 / 256, bias=epsc[:, 0:1])
            I('act', 'activation', [ss4b], [ss4b], out=ss4[:, 8:12], in_=ss4[:, 4:8], func=AF.Exp, scale=-0.5)
            for h in range(4):
                bk = 5 + h // 2
                oc = slice((h % 2) * 256, (h % 2) * 256 + 256)
                I('dve', 'scalar_tensor_tensor', [psb[bk], ss4b, gnormb], [ygb], out=yg[:, h * 256:(h + 1) * 256], in0=ps[bk][:, oc],
                  scalar=ss4[:, 8 + h:9 + h], in1=gnorm[:], op0=ALU.mult, op1=ALU.mult)
            hat, hatb = hab[i % 2]
            I('dve', 'tensor_tensor', [ygb, rsb], [hatb], out=hat[:], in0=yg[:], in1=rs[:, j, :], op=ALU.mult)
            P.dma('sp', ha_d[i * 128:(i + 1) * 128, :], hat[:], reads=[hatb], dsem=dsha[i % 2])
            if dbg:
                I('dve', 'tensor_copy', [hatb, ygb], [ygb], out=yg[:], in_=hat[:])
                P.dma('sp', dbg_d["ha"][i * 128:(i + 1) * 128, :], yg[:], reads=[ygb], dsem=dsha[i % 2])

        gla_front(0)
        for j in range(4):
            zero_fill((NT + 4 * NSB - 1) // (4 * NSB))
            if s + 1 < NSB:
                load_x(4 * (s + 1) + j)
            if j + 1 < 4:
                gla_front(j + 1)
            gla_back(j)
            if s + 1 < NSB:
                if j > 0:
                    load_norm_B(j - 1, lnb, lnbb)
                load_norm_A(4 * (s + 1) + j, j, dma=False)
        if s + 1 < NSB:
            load_norm_B(3, lnb, lnbb)
    zero_fill(NT)
    P.barrier()
    esG.close()
    if stop_after <= 1:
        P.emit(final_waits=[d for d in P.dsems if d.count > 0])
        es.close()
        return nc

    I('pool', 'memset', [], [cmb], cm[:], 0.0)
    I('pool', 'memset', [], [cmbfb], cmbf[:], 0.0)

    esB = ExitStack()
    qTa, qTab = sb("qTa", [128, 8, 512], BF16, stack=esB)
    kTa = [sb("kTa%d" % i, [128, 8, 512], BF16, stack=esB) for i in range(2)]
    vaug = [sb("vaug%d" % i, [128, 4, 16, 65], BF16, stack=esB) for i in range(2)]
    sgb, sgbb = sb("sgb", [128, 4, D], BF16, stack=esB)
    BMT, BMTb = sb("BMT", [128, 16, 5, 128], BF16, stack=esB)
    Wout, Woutb = sb("Wout", [128, 8, D], BF16, stack=esB)
    lnmoe, lnmoeb = sb("lnmoe", [128, D], F32, stack=esB)
    brout, broutb = sb("brout", [128, 32], F32, stack=esB)
    wr32, wr32b = sb("wr32", [128, 8, 32], F32, stack=esB)
    madd, maddb = sb("madd", [128, 5, 128], F32, stack=esB)
    bstg = [sb("bstg%d" % i, [128, 5, 128], F32, stack=esB) for i in range(2)]
    dsbs = [DSem(P) for _ in range(2)]
    PT = [sb("PT%d" % i, [128, 640], BF16, stack=esB) for i in range(2)]
    yb, ybb = sb("yb", [128, D], F32, stack=esB)
    rden, rdenb = sb("rden", [128, 4], F32, stack=esB)
    hld = [sb("hld%d" % i, [128, D], BF16, stack=esB) for i in range(2)]
    dshl = [DSem(P) for _ in range(2)]
    hm, hmb = sb("hm", [128, D], BF16, stack=esB)
    hT, hTb = sb("hT", [128, 8, 128], BF16, stack=esB)
    xres, xresb = sb("xres", [128, D], F32, stack=esB)
    dsxr = DSem(P)
    x1t, x1tb = sb("x1t", [128, D], F32, stack=esB)
    dsx1 = DSem(P)
    xm32, xm32b = sb("xm32", [128, D], F32, stack=esB)
    xmb = [sb("xmb%d" % i, [128, D], BF16, stack=esB) for i in range(2)]
    dsxm = [DSem(P) for _ in range(2)]
    xmT32, xmT32b = sb("xmT32", [128, 8, 128], F32, stack=esB)
    lg, lgb = sb("lg", [128, 32], F32, stack=esB)
    m8, m8b = sb("m8", [128, 8], F32, stack=esB)
    mask, maskb = sb("mask", [128, 32], F32, stack=esB)
    maskbf, maskbfb = sb("maskbf", [128, 32], BF16, stack=esB)
    ex, exb = sb("ex", [128, 32], F32, stack=esB)
    gates, gatesb = sb("gates", [128, 32], F32, stack=esB)
    slotf, slotfb = sb("slotf", [128, 32], F32, stack=esB)
    oh4, oh4b = sb("oh4", [128, 4, 32], F32, stack=esB)
    i8, i8b = sb("i8", [128, 8], U32, stack=esB)
    i8f, i8fb = sb("i8f", [128, 8], F32, stack=esB)
    jk32, jk32b = sb("jk32", [128, 32], F32, stack=esB)
    sm, smb = sb("sm", [128, 8], F32, stack=esB)
    slot4f, slot4fb = sb("slot4f", [128, 4], F32, stack=esB)
    YB = (5, 2)
    pending_tail = []
    _dbgs = []

    def dbgsem():
        if len(_dbgs) < 6:
            _dbgs.append(DSem(P))
            return _dbgs[-1]
        return _dbgs[len(_dbgs) % 6 - 1]
    ds_b = DSem(P)
    P.dma('sp', lnmoe[:], lnmoe_d, writes=[lnmoeb], dsem=ds_b)
    P.dma('sp', brout[:], brout_d, writes=[broutb], dsem=ds_b)
    P.dma('sp', wr32[:], w_router_d.rearrange("(kc p) e -> p kc e", p=128), writes=[wr32b], dsem=ds_b)
    P.dma('sp', madd[:], maskadd_d, writes=[maddb], dsem=ds_b)
    P.dma('pool', Wout[:], w_out_d.rearrange("(kc p) c -> p kc c", p=128), writes=[Woutb], dsem=ds_b)
    for h in range(16):
        st, stb = bstg[h % 2]
        P.dma('sp', st[:], biasT_d[:, h], writes=[stb], dsem=dsbs[h % 2])
        I('dve', 'tensor_tensor', [stb, maddb], [BMTb], out=BMT[:, h], in0=st[:], in1=madd[:], op=ALU.add)
    for i2 in range(2):
        I('pool', 'memset', [], [vaug[i2][1]], vaug[i2][0][:], 1.0)

    for j in range(4):
        load_norm_T(j, j, lnb, lnbb)
    for s in range(NSB):
        kT_t, kT_b = kTa[s % 2]
        va_t, va_b = vaug[s % 2]
        def pop_tail():
            if pending_tail:
                pending_tail.pop(0)()

        for hf in range(2):
            wt, wtb = load_wchunk(C_QA + hf * 512, 512)
            proj_fm(wt, wtb, 128, 4, lambda q, pt, ptb, hf=hf: I('act', 'activation', [ptb], [qTab], out=qTa[:, hf * 4 + q, :], in_=pt[:, :], func=AF.Copy, scale=0.125))
            pop_tail()
        for hf in range(2):
            wt, wtb = load_wchunk(C_KA + hf * 512, 512)
            proj_fm(wt, wtb, 128, 4, lambda q, pt, ptb, hf=hf: I('dve', 'tensor_copy', [ptb], [kT_b], out=kT_t[:, hf * 4 + q, :], in_=pt[:, :]))
            pop_tail()
        for hf in range(2):
            wt, wtb = load_wchunk(C_VA + hf * 512, 512)
            proj_tm(wt, wtb, lambda j, pt, ptb, hf=hf: I('act', 'activation', [ptb], [va_b], out=va_t[:, j, hf * 8:(hf + 1) * 8, 0:64],
                                                           in_=pt[:, :].rearrange("p (h d) -> p h d", h=8), func=AF.Copy))
            pop_tail()
        for hf in range(2):
            wt, wtb = load_wchunk(C_GB + hf * 512, 512)
            proj_tm(wt, wtb, lambda j, pt, ptb, hf=hf: I('act', 'activation', [ptb], [sgbb], out=sgb[:, j, hf * 512:(hf + 1) * 512], in_=pt[:, :], func=AF.Sigmoid))
            pop_tail()
        while pending_tail:
            pending_tail.pop(0)()

        for j in range(4):
            i = 4 * s + j
            tok = slice(j * 128, (j + 1) * 128)
            hl_t, hl_b = hld[i % 2]
            P.dma('sp', hl_t[:], ha_d[i * 128:(i + 1) * 128, :], writes=[hl_b], dsem=dshl[i % 2])
            if s + 1 < NSB:
                load_x(4 * (s + 1) + j)
            kbis = [kbi for kbi in range(5) if i - 4 + kbi >= 0]
            lo = kbis[0]

            def QK(h, i=i, tok=tok, kbis=kbis):
                hp, hh = h // 2, h % 2
                r0 = hh * 64
                xb_ = 3 + hh
                for kbi in kbis:
                    kb = i - 4 + kbi
                    kslot_t, kslot_b = kTa[(kb // 4) % 2]
                    koff = (kb % 4) * 128
                    if kbi < 4:
                        reg = ps[xb_][:, kbi * 128:(kbi + 1) * 128]
                        regb = psb[xb_]
                    else:
                        reg = ps[YB[hh]][:, 0:128]
                        regb = psb[YB[hh]]
                    I('pe', 'matmul', [cBb, BMTb], [regb], reg, lhsT=ident, rhs=BMT[:, h, kbi, :], start=True, stop=False)
                    I('pe', 'matmul', [kslot_b, qTab], [regb], reg, lhsT=kslot_t[r0:r0 + 64, hp, koff:koff + 128],
                      rhs=qTa[r0:r0 + 64, hp, tok], start=False, stop=True)

            def EXPPV(h, i=i, kbis=kbis, lo=lo):
                hh = h % 2
                xb_ = 3 + hh
                pt_t, pt_b = PT[hh]
                if lo < 4:
                    I('act', 'activation', [psb[xb_]], [pt_b], out=pt_t[:, lo * 128:512], in_=ps[xb_][:, lo * 128:512], func=AF.Exp)
                I('act', 'activation', [psb[YB[hh]]], [pt_b], out=pt_t[:, 512:640], in_=ps[YB[hh]][:, 0:128], func=AF.Exp)
                hq = h % 4
                for kbi in kbis:
                    kb = i - 4 + kbi
                    vslot_t, vslot_b = vaug[(kb // 4) % 2]
                    I('pe', 'matmul', [pt_b, vslot_b], [psb[6]], ps[6][:, hq * 65:(hq + 1) * 65], lhsT=pt_t[:, kbi * 128:(kbi + 1) * 128],
                      rhs=vslot_t[:, kb % 4, h, :], start=(kbi == kbis[0]), stop=(kbi == kbis[-1]))
                if hq == 3:
                    o3 = ps[6][:, 0:260].rearrange("p (h d) -> p h d", h=4)
                    I('dve', 'reciprocal', [psb[6]], [rdenb], out=rden[:], in_=o3[:, :, 64])
                    for q4 in range(4):
                        hh4 = h - 3 + q4
                        I('dve', 'tensor_scalar', [psb[6], rdenb], [ybb], out=yb[:, hh4 * 64:(hh4 + 1) * 64], in0=ps[6][:, q4 * 65:q4 * 65 + 64],
                          scalar1=rden[:, q4:q4 + 1], scalar2=None, op0=ALU.mult)

            if QK_AHEAD:
                QK(0)
            for h in range(16):
                if QK_AHEAD:
                    if h + 1 < 16:
                        QK(h + 1)
                else:
                    QK(h)
                EXPPV(h)
                if pending_tail and h in (1, 3, 5, 8, 11, 14):
                    pending_tail.pop(0)()
            while pending_tail:
                pending_tail.pop(0)()
            I('dve', 'tensor_tensor', [ybb, sgbb], [ybb], out=yb[:], in0=yb[:], in1=sgb[:, j, :], op=ALU.mult)
            I('dve', 'tensor_tensor', [ybb, hl_b], [hmb], out=hm[:], in0=yb[:], in1=hl_t[:], op=ALU.add)
            if dbg:
                I('dve', 'tensor_tensor', [ybb, hl_b], [ybb], out=yb[:], in0=yb[:], in1=hl_t[:], op=ALU.add)
                P.dma('sp', dbg_d["h"][i * 128:(i + 1) * 128, :], yb[:], reads=[ybb], dsem=dbgsem())

            def T1(i=i):
                pT = psbf(2)
                for kc in range(8):
                    I('pe', 'transpose', [hmb, cBb], [psb[2]], out=pT[:, kc * 128:(kc + 1) * 128], in_=hm[:, kc * 128:(kc + 1) * 128], identity=ident)
                I('act', 'activation', [psb[2]], [hTb], out=hT[:], in_=pT.rearrange("p (a b) -> p a b", a=8), func=AF.Copy)
                P.dma('sp', xres[:], x_d[i * 128:(i + 1) * 128, :], writes=[xresb], dsem=dsxr)

            def T2(i=i):
                for hf in range(2):
                    for kc in range(8):
                        I('pe', 'matmul', [hTb, Woutb], [psb[hf]], ps[hf][:, :], lhsT=hT[:, kc, :], rhs=Wout[:, kc, hf * 512:(hf + 1) * 512],
                          start=(kc == 0), stop=(kc == 7))
                    I('dve', 'tensor_tensor', [psb[hf], xresb], [x1tb], out=x1t[:, hf * 512:(hf + 1) * 512], in0=ps[hf][:, :],
                      in1=xres[:, hf * 512:(hf + 1) * 512], op=ALU.add)
                P.dma('sp', x1_d[i * 128:(i + 1) * 128, :], x1t[:], reads=[x1tb], dsem=dsx1)
                r = rms_rstd(x1t[:], x1tb, D, 0)
                I('dve', 'scalar_tensor_tensor', [x1tb, ssb, lnmoeb], [xm32b], out=xm32[:], in0=x1t[:], scalar=r, in1=lnmoe[:], op0=ALU.mult, op1=ALU.mult)
                xmb_t, xmb_b = xmb[i % 2]
                I('act', 'activation', [xm32b], [xmb_b], out=xmb_t[:], in_=xm32[:], func=AF.Copy)
                P.dma('sp', xm_d[i * 128:(i + 1) * 128, :], xmb_t[:], reads=[xmb_b], dsem=dsxm[i % 2])

            def T3(i=i):
                for kc in range(8):
                    bk = kc // 4
                    I('pe', 'transpose', [xm32b, cAb], [psb[bk]], out=ps[bk][:, (kc % 4) * 128:(kc % 4 + 1) * 128], in_=xm32[:, kc * 128:(kc + 1) * 128], identity=identf)
                I('act', 'activation', [psb[0]], [xmT32b], out=xmT32[:, 0:4, :], in_=ps[0][:, :].rearrange("p (a b) -> p a b", a=4), func=AF.Copy)
                I('dve', 'tensor_copy', [psb[1]], [xmT32b], out=xmT32[:, 4:8, :], in_=ps[1][:, :].rearrange("p (a b) -> p a b", a=4))

            def T4(i=i):
                for kc in range(8):
                    I('pe', 'matmul', [xmT32b, wr32b], [psb[7]], ps[7][:, 0:32], lhsT=xmT32[:, kc, :], rhs=wr32[:, kc, :], start=(kc == 0), stop=(kc == 7))
                I('dve', 'tensor_tensor', [psb[7], broutb], [lgb], out=lg[:], in0=ps[7][:, 0:32], in1=brout[:], op=ALU.add)
                if dbg:
                    P.dma('sp', dbg_d["lg"][i * 128:(i + 1) * 128, :], lg[:], reads=[lgb], dsem=dbgsem())
                I('dve', 'max', [lgb], [m8b], out=m8[:], in_=lg[:])
                I('dve', 'max_index', [lgb, m8b], [i8b], out=i8[:], in_max=m8[:], in_values=lg[:])
                I('dve', 'tensor_copy', [i8b], [i8fb], out=i8f[:], in_=i8[:])
                for k in range(4):
                    I('dve', 'tensor_scalar', [cAb, i8fb], [oh4b], out=oh4[:, k, :], in0=tvals[:, 0:32], scalar1=i8f[:, k:k + 1], scalar2=None, op0=ALU.is_equal)
                I('dve', 'tensor_tensor', [oh4b], [maskb], out=mask[:], in0=oh4[:, 0, :], in1=oh4[:, 1, :], op=ALU.add)
                I('dve', 'tensor_tensor', [oh4b, maskb], [maskb], out=mask[:], in0=mask[:], in1=oh4[:, 2, :], op=ALU.add)
                I('dve', 'tensor_tensor', [oh4b, maskb], [maskb], out=mask[:], in0=mask[:], in1=oh4[:, 3, :], op=ALU.add)
                I('dve', 'tensor_copy', [maskb], [maskbfb], out=maskbf[:], in_=mask[:])

            def T5(i=i):
                I('pe', 'matmul', [cBb, maskbfb], [psb[7]], ps[7][:, 64:96], lhsT=tristrict, rhs=maskbf[:], start=True, stop=False)
                I('pe', 'matmul', [cBb, cmbfb], [psb[7]], ps[7][:, 64:96], lhsT=ones_bf, rhs=cmbf[:], start=False, stop=True)
                I('dve', 'tensor_scalar', [m8b], [smb], out=sm[:, 0:1], in0=m8[:, 0:1], scalar1=-1.0, scalar2=None, op0=ALU.mult)
                I('act', 'activation', [lgb, smb], [exb], out=ex[:], in_=lg[:], func=AF.Exp, bias=sm[:, 0:1], scale=1.0)
                I('dve', 'tensor_tensor', [exb, maskb], [exb], out=ex[:], in0=ex[:], in1=mask[:], op=ALU.mult)
                I('dve', 'tensor_reduce', [exb], [smb], out=sm[:, 1:2], in_=ex[:], axis=AX.X, op=ALU.add)
                I('dve', 'reciprocal', [smb], [smb], out=sm[:, 2:3], in_=sm[:, 1:2])
                I('dve', 'tensor_scalar', [exb, smb], [gatesb], out=gates[:], in0=ex[:], scalar1=sm[:, 2:3], scalar2=None, op0=ALU.mult)
                I('dve', 'tensor_tensor', [psb[7], cAb], [slotfb], out=slotf[:], in0=ps[7][:, 64:96], in1=eoff, op=ALU.add)
                I('dve', 'tensor_tensor', [cmb, maskb], [cmb], out=cm[:], in0=cm[:], in1=mask[:], op=ALU.add)
                I('dve', 'tensor_copy', [cmb], [cmbfb], out=cmbf[:], in_=cm[:])
                for k in range(4):
                    I('dve', 'tensor_tensor', [oh4b, slotfb], [jk32b], out=jk32[:], in0=oh4[:, k, :], in1=slotf[:], op=ALU.mult)
                    I('dve', 'tensor_reduce', [jk32b], [slot4fb], out=slot4f[:, k:k + 1], in_=jk32[:], axis=AX.X, op=ALU.add)
                    I('dve', 'tensor_tensor', [oh4b, gatesb], [jk32b], out=jk32[:], in0=oh4[:, k, :], in1=gates[:], op=ALU.mult)
                    I('dve', 'tensor_reduce', [jk32b], [gate4b], out=gate4[:, i, k:k + 1], in_=jk32[:], axis=AX.X, op=ALU.add)
                I('dve', 'tensor_copy', [slot4fb], [slots_ib], out=slots_i[:, i, :], in_=slot4f[:])

            if s + 1 < NSB:
                load_norm_A(4 * (s + 1) + j, j, dma=False)
                pending_tail.append(lambda j=j: load_norm_B(j, lnb, lnbb))
            pending_tail.extend([T1, T2, T3, T4, T5])
            if not DEFER_TAIL or (j == 3 and s + 1 == NSB):
                while pending_tail:
                    pending_tail.pop(0)()
            elif j == 3:
                pending_tail.pop(0)()
    P.barrier()
    esB.close()
    esAB.close()

    esD = ExitStack()
    NK = NB * 4
    cnt, cntb_ = sb("cnt", [128, 32], F32, stack=esD)
    cnti, cntib = sb("cnti", [128, 32], I32, stack=esD)
    ntl, ntlb = sb("ntl", [128, 32], F32, stack=esD)
    cinc, cincb = sb("cinc", [128, 32], F32, stack=esD)
    offs, offsb = sb("offs", [128, 32], F32, stack=esD)
    s_e, s_eb = sb("s_e", [128, NK], I32, stack=esD)
    s_p, s_pb = sb("s_p", [128, NK], I32, stack=esD)
    f_e, f_eb = sb("f_e", [128, NK], F32, stack=esD)
    f_p, f_pb = sb("f_p", [128, NK], F32, stack=esD)
    f_t, f_tb = sb("f_t", [128, NK], F32, stack=esD)
    xml = [sb("xml%d" % i, [128, D], BF16, stack=esD) for i in range(4)]
    dsxl = [DSem(P) for _ in range(4)]
    I('pe', 'matmul', [cBb, cmbfb], [psb[7]], ps[7][:, 0:32], lhsT=ones_bf, rhs=cmbf[:], start=True, stop=True)
    I('dve', 'tensor_scalar', [psb[7]], [cntb_], out=cnt[:], in0=ps[7][:, 0:32], scalar1=511.0, scalar2=None, op0=ALU.add)
    I('dve', 'tensor_copy', [cntb_], [cntib], out=cnti[:], in_=cnt[:])
    I('dve', 'tensor_single_scalar', [cntib], [cntib], out=cnti[:], in_=cnti[:], scalar=9, op=ALU.arith_shift_right)
    I('dve', 'tensor_copy', [cntib], [ntlb], out=ntl[:], in_=cnti[:])
    I('dve', 'tensor_tensor_scan', [ntlb, cAb], [cincb], out=cinc[:], data0=c_ones[:, 0:32], data1=ntl[:], initial=0.0, op0=ALU.mult, op1=ALU.add)
    I('dve', 'tensor_tensor', [cincb, ntlb], [offsb], out=offs[:], in0=cinc[:], in1=ntl[:], op=ALU.subtract)
    I('dve', 'tensor_scalar', [offsb], [offsb], out=offs[:], in0=offs[:], scalar1=512.0, scalar2=None, op0=ALU.mult)
    sl2 = slots_i[:].rearrange("p a b -> p (a b)")
    I('dve', 'tensor_single_scalar', [slots_ib], [s_eb], out=s_e[:], in_=sl2, scalar=LOGCAP, op=ALU.arith_shift_right)
    I('dve', 'tensor_single_scalar', [slots_ib], [s_pb], out=s_p[:], in_=sl2, scalar=CAP - 1, op=ALU.bitwise_and)
    I('dve', 'tensor_copy', [s_eb], [f_eb], out=f_e[:], in_=s_e[:])
    I('dve', 'tensor_copy', [s_pb], [f_pb], out=f_p[:], in_=s_p[:])
    for e_ in range(32):
        I('dve', 'tensor_scalar', [f_eb, offsb], [f_tb], out=f_t[:], in0=f_e[:], scalar1=float(e_), scalar2=offs[:, e_:e_ + 1], op0=ALU.is_equal, op1=ALU.mult)
        I('dve', 'tensor_tensor', [f_tb, f_pb], [f_pb], out=f_p[:], in0=f_p[:], in1=f_t[:], op=ALU.add)
    I('dve', 'tensor_copy', [f_pb], [slots_ib], out=sl2, in_=f_p[:])
    for i in range(NB):
        xl_t, xl_b = xml[i % 4]
        P.dma('sp', xl_t[:], xm_d[i * 128:(i + 1) * 128, :], writes=[xl_b], dsem=dsxl[i % 4])
        for k in range(4):
            P.op('pool', lambda e, k=k, i=i, xl_t=xl_t: e.indirect_dma_start(
                out=xs_d, out_offset=bass.IndirectOffsetOnAxis(ap=slots_i[:, i, k:k + 1], axis=0), in_=xl_t[:], in_offset=None),
                reads=[xl_b, slots_ib], writes=[], dsem=dsxl[i % 4], is_dma=True)
    P.barrier()
    if stop_after <= 2:
        if dbg:
            dump("slots", slots_i[:], slots_ib, [128, NB, 4], I32)
            dump("gate4", gate4[:], gate4b, [128, NB, 4])
            dump("cm", cm[:], cmb, [128, 32])
        P.emit(final_waits=[d for d in P.dsems if d.count > 0])
        es.close()
        return nc

    esE = ExitStack()
    cmp_, cmpb = sb("cmp", [128, 32], F32, stack=esE)
    ET, ETb = sb("ET", [128, NT], F32, stack=esE)
    CEX, CEXb = sb("CEX", [128, NT], F32, stack=esE)
    JT, JTb = sb("JT", [128, NT], F32, stack=esE)
    EC, ECb = sb("EC", [128, NT], F32, stack=esE)
    BASE, BASEb = sb("BASE", [128, NT], F32, stack=esE)
    idxf, idxfb = sb("idxf", [128, NT, 12], F32, stack=esE)
    idxi, idxib = sb("idxi", [128, NT, 12], I32, stack=esE)
    OH, OHb = sb("OH", [32, NT], F32, stack=esE)
    ones512, ones512b = sb("ones512", [32, 512], F32, stack=esE)
    ohb, ohbb = sb("ohb", [32, 512], BF16, stack=esE)
    b1T = [sb("b1T%d" % i, [128, 16], F32, stack=esE) for i in range(2)]
    dsB1 = [DSem(P) for _ in range(2)]
    xsT2 = [sb("xsT%d" % i, [128, 8, 512], BF16, stack=esE) for i in range(2)]
    b2n, b2nb = sb("b2n", [32, D], BF16, stack=esE)
    W1t = [sb("W1t%d" % i, [128, 8, 2048], BF16, stack=esE) for i in range(2)]
    W2t = [sb("W2t%d" % i, [128, 8, D], BF16, stack=esE) for i in range(2)]
    xst = [sb("xst%d" % i, [128, 4, D], BF16, stack=esE) for i in range(2)]
    dsW1 = [DSem(P) for _ in range(2)]
    dsW2 = [DSem(P) for _ in range(2)]
    dsXs = [DSem(P) for _ in range(2)]
    actT, actTb = sb("actT", [128, 8, 512], BF16, stack=esE)
    gbuf = [sb("gb%d" % i, [128, 512], F32, stack=esE) for i in range(2)]
    sgbuf = [sb("sgb%d" % i, [128, 512], F32, stack=esE) for i in range(2)]
    lbuf = [sb("lb%d" % i, [128, 512], F32, stack=esE) for i in range(2)]
    yst = [sb("yst%d" % i, [128, D], F32, stack=esE) for i in range(2)]
    dsYs = [DSem(P) for _ in range(2)]
    ds_e = DSem(P)
    P.dma('pool', b2n[:], b2_d, writes=[b2nb], dsem=ds_e)
    I('dve', 'memset', [], [ones512b], ones512[:], 1.0)
    for i2 in range(2):
        I('dve', 'memset', [], [b1T[i2][1]], b1T[i2][0][:], 0.0)
    for t in range(NT):
        I('dve', 'tensor_scalar', [cincb], [cmpb], out=cmp_[:], in0=cinc[:], scalar1=float(t), scalar2=None, op0=ALU.is_le)
        I('dve', 'tensor_reduce', [cmpb], [ETb], out=ET[:, t:t + 1], in_=cmp_[:], axis=AX.X, op=ALU.add)
    I('dve', 'tensor_scalar', [ETb], [ECb], out=EC[:], in0=ET[:], scalar1=31.0, scalar2=128.0, op0=ALU.min, op1=ALU.mult)
    for t in range(NT):
        I('dve', 'tensor_scalar', [cAb, ECb], [idxfb], out=idxf[:, t, 4:12], in0=pidx.to_broadcast([128, 8]), scalar1=EC[:, t:t + 1], scalar2=None, op0=ALU.add)
        I('dve', 'tensor_scalar', [cAb, ECb], [idxfb], out=idxf[:, t, 0:4], in0=pc4, scalar1=0.0, scalar2=None, op0=ALU.add)
    I('dve', 'tensor_copy', [idxfb], [idxib], out=idxi[:], in_=idxf[:])
    I('dve', 'tensor_scalar', [ETb, cAb], [OHb], out=OH[:], in0=ET[0:32, :], scalar1=pidx[0:32, 0:1], scalar2=None, op0=ALU.is_equal)
    if dbg:
        dump("ET", ET[:], ETb, [128, NT])
        dump("idxi", idxi[:], idxib, [128, NT, 12], I32)
        dump("ntl", ntl[:], ntlb, [128, 32])

    def issue_gathers(t):
        k = t % 2
        P.dma('sp', xst[k][0][:], xs_d[t * 512:(t + 1) * 512, :].rearrange("(c p) d -> p c d", p=128), writes=[xst[k][1]], dsem=dsXs[k])
        P.op('pool', lambda e, t=t, k=k: e.indirect_dma_start(
            out=b1T[k][0][:], out_offset=None, in_=b1_d, in_offset=bass.IndirectOffsetOnAxis(ap=idxi[:, t, 4:5], axis=0)),
            reads=[idxib], writes=[b1T[k][1]], dsem=dsB1[k], is_dma=True)
        P.op('pool', lambda e, t=t, k=k: e.indirect_dma_start(
            out=W1t[k][0][:].rearrange("p a b -> p (a b)"), out_offset=None, in_=w1_d, in_offset=bass.IndirectOffsetOnAxis(ap=idxi[:, t, 4:5], axis=0)),
            reads=[idxib], writes=[W1t[k][1]], dsem=dsW1[k], is_dma=True)
        P.op('pool', lambda e, t=t, k=k: e.indirect_dma_start(
            out=W2t[k][0][:].rearrange("p a b -> p (a b)"), out_offset=None, in_=w2_d, in_offset=bass.IndirectOffsetOnAxis(ap=idxi[:, t, 4:5], axis=0)),
            reads=[idxib], writes=[W2t[k][1]], dsem=dsW2[k], is_dma=True)

    def do_transposes(t):
        k = t % 2
        xs_t, xs_b = xst[k]
        xsT, xsTb = xsT2[k]
        for c in range(4):
            bk = 6 + (c % 2)
            pT = psbf(bk)
            for kc in range(8):
                I('pe', 'transpose', [xs_b, cBb], [psb[bk]], out=pT[:, kc * 128:(kc + 1) * 128], in_=xs_t[:, c, kc:D:8], identity=ident)
            if c % 2 == 0:
                I('act', 'activation', [psb[bk]], [xsTb], out=xsT[:, :, c * 128:(c + 1) * 128], in_=pT.rearrange("p (a b) -> p a b", a=8), func=AF.Copy)
            else:
                I('dve', 'tensor_copy', [psb[bk]], [xsTb], out=xsT[:, :, c * 128:(c + 1) * 128], in_=pT.rearrange("p (a b) -> p a b", a=8))

    issue_gathers(0)
    do_transposes(0)
    ysn = 0
    for t in range(NT):
        k = t % 2
        if t + 1 < NT:
            issue_gathers(t + 1)
        w1_t, w1_b = W1t[k]
        w2_t, w2_b = W2t[k]
        b1_t, b1_b = b1T[k]
        xsT, xsTb = xsT2[k]
        I('dve', 'tensor_scalar', [ones512b, OHb], [ohbb], out=ohb[:], in0=ones512[:], scalar1=OH[:, t:t + 1], scalar2=None, op0=ALU.mult)
        I('dve', 'tensor_scalar', [b1_b], [b1_b], out=b1_t[:, 1:16:2], in0=b1_t[:, 1:16:2], scalar1=1.0, scalar2=None, op0=ALU.add)
        for fj in range(8):
            bA = (fj % 2) * 2
            bB = bA + 1
            for (bk, off) in ((bA, 0), (bB, 1)):
                for kc in range(8):
                    I('pe', 'matmul', [w1_b, xsTb], [psb[bk]], ps[bk][:, :], lhsT=w1_t[:, kc, 2 * fj + off:2048:16], rhs=xsT[:, kc, :],
                      start=(kc == 0), stop=(kc == 7))
            g_t, g_b = gbuf[fj % 2]
            s_t, s_b = sgbuf[fj % 2]
            l_t, l_b = lbuf[fj % 2]
            I('dve', 'tensor_scalar', [psb[bA], b1_b], [g_b], out=g_t[:], in0=ps[bA][:, :], scalar1=b1_t[:, 2 * fj:2 * fj + 1], scalar2=7.0, op0=ALU.add, op1=ALU.min)
            I('act', 'activation', [g_b], [s_b], out=s_t[:], in_=g_t[:], func=AF.Sigmoid, scale=1.702)
            I('act', 'activation', [psb[bB], b1_b], [l_b], out=l_t[:], in_=ps[bB][:, :], func=AF.Identity, bias=b1_t[:, 2 * fj + 1:2 * fj + 2], scale=1.0)
            I('dve', 'tensor_scalar', [l_b], [l_b], out=l_t[:], in0=l_t[:], scalar1=8.0, scalar2=-6.0, op0=ALU.min, op1=ALU.max)
            I('dve', 'tensor_tensor', [l_b, g_b], [l_b], out=l_t[:], in0=l_t[:], in1=g_t[:], op=ALU.mult)
            I('dve', 'tensor_tensor', [l_b, s_b], [actTb], out=actT[:, fj, :], in0=l_t[:], in1=s_t[:], op=ALU.mult)
        if t + 1 < NT:
            do_transposes(t + 1)
        for c in range(4):
            ys_t, ys_b = yst[ysn % 2]
            dsy = dsYs[ysn % 2]
            ysn += 1
            for hf in range(2):
                bk = 4 + hf
                for fj in range(8):
                    I('pe', 'matmul', [actTb, w2_b], [psb[bk]], ps[bk][:, :], lhsT=actT[:, fj, c * 128:(c + 1) * 128], rhs=w2_t[:, fj, hf * 512:(hf + 1) * 512],
                      start=(fj == 0), stop=False)
                I('pe', 'matmul', [ohbb, b2nb], [psb[bk]], ps[bk][:, :], lhsT=ohb[0:32, 0:128], rhs=b2n[0:32, hf * 512:(hf + 1) * 512], start=False, stop=True)
                if hf == 0:
                    I('act', 'activation', [psb[bk]], [ys_b], out=ys_t[:, 0:512], in_=ps[bk][:, :], func=AF.Copy)
                else:
                    I('dve', 'tensor_copy', [psb[bk]], [ys_b], out=ys_t[:, 512:1024], in_=ps[bk][:, :])
            P.dma('sp', ys_d[t * 512 + c * 128:t * 512 + (c + 1) * 128, :], ys_t[:], reads=[ys_b], dsem=dsy)
    P.barrier()
    esE.close()
    esD.close()
    if stop_after <= 3 and False:
        pass

    esC = ExitStack()
    Wpg, Wpgb = sb("Wpg", [128, 8, D], BF16, stack=esC)
    Wpp, Wppb = sb("Wpp", [128, 2, D], BF16, stack=esC)
    lnfin, lnfinb = sb("lnfin", [128, D], F32, stack=esC)
    lnbp, lnbpb = sb("lnbp", [128, 8, 128], F32, stack=esC)
    ds_p = DSem(P)
    P.dma('pool', Wpg[:], w_pg_d.rearrange("(kc p) c -> p kc c", p=128), writes=[Wpgb], dsem=ds_p)
    P.dma('pool', Wpp[:], w_pp_d.rearrange("(kc p) c -> p kc c", p=128), writes=[Wppb], dsem=ds_p)
    P.dma('sp', lnfin[:], lnfin_d, writes=[lnfinb], dsem=ds_p)
    for kc in range(8):
        I('dve', 'tensor_scalar', [cAb, lnpleTb], [lnbpb], out=lnbp[:, kc, :], in0=c_ones, scalar1=lnpleT[:, kc:kc + 1], scalar2=None, op0=ALU.mult)
    NP3 = 3
    x1l = [sb("x1l%d" % i, [128, D], F32, stack=esC) for i in range(NP3)]
    dsx1l = [DSem(P) for _ in range(NP3)]
    yk = [[sb("yk%d_%d" % (i, k), [128, D], F32, stack=esC) for k in range(4)] for i in range(NP3)]
    dsyk = [DSem(P) for _ in range(NP3)]
    pb32 = [sb("pb32_%d" % i, [128, 256], F32, stack=esC) for i in range(NP3)]
    dspb = [DSem(P) for _ in range(NP3)]
    pbl = [sb("pbl%d" % i, [128, 256], BF16, stack=esC) for i in range(2)]
    xp, xpb = sb("xp", [128, D], BF16, stack=esC)
    xpTs = [sb("xpT%d" % i, [128, 8, 128], BF16, stack=esC) for i in range(2)]
    pTs = [sb("pTt%d" % i, [128, 2, 128], BF16, stack=esC) for i in range(2)]
    sgp, sgpb = sb("sgp", [128, 512], F32, stack=esC)
    x3, x3b = sb("x3", [128, D], F32, stack=esC)
    ot = [sb("ot%d" % i, [128, D], F32, stack=esC) for i in range(2)]
    dso = [DSem(P) for _ in range(2)]

    nhalf, nhalfb = sb("nhalf", [128, 1], F32, stack=esC)
    I('dve', 'memset', [], [nhalfb], nhalf[:], -0.5)

    def rms_rstd_pow(src_ap, srcb, n, col):
        I('act', 'activation', [srcb], [junkb], out=junk[:, 0:n], in_=src_ap, func=AF.Square)
        I('dve', 'tensor_reduce', [junkb], [ssb], out=ss[:, col:col + 1], in_=junk[:, 0:n], axis=AX.X, op=ALU.add)
        I('dve', 'tensor_scalar', [ssb], [ssb], out=ss[:, col + 1:col + 2], in0=ss[:, col:col + 1], scalar1=1.0 / n, scalar2=EPS, op0=ALU.mult, op1=ALU.add)
        I('pool', 'tensor_tensor', [ssb, nhalfb], [ssb], out=ss[:, col + 2:col + 3], in0=ss[:, col + 1:col + 2], in1=nhalf[:], op=ALU.pow)
        return ss[:, col + 2:col + 3]

    def stageL(i):
        x1_t, x1_b = x1l[i % NP3]
        P.dma('sp', x1_t[:], x1_d[i * 128:(i + 1) * 128, :], writes=[x1_b], dsem=dsx1l[i % NP3])
        P.dma('sp', pb32[i % NP3][0][:], p_d[i * 128:(i + 1) * 128, :], writes=[pb32[i % NP3][1]], dsem=dspb[i % NP3])
        for k in range(4):
            P.op('pool', lambda e, k=k, i=i: e.indirect_dma_start(
                out=yk[i % NP3][k][0][:], out_offset=None, in_=ys_d, in_offset=bass.IndirectOffsetOnAxis(ap=slots_i[:, i, k:k + 1], axis=0)),
                reads=[slots_ib], writes=[yk[i % NP3][k][1]], dsem=dsyk[i % NP3], is_dma=True)

    def stageA(i):
        x1_t, x1_b = x1l[i % NP3]
        pb_t, pb_b = pbl[i % 2]
        I('act', 'activation', [pb32[i % NP3][1]], [pb_b], out=pb_t[:], in_=pb32[i % NP3][0][:], func=AF.Copy)
        for k in range(4):
            y_t, y_b = yk[i % NP3][k]
            I('dve', 'scalar_tensor_tensor', [y_b, gate4b, x1_b], [x1_b], out=x1_t[:], in0=y_t[:], scalar=gate4[:, i, k:k + 1], in1=x1_t[:], op0=ALU.mult, op1=ALU.add)
        if dbg:
            P.dma('sp', dbg_d["x2"][i * 128:(i + 1) * 128, :], x1_t[:], reads=[x1_b], dsem=DSem(P))
        r = rms_rstd(x1_t[:], x1_b, D, 0)
        I('dve', 'tensor_scalar', [x1_b, ssb], [xpb], out=xp[:], in0=x1_t[:], scalar1=r, scalar2=None, op0=ALU.mult)
        xpT, xpTb = xpTs[i % 2]
        pT_, pTb_ = pTs[i % 2]
        pT = psbf(2)
        for kc in range(8):
            I('pe', 'transpose', [xpb, cBb], [psb[2]], out=pT[:, kc * 128:(kc + 1) * 128], in_=xp[:, kc * 128:(kc + 1) * 128], identity=ident)
        I('dve', 'tensor_tensor', [psb[2], lnbpb], [xpTb], out=xpT[:], in0=pT.rearrange("p (a b) -> p a b", a=8), in1=lnbp[:], op=ALU.mult)
        pT5 = psbf(5)
        for kc in range(2):
            I('pe', 'transpose', [pb_b, cBb], [psb[5]], out=pT5[:, kc * 128:(kc + 1) * 128], in_=pb_t[:, kc * 128:(kc + 1) * 128], identity=ident)
        I('act', 'activation', [psb[5]], [pTb_], out=pT_[:], in_=pT5[:, 0:256].rearrange("p (a b) -> p a b", a=2), func=AF.Copy)

    def stageB(i):
        x1_t, x1_b = x1l[i % NP3]
        xpT, xpTb = xpTs[i % 2]
        pT_, pTb_ = pTs[i % 2]
        for hf in range(2):
            for kc in range(8):
                I('pe', 'matmul', [xpTb, Wpgb], [psb[hf]], ps[hf][:, :], lhsT=xpT[:, kc, :], rhs=Wpg[:, kc, hf * 512:(hf + 1) * 512], start=(kc == 0), stop=(kc == 7))
            for kc in range(2):
                I('pe', 'matmul', [pTb_, Wppb], [psb[3 + hf]], ps[3 + hf][:, :], lhsT=pT_[:, kc, :], rhs=Wpp[:, kc, hf * 512:(hf + 1) * 512], start=(kc == 0), stop=(kc == 1))
            I('act', 'activation', [psb[hf]], [sgpb], out=sgp[:], in_=ps[hf][:, :], func=AF.Sigmoid)
            I('dve', 'tensor_tensor', [sgpb, psb[3 + hf]], [sgpb], out=sgp[:], in0=sgp[:], in1=ps[3 + hf][:, :], op=ALU.mult)
            I('dve', 'tensor_tensor', [sgpb, x1_b], [x3b], out=x3[:, hf * 512:(hf + 1) * 512], in0=sgp[:], in1=x1_t[:, hf * 512:(hf + 1) * 512], op=ALU.add)
        r = rms_rstd(x3[:], x3b, D, 4)
        o_t, o_b = ot[i % 2]
        I('dve', 'scalar_tensor_tensor', [x3b, ssb, lnfinb], [o_b], out=o_t[:], in0=x3[:], scalar=r, in1=lnfin[:], op0=ALU.mult, op1=ALU.mult)
        P.dma('sp', out_d[i * 128:(i + 1) * 128, :], o_t[:], reads=[o_b], dsem=dso[i % 2])

    stageL(0)
    if NB > 1:
        stageL(1)
    stageA(0)
    for i in range(NB):
        if i + 2 < NB:
            stageL(i + 2)
        if i + 1 < NB:
            stageA(i + 1)
        stageB(i)
    P.emit(final_waits=[d for d in P.dsems if d.count > 0])
    esC.close()
    es.close()
    return nc


def make_consts(T):
    CAP = T
    s = np.arange(128)[:, None]
    t = np.arange(128)[None, :]
    ident = (s == t).astype(np.float32)
    triS = np.where(s <= t, -1.0 / 16.0, 0.0).astype(np.float32)
    tristrict = (s < t).astype(np.float32)
    ones = np.ones((128, 128), np.float32)
    maskA = (s <= t).astype(np.float32)
    maskA4 = np.tile(maskA, (1, 4))
    eoff = np.tile((np.arange(32) * CAP).astype(np.float32)[None, :], (128, 1))
    pk8 = (np.arange(8)[None, :] * 128 + np.arange(128)[:, None]).astype(np.float32)
    pc4 = (np.arange(4)[None, :] * 128 + np.arange(128)[:, None]).astype(np.float32)
    pidx = np.arange(128, dtype=np.float32)[:, None]
    tv = np.tile(np.arange(64, dtype=np.float32)[None, :], (128, 1))
    cA = np.concatenate([ident, triS, tristrict, ones, ident, ones, maskA4, eoff, pk8, pc4, pidx, tv], axis=1)
    maskadd = np.zeros((128, 5, 128), np.float32)
    maskadd[0:64, 0, 64:128] = NEG
    maskadd[64:128, 4, 0:64] = NEG
    return np.ascontiguousarray(cA.astype(np.float32)), maskadd


def make_shared(inputs, T):
    f = lambda a: np.ascontiguousarray(np.asarray(a, dtype=np.float32))
    cA, maskadd = make_consts(T)
    rel_bias = np.asarray(inputs["rel_bias"][0], np.float32)
    k = np.arange(128)[:, None, None]
    kb = np.arange(5)[None, :, None]
    q = np.arange(128)[None, None, :]
    rel = np.clip((512 + q) - (kb * 128 + k), -256, 256) + 256
    biasT = np.transpose(rel_bias[:, rel], (1, 0, 2, 3))
    sh = {
        "w_in": f(inputs["w_in"][0]),
        "wgk_aug": f(np.concatenate([inputs["w_gk"][0], inputs["b_gk"][0][None, :]], axis=0)),
        "biasT": f(biasT),
        "w_out": f(inputs["w_out"][0]),
        "w_router": f(inputs["w_router"][0]),
        "w1": f(np.asarray(inputs["w1"][0]).reshape(32 * 128, 8 * 2048)),
        "b1": f(np.asarray(inputs["b1"][0]).reshape(32 * 128, 16)),
        "w2": f(np.asarray(inputs["w2"][0]).reshape(32 * 128, 8 * D)),
        "b2": f(inputs["b2"][0]),
        "w_pg": f(inputs["w_ple_gate"][0]),
        "w_pp": f(inputs["w_ple_proj"][0]),
        "lnmixT": f(np.asarray(inputs["ln_mix"][0]).reshape(8, 128).T),
        "lnpleT": f(np.asarray(inputs["ln_ple"][0]).reshape(8, 128).T),
        "lnmoe_b": f(np.broadcast_to(np.asarray(inputs["ln_moe"][0])[None, :], (128, D))),
        "lnfin_b": f(np.broadcast_to(np.asarray(inputs["ln_final"])[None, :], (128, D))),
        "gnorm_b": f(np.broadcast_to(np.asarray(inputs["gla_norm"][0])[None, :], (128, 256))),
        "brout_b": f(np.broadcast_to(np.asarray(inputs["b_router"][0])[None, :], (128, 32))),
        "cA": cA,
        "maskadd": maskadd,
    }
    return sh


def kernel(**inputs):
    x = np.asarray(inputs["x"], np.float32)
    p = np.asarray(inputs["p"], np.float32)
    Bn, T, _ = x.shape
    nc = build_program(T)
    sh = make_shared(inputs, T)
    in_maps = []
    for c in range(Bn):
        m = dict(sh)
        m["x"] = np.ascontiguousarray(x[c])
        m["p"] = np.ascontiguousarray(p[0, c])
        in_maps.append(m)
    res = run_bass_kernel_spmd(nc, in_maps, core_ids=list(range(Bn)))
    return np.stack([np.asarray(r["out"], np.float32) for r in res.results], axis=0)
```

```python
import numpy as np
from contextlib import ExitStack
import concourse.bass as bass
import concourse.mybir as mybir
from concourse.bass_utils import run_bass_kernel_spmd

F32 = mybir.dt.float32
BF16 = mybir.dt.bfloat16
I32 = mybir.dt.int32
U32 = mybir.dt.uint32
AF = mybir.ActivationFunctionType
ALU = mybir.AluOpType
AX = mybir.AxisListType


class Buf:
    __slots__ = ("name", "last_w", "readers")

    def __init__(self, name):
        self.name = name
        self.last_w = None
        self.readers = []


class DSem:
    def __init__(self, prog):
        self.prog = prog
        self.count = 0
        self.handle = prog.new_sem()
        prog.dsems.append(self)


class Op:
    __slots__ = ("eng", "fn", "cdeps", "ddeps", "is_dma", "dsem", "needs_inc", "inc_val", "dma_val")


class Prog:
    ENGS = ("pe", "act", "dve", "pool", "sp")

    def __init__(self, nc, same_engine_sync=True):
        self.nc = nc
        self.es = ExitStack()
        self.q = {e: [] for e in self.ENGS}
        self.esem = {}
        self.same_engine_sync = same_engine_sync
        self.nsem = 0
        self.dsems = []
        self.pending = {}
        for e in self.ENGS:
            self.esem[e] = self.new_sem()

    def new_sem(self):
        self.nsem += 1
        return self.es.enter_context(self.nc.semaphore("s%d" % self.nsem))

    def op(self, eng, fn, reads=(), writes=(), dsem=None, is_dma=False):
        o = Op()
        o.eng = eng
        o.fn = fn
        o.is_dma = is_dma
        o.dsem = dsem
        o.needs_inc = False
        o.inc_val = None
        o.dma_val = None
        cdeps = set()
        ddeps = {}

        def add_dep(p):
            if p is None or p is o:
                return
            if p.is_dma:
                ds = p.dsem
                ddeps[id(ds)] = (ds, ds.count)
            else:
                if p.eng == eng and not is_dma and (eng == "pe" or not self.same_engine_sync):
                    return
                cdeps.add(p)

        pend = self.pending.pop(eng, None)
        if pend is not None:
            for p in pend[0]:
                if p.eng != eng or self.same_engine_sync:
                    if not (p.eng == eng and eng == "pe"):
                        cdeps.add(p)
            for ds, v in pend[1]:
                ddeps[id(ds)] = (ds, v)
        for b in reads:
            add_dep(b.last_w)
        for b in writes:
            add_dep(b.last_w)
            for r in b.readers:
                add_dep(r)
        for b in reads:
            b.readers.append(o)
        for b in writes:
            b.last_w = o
            b.readers = []
        for p in cdeps:
            p.needs_inc = True
        o.cdeps = cdeps
        o.ddeps = list(ddeps.values())
        if is_dma:
            dsem.count += 16
            o.dma_val = dsem.count
        self.q[eng].append(o)
        return o

    def barrier(self):
        last = []
        for e in self.ENGS:
            for o in reversed(self.q[e]):
                if not o.is_dma:
                    last.append(o)
                    break
        dsv = [(ds, ds.count) for ds in self.dsems if ds.count > 0]
        for e in self.ENGS:
            self.pending[e] = (last, dsv)

    def dma(self, eng, out, in_, reads=(), writes=(), dsem=None):
        return self.op(eng, lambda e: e.dma_start(out=out, in_=in_), reads=reads, writes=writes,
                       dsem=dsem, is_dma=True)

    def emit(self, final_waits=()):
        nc = self.nc
        for e in self.ENGS:
            c = 0
            for o in self.q[e]:
                if o.needs_inc and not o.is_dma:
                    c += 1
                    o.inc_val = c
        engmap = {"pe": "tensor", "act": "scalar", "dve": "vector", "pool": "gpsimd", "sp": "sync"}
        with nc.Block() as block:
            for e in self.ENGS:
                ops = self.q[e]
                esem = self.esem
                fw = final_waits if e == "sp" else ()

                def body(eng, ops=ops, e=e, fw=fw):
                    waited = {}
                    for o in ops:
                        waits = []
                        for p in o.cdeps:
                            waits.append((esem[p.eng], p.inc_val))
                        for ds, v in o.ddeps:
                            waits.append((ds.handle, v))
                        for h, v in waits:
                            k = id(h)
                            if waited.get(k, 0) >= v:
                                continue
                            waited[k] = v
                            eng.wait_ge(h, v)
                        ins = o.fn(eng)
                        if o.is_dma:
                            ins.then_inc(o.dsem.handle, 16)
                        elif o.needs_inc:
                            ins.then_inc(esem[e], 1)
                    for ds in fw:
                        eng.wait_ge(ds.handle, ds.count)

                getattr(block, engmap[e])(body)
        self.es.close()


D = 1024
DIN = 8208
EPS = 1e-6
NEG = -30000.0
DEFER_TAIL = True
QK_AHEAD = True
C_QG, C_KG, C_VG, C_GK, C_RG, C_QA, C_KA, C_VA, C_GA, C_GB = 0, 512, 1024, 2048, 2064, 3088, 4112, 5136, 6160, 7184


def build_program(T, dbg=False, stop_after=99, fill=0.0):
    NB = T // 128
    NSB = T // 512
    CAP = T
    NT = (4 * T) // 512 + 31
    NROWS = 32 * CAP + NT * 512
    nc = bass.Bass("TRN2", target_bir_lowering=False)

    def din(name, shape, dt=F32):
        return nc.dram_tensor(name, shape, dt, kind="ExternalInput").ap()

    def dscr(name, shape, dt, out=False):
        return nc.dram_tensor(name, shape, dt, kind=("ExternalOutput" if out else "Internal")).ap()

    x_d = din("x", [T, D])
    p_d = din("p", [T, 256])
    w_in = din("w_in", [D, DIN])
    wgk_d = din("wgk_aug", [17, 512])
    biasT_d = din("biasT", [128, 16, 5, 128])
    w_out_d = din("w_out", [D, D])
    w_router_d = din("w_router", [D, 32])
    w1_d = din("w1", [32 * 128, 8 * 2048])
    b1_d = din("b1", [32 * 128, 16])
    w2_d = din("w2", [32 * 128, 8 * D + 16])
    b2_d = din("b2", [32, D])
    w_pg_d = din("w_pg", [D, D])
    w_pp_d = din("w_pp", [256, D])
    lnmixT_d = din("lnmixT", [128, 8])
    lnpleT_d = din("lnpleT", [128, 8])
    lnmoe_d = din("lnmoe_b", [128, D])
    lnfin_d = din("lnfin_b", [128, D])
    gnorm_d = din("gnorm_b", [128, 256])
    brout_d = din("brout_b", [128, 32])
    cA_d = din("cA", [128, 128 * 6 + 512 + 32 + 8 + 4 + 1 + 64])
    maskadd_d = din("maskadd", [128, 5, 128])

    out_d = nc.dram_tensor("out", [T, D], F32, kind="ExternalOutput").ap()
    ha_d = dscr("ha_s", [T, D], BF16)
    x1_d = dscr("x1_s", [T, D], F32, out=dbg)
    xs_d = dscr("xs_s", [NT * 512, D], BF16)
    ys_d = dscr("ys_s", [NT * 512, D], F32)
    xm_d = dscr("xm_s", [T, D], BF16)
    LOGCAP = CAP.bit_length() - 1
    assert (1 << LOGCAP) == CAP
    dbg_d = {}
    if dbg:
        dbg_d["ha"] = dscr("dbg_ha", [T, D], F32, out=True)
        dbg_d["h"] = dscr("dbg_h", [T, D], F32, out=True)
        dbg_d["lg"] = dscr("dbg_lg", [T, 32], F32, out=True)
        dbg_d["x2"] = dscr("dbg_x2", [T, D], F32, out=True)

    P = Prog(nc)
    es = ExitStack()
    dumped = set()

    def dump(name, ap, b, shape, dt=F32):
        if not dbg or name in dumped:
            return
        dumped.add(name)
        dd = nc.dram_tensor("dmp_" + name, list(shape), dt, kind="ExternalOutput").ap()
        P.dma('sp', dd, ap, reads=[b], dsem=DSem(P))

    def sb(name, shape, dt, nb=1, stack=None):
        t = (stack or es).enter_context(nc.sbuf_tensor("sb_" + name, shape, dt))
        bufs = [Buf("%s_%d" % (name, i)) for i in range(nb)]
        return t, (bufs[0] if nb == 1 else bufs)

    def I(eng, method, reads, writes, *a, **k):
        return P.op(eng, lambda e: getattr(e, method)(*a, **k), reads=reads, writes=writes)

    ps = []
    psb = []
    for b in range(8):
        ps.append(es.enter_context(nc.psum_tensor("ps%d" % b, [128, 512], F32)))
        psb.append(Buf("ps%d" % b))

    def psbf(b):
        return ps[b][:].bitcast(BF16)

    NCA = 128 * 6 + 512 + 32 + 8 + 4 + 1 + 64
    cA, cAb = sb("cA", [128, NCA], F32)
    o = 0
    identf = cA[:, o:o + 128]; o += 128
    triS = cA[:, o:o + 128]; o += 128
    c_tristrict = cA[:, o:o + 128]; o += 128
    c_ones = cA[:, o:o + 128]; o += 128
    c_ident2 = cA[:, o:o + 128]; o += 128
    c_spare = cA[:, o:o + 128]; o += 128
    maskA4 = cA[:, o:o + 512]; o += 512
    eoff = cA[:, o:o + 32]; o += 32
    pk8 = cA[:, o:o + 8]; o += 8
    pc4 = cA[:, o:o + 4]; o += 4
    pidx = cA[:, o:o + 1]; o += 1
    tvals = cA[:, o:o + 64]; o += 64
    ds_c = DSem(P)
    P.dma('sp', cA[:], cA_d, writes=[cAb], dsem=ds_c)
    cB, cBb = sb("cB", [128, 3 * 128], BF16)
    ident = cB[:, 0:128]
    tristrict = cB[:, 128:256]
    ones_bf = cB[:, 256:384]
    lnmixT, lnmixTb = sb("lnmixT", [128, 8], F32)
    lnpleT, lnpleTb = sb("lnpleT", [128, 8], F32)
    P.dma('sp', lnmixT[:], lnmixT_d, writes=[lnmixTb], dsem=ds_c)
    P.dma('sp', lnpleT[:], lnpleT_d, writes=[lnpleTb], dsem=ds_c)
    gnorm, gnormb = sb("gnorm", [128, 256], F32)
    P.dma('sp', gnorm[:], gnorm_d, writes=[gnormb], dsem=ds_c)
    wgk, wgkb = sb("wgk", [17, 512], F32)
    P.dma('sp', wgk[:], wgk_d, writes=[wgkb], dsem=ds_c)
    I('dve', 'tensor_copy', [cAb], [cBb], out=cB[:, 0:128], in_=c_ident2)
    I('dve', 'tensor_copy', [cAb], [cBb], out=cB[:, 128:384], in_=cA[:, 256:512])
    lnb, lnbb = sb("lnb", [128, 8, 128], F32)
    for kc in range(8):
        I('dve', 'tensor_scalar', [cAb, lnmixTb], [lnbb], out=lnb[:, kc, :], in0=c_ones, scalar1=lnmixT[:, kc:kc + 1],
          scalar2=None, op0=ALU.mult)

    ss, ssb = sb("ss", [128, 8], F32)
    junk, junkb = sb("junk", [128, 1024], F32)
    slots_i, slots_ib = sb("slots_i", [128, NB, 4], I32)
    gate4, gate4b = sb("gate4", [128, NB, 4], F32)
    cm, cmb = sb("cm", [128, 32], F32)
    cmbf, cmbfb = sb("cmbf", [128, 32], BF16)
    epsc, epscb = sb("epsc", [128, 1], F32)
    zt, ztb = sb("zt", [128, D], BF16)
    I('pool', 'memset', [], [ztb], zt[:], fill)
    ds_z = DSem(P)
    zfill_next = [0]

    def zero_fill(n):
        for _ in range(n):
            t_ = zfill_next[0]
            if t_ >= NT:
                return
            zfill_next[0] += 1
            P.dma('sp', xs_d[t_ * 512:(t_ + 1) * 512, :].rearrange("(c p) d -> p c d", p=128), zt[:].unsqueeze(1).to_broadcast([128, 4, D]), reads=[ztb], dsem=ds_z)
    esAB = ExitStack()
    xbuf = [sb("xb%d" % i, [128, D], F32, stack=esAB) for i in range(2)]
    dsx = [DSem(P) for _ in range(2)]
    xn, xnb = sb("xn", [128, D], BF16, stack=esAB)
    xnT, xnTb = sb("xnT", [128, 8, 512], BF16, stack=esAB)
    NRING = 4
    wring = [sb("wr%d" % i, [128, 8, 512], BF16, stack=esAB) for i in range(NRING)]
    dsw = [DSem(P) for _ in range(NRING)]
    wcount = [0]

    def rms_rstd(src_ap, srcb, n, col):
        I('act', 'activation', [srcb], [junkb], out=junk[:, 0:n], in_=src_ap, func=AF.Square)
        I('dve', 'tensor_reduce', [junkb], [ssb], out=ss[:, col:col + 1], in_=junk[:, 0:n], axis=AX.X, op=ALU.add)
        I('act', 'activation', [ssb], [ssb], out=ss[:, col + 1:col + 2], in_=ss[:, col:col + 1], func=AF.Ln,
          scale=1.0 / n, bias=epsc[:, 0:1])
        I('act', 'activation', [ssb], [ssb], out=ss[:, col + 2:col + 3], in_=ss[:, col + 1:col + 2], func=AF.Exp,
          scale=-0.5)
        return ss[:, col + 2:col + 3]

    I('dve', 'memset', [], [epscb], epsc[:], EPS)

    def load_x(i):
        xt, xtb = xbuf[i % 2]
        P.dma('sp', xt[:], x_d[i * 128:(i + 1) * 128, :], writes=[xtb], dsem=dsx[i % 2])

    def load_norm_A(i, j, dma=True):
        xt, xtb = xbuf[i % 2]
        if dma:
            load_x(i)
        r = rms_rstd(xt[:], xtb, D, 0)
        I('dve', 'tensor_scalar', [xtb, ssb], [xnb], out=xn[:], in0=xt[:], scalar1=r, scalar2=None, op0=ALU.mult)

    def load_norm_T(i, j, lnbt, lnbtb):
        load_norm_A(i, j)
        load_norm_B(j, lnbt, lnbtb)

    def load_norm_B(j, lnbt, lnbtb):
        pT = psbf(2)
        for kc in range(8):
            I('pe', 'transpose', [xnb, cBb], [psb[2]], out=pT[:, kc * 128:(kc + 1) * 128],
              in_=xn[:, kc * 128:(kc + 1) * 128], identity=ident)
        I('dve', 'tensor_tensor', [psb[2], lnbtb], [xnTb], out=xnT[:, :, j * 128:(j + 1) * 128],
          in0=pT.rearrange("p (a b) -> p a b", a=8), in1=lnbt[:], op=ALU.mult)

    w_in_v = w_in.rearrange("(kc p) c -> p kc c", p=128)

    def load_wchunk(c0, ncols):
        k = wcount[0] % NRING
        wcount[0] += 1
        wt, wtb = wring[k]
        P.dma('pool', wt[:, :, 0:ncols], w_in_v[:, :, c0:c0 + ncols], writes=[wtb], dsem=dsw[k])
        return wt, wtb

    pbank = [0]

    def proj_fm(wt, wtb, sub, nsub, evac):
        for q in range(nsub):
            b = pbank[0] % 2
            pbank[0] += 1
            for kc in range(8):
                I('pe', 'matmul', [wtb, xnTb], [psb[b]], ps[b][0:sub, :], lhsT=wt[:, kc, q * sub:(q + 1) * sub],
                  rhs=xnT[:, kc, :], start=(kc == 0), stop=(kc == 7))
            evac(q, ps[b], psb[b])

    def proj_tm(wt, wtb, evac):
        for j in range(4):
            b = pbank[0] % 2
            pbank[0] += 1
            for kc in range(8):
                I('pe', 'matmul', [wtb, xnTb], [psb[b]], ps[b][:, :], lhsT=xnT[:, kc, j * 128:(j + 1) * 128],
                  rhs=wt[:, kc, :], start=(kc == 0), stop=(kc == 7))
            evac(j, ps[b], psb[b])

    esG = ExitStack()
    qTg, qTgb = sb("qTg", [128, 4, 512], BF16, stack=esG)
    kTg, kTgb = sb("kTg", [128, 4, 512], BF16, stack=esG)
    gka, gkab = sb("gka", [32, 512], F32, stack=esG)
    vg, vgb = sb("vg", [128, 4, D], BF16, stack=esG)
    rs, rsb = sb("rs", [128, 4, D], BF16, stack=esG)
    sgt, sgtb = sb("sgt", [128, 512], BF16, stack=esG)
    Sst, Sstb = sb("Sst", [128, 4, 256], F32, stack=esG)
    Sbf, Sbfb = sb("Sbf", [128, 4, 256], BF16, stack=esG)
    e1t, e1tb = sb("e1t", [128, 512], F32, stack=esG)
    lt, ltb = sb("lt", [128, 512], F32, stack=esG)
    E1s = [sb("E1_%d" % i, [128, 512], F32, stack=esG) for i in range(2)]
    E3, E3b = sb("E3", [128, 512], F32, stack=esG)
    qtls = [sb("qtl%d" % i, [128, 512], BF16, stack=esG) for i in range(2)]
    ktl, ktlb = sb("ktl", [128, 512], BF16, stack=esG)
    khT, khTb = sb("khT", [128, 512], BF16, stack=esG)
    khs = [sb("kh%d" % i, [128, 512], BF16, stack=esG) for i in range(2)]
    ATs = [sb("AT%d" % i, [128, 512], BF16, stack=esG) for i in range(2)]
    yg, ygb = sb("yg", [128, D], F32, stack=esG)
    ss4, ss4b = sb("ss4", [128, 12], F32, stack=esG)
    hab = [sb("ha%d" % i, [128, D], BF16, stack=esG) for i in range(2)]
    dsha = [DSem(P) for _ in range(2)]
    I('pool', 'memset', [], [gkab], gka[:], 1.0)
    I('pool', 'memset', [], [Sstb], Sst[:], 0.0)
    I('pool', 'memset', [], [Sbfb], Sbf[:], 0.0)

    WG, _ = sb("WG", [128, 8, 4112], BF16, stack=esG)
    wg_chunks = {}
    for (c0, ncols, o0) in ((C_QG, 512, 0), (C_KG, 512, 512), (C_GK, 16, 2048), (C_VG, 512, 1024), (C_VG + 512, 512, 1536),
                            (C_RG, 512, 2064), (C_RG + 512, 512, 2576), (C_GA, 512, 3088), (C_GA + 512, 512, 3600)):
        bb = Buf("wg%d" % c0)
        P.dma('pool', WG[:, :, o0:o0 + ncols], w_in_v[:, :, c0:c0 + ncols], writes=[bb], dsem=DSem(P))
        wg_chunks[c0] = (WG[:, :, o0:o0 + ncols], bb)

    def load_wchunk_g(c0, ncols):
        return wg_chunks[c0]

    for j in range(4):
        load_norm_T(j, j, lnb, lnbb)
    for s in range(NSB):
        dump("xnT", xnT[:], xnTb, [128, 8, 512], BF16)
        wt, wtb = load_wchunk_g(C_QG, 512)
        proj_fm(wt, wtb, 128, 4, lambda q, pt, ptb: I('act', 'activation', [ptb], [qTgb], out=qTg[:, q, :], in_=pt[:, :], func=AF.Copy))
        wt, wtb = load_wchunk_g(C_KG, 512)
        proj_fm(wt, wtb, 128, 4, lambda q, pt, ptb: I('dve', 'tensor_copy', [ptb], [kTgb], out=kTg[:, q, :], in_=pt[:, :]))
        wt, wtb = load_wchunk_g(C_GK, 16)
        proj_fm(wt, wtb, 16, 1, lambda q, pt, ptb: I('dve', 'tensor_copy', [ptb], [gkab], out=gka[0:16, :], in_=pt[0:16, :]))
        for hf in range(2):
            wt, wtb = load_wchunk_g(C_VG + hf * 512, 512)
            proj_tm(wt, wtb, lambda j, pt, ptb, hf=hf: I('act', 'activation', [ptb], [vgb], out=vg[:, j, hf * 512:(hf + 1) * 512], in_=pt[:, :], func=AF.Copy))
        for hf in range(2):
            wt, wtb = load_wchunk_g(C_RG + hf * 512, 512)
            proj_tm(wt, wtb, lambda j, pt, ptb, hf=hf: I('act', 'activation', [ptb], [rsb], out=rs[:, j, hf * 512:(hf + 1) * 512], in_=pt[:, :], func=AF.Silu))
        for hf in range(2):
            wt, wtb = load_wchunk_g(C_GA + hf * 512, 512)

            def ev(j, pt, ptb, hf=hf):
                I('act', 'activation', [ptb], [sgtb], out=sgt[:], in_=pt[:, :], func=AF.Sigmoid)
                I('dve', 'tensor_tensor', [sgtb, rsb], [rsb], out=rs[:, j, hf * 512:(hf + 1) * 512],
                  in0=rs[:, j, hf * 512:(hf + 1) * 512], in1=sgt[:], op=ALU.mult)
            proj_tm(wt, wtb, ev)
        dump("qTg", qTg[:], qTgb, [128, 4, 512], BF16)
        dump("kTg", kTg[:], kTgb, [128, 4, 512], BF16)
        dump("gka", gka[:], gkab, [32, 512])
        dump("vg", vg[:], vgb, [128, 4, D], BF16)
        dump("rs", rs[:], rsb, [128, 4, D], BF16)
        def gla_front(j, s=s):
            tok = slice(j * 128, (j + 1) * 128)
            E1, E1b = E1s[j % 2]
            qtl, qtlb = qtls[j % 2]
            kh, khb = khs[j % 2]
            AT, ATb = ATs[j % 2]
            I('pe', 'matmul', [gkab, wgkb], [psb[3]], ps[3][:, :], lhsT=gka[0:17, tok], rhs=wgk[0:17, :], start=True, stop=True)
            I('act', 'activation', [psb[3]], [e1tb], out=e1t[:], in_=ps[3][:, :], func=AF.Exp, scale=-1.0)
            I('act', 'activation', [e1tb], [ltb], out=lt[:], in_=e1t[:], func=AF.Ln, bias=1.0)
            for h in range(4):
                I('pe', 'matmul', [ltb, cAb], [psb[4]], ps[4][:, h * 128:(h + 1) * 128], lhsT=lt[:, h * 128:(h + 1) * 128],
                  rhs=triS, start=True, stop=True)
            I('act', 'activation', [psb[4]], [E1b], out=E1[:], in_=ps[4][:, :], func=AF.Exp)
            I('act', 'activation', [psb[4]], [E3b], out=E3[:], in_=ps[4][:, :], func=AF.Exp, scale=-1.0)
            I('dve', 'scalar_tensor_tensor', [E1b, qTgb], [qtlb], out=qtl[:].rearrange("p (h t) -> p h t", h=4), in0=E1[:].rearrange("p (h t) -> p h t", h=4),
              scalar=128.0 ** -0.5, in1=qTg[:, :, tok], op0=ALU.mult, op1=ALU.mult)
            I('dve', 'tensor_tensor', [E3b, kTgb], [ktlb], out=ktl[:].rearrange("p (h t) -> p h t", h=4), in0=E3[:].rearrange("p (h t) -> p h t", h=4),
              in1=kTg[:, :, tok], op=ALU.mult)
            for h in range(4):
                I('dve', 'tensor_scalar', [ktlb, E1b], [khTb], out=khT[:, h * 128:(h + 1) * 128], in0=ktl[:, h * 128:(h + 1) * 128],
                  scalar1=E1[:, h * 128 + 127:h * 128 + 128], scalar2=None, op0=ALU.mult)
            pT = psbf(2)
            for h in range(4):
                I('pe', 'transpose', [khTb, cBb], [psb[2]], out=pT[:, h * 128:(h + 1) * 128], in_=khT[:, h * 128:(h + 1) * 128], identity=ident)
            I('act', 'activation', [psb[2]], [khb], out=kh[:], in_=pT[:, 0:512], func=AF.Copy)
            for h in range(4):
                I('pe', 'matmul', [ktlb, qtlb], [psb[3]], ps[3][:, h * 128:(h + 1) * 128], lhsT=ktl[:, h * 128:(h + 1) * 128],
                  rhs=qtl[:, h * 128:(h + 1) * 128], start=True, stop=True)
            I('dve', 'tensor_tensor', [psb[3], cAb], [ATb], out=AT[:], in0=ps[3][:, :], in1=maskA4, op=ALU.mult)

        def gla_back(j, s=s):
            i = 4 * s + j
            E1, E1b = E1s[j % 2]
            qtl, qtlb = qtls[j % 2]
            kh, khb = khs[j % 2]
            AT, ATb = ATs[j % 2]
            for h in range(4):
                bk = 5 + h // 2
                oc = slice((h % 2) * 256, (h % 2) * 256 + 256)
                I('pe', 'matmul', [ATb, vgb], [psb[bk]], ps[bk][:, oc], lhsT=AT[:, h * 128:(h + 1) * 128], rhs=vg[:, j, h * 256:(h + 1) * 256],
                  start=True, stop=False)
                I('pe', 'matmul', [qtlb, Sbfb], [psb[bk]], ps[bk][:, oc], lhsT=qtl[:, h * 128:(h + 1) * 128], rhs=Sbf[:, h, :],
                  start=False, stop=True)
            for hh in range(2):
                for h2 in range(2):
                    h = hh * 2 + h2
                    I('pe', 'matmul', [khb, vgb], [psb[7]], ps[7][:, h2 * 256:(h2 + 1) * 256], lhsT=kh[:, h * 128:(h + 1) * 128],
                      rhs=vg[:, j, h * 256:(h + 1) * 256], start=True, stop=True)
                for h2 in range(2):
                    h = hh * 2 + h2
                    I('dve', 'scalar_tensor_tensor', [Sstb, E1b, psb[7]], [Sstb], out=Sst[:, h, :], in0=Sst[:, h, :],
                      scalar=E1[:, h * 128 + 127:h * 128 + 128], in1=ps[7][:, h2 * 256:(h2 + 1) * 256], op0=ALU.mult, op1=ALU.add)
            I('pool', 'tensor_copy', [Sstb], [Sbfb], out=Sbf[:], in_=Sst[:])
            I('act', 'activation', [psb[5]], [junkb], out=junk[:, 0:512], in_=ps[5][:, :], func=AF.Square)
            I('act', 'activation', [psb[6]], [junkb], out=junk[:, 512:1024], in_=ps[6][:, :], func=AF.Square)
            I('dve', 'tensor_reduce', [junkb], [ss4b], out=ss4[:, 0:4], in_=junk[:].rearrange("p (h d) -> p h d", h=4), axis=AX.X, op=ALU.add)
            I('act', 'activation', [ss4b], [ss4b], out=ss4[:, 4:8], in_=ss4[:, 0:4], func=AF.Ln, scale=1.0 / 256, bias=epsc[:, 0:1])
            I('act', 'activation', [ss4b], [ss4b], out=ss4[:, 8:12], in_=ss4[:, 4:8], func=AF.Exp, scale=-0.5)
            for h in range(4):
                bk = 5 + h // 2
                oc = slice((h % 2) * 256, (h % 2) * 256 + 256)
                I('dve', 'scalar_tensor_tensor', [psb[bk], ss4b, gnormb], [ygb], out=yg[:, h * 256:(h + 1) * 256], in0=ps[bk][:, oc],
                  scalar=ss4[:, 8 + h:9 + h], in1=gnorm[:], op0=ALU.mult, op1=ALU.mult)
            hat, hatb = hab[i % 2]
            I('dve', 'tensor_tensor', [ygb, rsb], [hatb], out=hat[:], in0=yg[:], in1=rs[:, j, :], op=ALU.mult)
            P.dma('sp', ha_d[i * 128:(i + 1) * 128, :], hat[:], reads=[hatb], dsem=dsha[i % 2])
            if dbg:
                I('dve', 'tensor_copy', [hatb, ygb], [ygb], out=yg[:], in_=hat[:])
                P.dma('sp', dbg_d["ha"][i * 128:(i + 1) * 128, :], yg[:], reads=[ygb], dsem=dsha[i % 2])

        gla_front(0)
        for j in range(4):
            zero_fill((NT + 4 * NSB - 1) // (4 * NSB))
            if s + 1 < NSB:
                load_x(4 * (s + 1) + j)
            if j + 1 < 4:
                gla_front(j + 1)
            gla_back(j)
            if s + 1 < NSB:
                if j > 0:
                    load_norm_B(j - 1, lnb, lnbb)
                load_norm_A(4 * (s + 1) + j, j, dma=False)
        if s + 1 < NSB:
            load_norm_B(3, lnb, lnbb)
    zero_fill(NT)
    P.barrier()
    esG.close()
    if stop_after <= 1:
        P.emit(final_waits=[d for d in P.dsems if d.count > 0])
        es.close()
        return nc

    I('pool', 'memset', [], [cmb], cm[:], 0.0)
    I('pool', 'memset', [], [cmbfb], cmbf[:], 0.0)

    esB = ExitStack()
    qTa, qTab = sb("qTa", [128, 8, 512], BF16, stack=esB)
    kTa = [sb("kTa%d" % i, [128, 8, 512], BF16, stack=esB) for i in range(2)]
    vaug = [sb("vaug%d" % i, [128, 4, 16, 65], BF16, stack=esB) for i in range(2)]
    sgb, sgbb = sb("sgb", [128, 4, D], BF16, stack=esB)
    BMT, BMTb = sb("BMT", [128, 16, 5, 128], BF16, stack=esB)
    Wout, Woutb = sb("Wout", [128, 8, D], BF16, stack=esB)
    lnmoe, lnmoeb = sb("lnmoe", [128, D], F32, stack=esB)
    brout, broutb = sb("brout", [128, 32], F32, stack=esB)
    wr32, wr32b = sb("wr32", [128, 8, 32], F32, stack=esB)
    madd, maddb = sb("madd", [128, 5, 128], F32, stack=esB)
    bstg = [sb("bstg%d" % i, [128, 5, 128], F32, stack=esB) for i in range(2)]
    dsbs = [DSem(P) for _ in range(2)]
    PT = [sb("PT%d" % i, [128, 640], BF16, stack=esB) for i in range(2)]
    yb, ybb = sb("yb", [128, D], F32, stack=esB)
    rden, rdenb = sb("rden", [128, 4], F32, stack=esB)
    hld = [sb("hld%d" % i, [128, D], BF16, stack=esB) for i in range(2)]
    dshl = [DSem(P) for _ in range(2)]
    hm, hmb = sb("hm", [128, D], BF16, stack=esB)
    hT, hTb = sb("hT", [128, 8, 128], BF16, stack=esB)
    xres, xresb = sb("xres", [128, D], F32, stack=esB)
    dsxr = DSem(P)
    x1t, x1tb = sb("x1t", [128, D], F32, stack=esB)
    dsx1 = DSem(P)
    xm32, xm32b = sb("xm32", [128, D], F32, stack=esB)
    xmb = [sb("xmb%d" % i, [128, D], BF16, stack=esB) for i in range(2)]
    dsxm = [DSem(P) for _ in range(2)]
    xmT32, xmT32b = sb("xmT32", [128, 8, 128], F32, stack=esB)
    lg, lgb = sb("lg", [128, 32], F32, stack=esB)
    m8, m8b = sb("m8", [128, 8], F32, stack=esB)
    mask, maskb = sb("mask", [128, 32], F32, stack=esB)
    maskbf, maskbfb = sb("maskbf", [128, 32], BF16, stack=esB)
    ex, exb = sb("ex", [128, 32], F32, stack=esB)
    gates, gatesb = sb("gates", [128, 32], F32, stack=esB)
    slotf, slotfb = sb("slotf", [128, 32], F32, stack=esB)
    oh4, oh4b = sb("oh4", [128, 4, 32], F32, stack=esB)
    i8, i8b = sb("i8", [128, 8], U32, stack=esB)
    i8f, i8fb = sb("i8f", [128, 8], F32, stack=esB)
    jk32, jk32b = sb("jk32", [128, 32], F32, stack=esB)
    sm, smb = sb("sm", [128, 8], F32, stack=esB)
    slot4f, slot4fb = sb("slot4f", [128, 4], F32, stack=esB)
    YB = (5, 2)
    pending_tail = []
    _dbgs = []

    def dbgsem():
        if len(_dbgs) < 6:
            _dbgs.append(DSem(P))
            return _dbgs[-1]
        return _dbgs[len(_dbgs) % 6 - 1]
    ds_b = DSem(P)
    P.dma('sp', lnmoe[:], lnmoe_d, writes=[lnmoeb], dsem=ds_b)
    P.dma('sp', brout[:], brout_d, writes=[broutb], dsem=ds_b)
    P.dma('sp', wr32[:], w_router_d.rearrange("(kc p) e -> p kc e", p=128), writes=[wr32b], dsem=ds_b)
    P.dma('sp', madd[:], maskadd_d, writes=[maddb], dsem=ds_b)
    P.dma('pool', Wout[:], w_out_d.rearrange("(kc p) c -> p kc c", p=128), writes=[Woutb], dsem=ds_b)
    for h in range(16):
        st, stb = bstg[h % 2]
        P.dma('sp', st[:], biasT_d[:, h], writes=[stb], dsem=dsbs[h % 2])
        I('dve', 'tensor_tensor', [stb, maddb], [BMTb], out=BMT[:, h], in0=st[:], in1=madd[:], op=ALU.add)
    for i2 in range(2):
        I('pool', 'memset', [], [vaug[i2][1]], vaug[i2][0][:], 1.0)

    for j in range(4):
        load_norm_T(j, j, lnb, lnbb)
    for s in range(NSB):
        kT_t, kT_b = kTa[s % 2]
        va_t, va_b = vaug[s % 2]
        def pop_tail():
            if pending_tail:
                pending_tail.pop(0)()

        for hf in range(2):
            wt, wtb = load_wchunk(C_QA + hf * 512, 512)
            proj_fm(wt, wtb, 128, 4, lambda q, pt, ptb, hf=hf: I('act', 'activation', [ptb], [qTab], out=qTa[:, hf * 4 + q, :], in_=pt[:, :], func=AF.Copy, scale=0.125))
            pop_tail()
        for hf in range(2):
            wt, wtb = load_wchunk(C_KA + hf * 512, 512)
            proj_fm(wt, wtb, 128, 4, lambda q, pt, ptb, hf=hf: I('dve', 'tensor_copy', [ptb], [kT_b], out=kT_t[:, hf * 4 + q, :], in_=pt[:, :]))
            pop_tail()
        for hf in range(2):
            wt, wtb = load_wchunk(C_VA + hf * 512, 512)
            proj_tm(wt, wtb, lambda j, pt, ptb, hf=hf: I('act', 'activation', [ptb], [va_b], out=va_t[:, j, hf * 8:(hf + 1) * 8, 0:64],
                                                           in_=pt[:, :].rearrange("p (h d) -> p h d", h=8), func=AF.Copy))
            pop_tail()
        for hf in range(2):
            wt, wtb = load_wchunk(C_GB + hf * 512, 512)
            proj_tm(wt, wtb, lambda j, pt, ptb, hf=hf: I('act', 'activation', [ptb], [sgbb], out=sgb[:, j, hf * 512:(hf + 1) * 512], in_=pt[:, :], func=AF.Sigmoid))
            pop_tail()
        while pending_tail:
            pending_tail.pop(0)()

        for j in range(4):
            i = 4 * s + j
            tok = slice(j * 128, (j + 1) * 128)
            hl_t, hl_b = hld[i % 2]
            P.dma('sp', hl_t[:], ha_d[i * 128:(i + 1) * 128, :], writes=[hl_b], dsem=dshl[i % 2])
            if s + 1 < NSB:
                load_x(4 * (s + 1) + j)
            kbis = [kbi for kbi in range(5) if i - 4 + kbi >= 0]
            lo = kbis[0]

            def QK(h, i=i, tok=tok, kbis=kbis):
                hp, hh = h // 2, h % 2
                r0 = hh * 64
                xb_ = 3 + hh
                for kbi in kbis:
                    kb = i - 4 + kbi
                    kslot_t, kslot_b = kTa[(kb // 4) % 2]
                    koff = (kb % 4) * 128
                    if kbi < 4:
                        reg = ps[xb_][:, kbi * 128:(kbi + 1) * 128]
                        regb = psb[xb_]
                    else:
                        reg = ps[YB[hh]][:, 0:128]
                        regb = psb[YB[hh]]
                    I('pe', 'matmul', [cBb, BMTb], [regb], reg, lhsT=ident, rhs=BMT[:, h, kbi, :], start=True, stop=False)
                    I('pe', 'matmul', [kslot_b, qTab], [regb], reg, lhsT=kslot_t[r0:r0 + 64, hp, koff:koff + 128],
                      rhs=qTa[r0:r0 + 64, hp, tok], start=False, stop=True)

            def EXPPV(h, i=i, kbis=kbis, lo=lo):
                hh = h % 2
                xb_ = 3 + hh
                pt_t, pt_b = PT[hh]
                if lo < 4:
                    I('act', 'activation', [psb[xb_]], [pt_b], out=pt_t[:, lo * 128:512], in_=ps[xb_][:, lo * 128:512], func=AF.Exp)
                I('act', 'activation', [psb[YB[hh]]], [pt_b], out=pt_t[:, 512:640], in_=ps[YB[hh]][:, 0:128], func=AF.Exp)
                hq = h % 4
                for kbi in kbis:
                    kb = i - 4 + kbi
                    vslot_t, vslot_b = vaug[(kb // 4) % 2]
                    I('pe', 'matmul', [pt_b, vslot_b], [psb[6]], ps[6][:, hq * 65:(hq + 1) * 65], lhsT=pt_t[:, kbi * 128:(kbi + 1) * 128],
                      rhs=vslot_t[:, kb % 4, h, :], start=(kbi == kbis[0]), stop=(kbi == kbis[-1]))
                if hq == 3:
                    o3 = ps[6][:, 0:260].rearrange("p (h d) -> p h d", h=4)
                    I('dve', 'reciprocal', [psb[6]], [rdenb], out=rden[:], in_=o3[:, :, 64])
                    for q4 in range(4):
                        hh4 = h - 3 + q4
                        I('dve', 'tensor_scalar', [psb[6], rdenb], [ybb], out=yb[:, hh4 * 64:(hh4 + 1) * 64], in0=ps[6][:, q4 * 65:q4 * 65 + 64],
                          scalar1=rden[:, q4:q4 + 1], scalar2=None, op0=ALU.mult)

            if QK_AHEAD:
                QK(0)
            for h in range(16):
                if QK_AHEAD:
                    if h + 1 < 16:
                        QK(h + 1)
                else:
                    QK(h)
                EXPPV(h)
                if pending_tail and h in (1, 3, 5, 8, 11, 14):
                    pending_tail.pop(0)()
            while pending_tail:
                pending_tail.pop(0)()
            I('dve', 'tensor_tensor', [ybb, sgbb], [ybb], out=yb[:], in0=yb[:], in1=sgb[:, j, :], op=ALU.mult)
            I('dve', 'tensor_tensor', [ybb, hl_b], [hmb], out=hm[:], in0=yb[:], in1=hl_t[:], op=ALU.add)
            if dbg:
                I('dve', 'tensor_tensor', [ybb, hl_b], [ybb], out=yb[:], in0=yb[:], in1=hl_t[:], op=ALU.add)
                P.dma('sp', dbg_d["h"][i * 128:(i + 1) * 128, :], yb[:], reads=[ybb], dsem=dbgsem())

            def T1(i=i):
                pT = psbf(2)
                for kc in range(8):
                    I('pe', 'transpose', [hmb, cBb], [psb[2]], out=pT[:, kc * 128:(kc + 1) * 128], in_=hm[:, kc * 128:(kc + 1) * 128], identity=ident)
                I('act', 'activation', [psb[2]], [hTb], out=hT[:], in_=pT.rearrange("p (a b) -> p a b", a=8), func=AF.Copy)
                P.dma('sp', xres[:], x_d[i * 128:(i + 1) * 128, :], writes=[xresb], dsem=dsxr)

            def T2(i=i):
                for hf in range(2):
                    for kc in range(8):
                        I('pe', 'matmul', [hTb, Woutb], [psb[hf]], ps[hf][:, :], lhsT=hT[:, kc, :], rhs=Wout[:, kc, hf * 512:(hf + 1) * 512],
                          start=(kc == 0), stop=(kc == 7))
                    I('dve', 'tensor_tensor', [psb[hf], xresb], [x1tb], out=x1t[:, hf * 512:(hf + 1) * 512], in0=ps[hf][:, :],
                      in1=xres[:, hf * 512:(hf + 1) * 512], op=ALU.add)
                P.dma('sp', x1_d[i * 128:(i + 1) * 128, :], x1t[:], reads=[x1tb], dsem=dsx1)
                r = rms_rstd(x1t[:], x1tb, D, 0)
                I('dve', 'scalar_tensor_tensor', [x1tb, ssb, lnmoeb], [xm32b], out=xm32[:], in0=x1t[:], scalar=r, in1=lnmoe[:], op0=ALU.mult, op1=ALU.mult)
                xmb_t, xmb_b = xmb[i % 2]
                I('act', 'activation', [xm32b], [xmb_b], out=xmb_t[:], in_=xm32[:], func=AF.Copy)
                P.dma('sp', xm_d[i * 128:(i + 1) * 128, :], xmb_t[:], reads=[xmb_b], dsem=dsxm[i % 2])

            def T3(i=i):
                for kc in range(8):
                    bk = kc // 4
                    I('pe', 'transpose', [xm32b, cAb], [psb[bk]], out=ps[bk][:, (kc % 4) * 128:(kc % 4 + 1) * 128], in_=xm32[:, kc * 128:(kc + 1) * 128], identity=identf)
                I('act', 'activation', [psb[0]], [xmT32b], out=xmT32[:, 0:4, :], in_=ps[0][:, :].rearrange("p (a b) -> p a b", a=4), func=AF.Copy)
                I('dve', 'tensor_copy', [psb[1]], [xmT32b], out=xmT32[:, 4:8, :], in_=ps[1][:, :].rearrange("p (a b) -> p a b", a=4))

            def T4(i=i):
                for kc in range(8):
                    I('pe', 'matmul', [xmT32b, wr32b], [psb[7]], ps[7][:, 0:32], lhsT=xmT32[:, kc, :], rhs=wr32[:, kc, :], start=(kc == 0), stop=(kc == 7))
                I('dve', 'tensor_tensor', [psb[7], broutb], [lgb], out=lg[:], in0=ps[7][:, 0:32], in1=brout[:], op=ALU.add)
                if dbg:
                    P.dma('sp', dbg_d["lg"][i * 128:(i + 1) * 128, :], lg[:], reads=[lgb], dsem=dbgsem())
                I('dve', 'max', [lgb], [m8b], out=m8[:], in_=lg[:])
                I('dve', 'max_index', [lgb, m8b], [i8b], out=i8[:], in_max=m8[:], in_values=lg[:])
                I('dve', 'tensor_copy', [i8b], [i8fb], out=i8f[:], in_=i8[:])
                for k in range(4):
                    I('dve', 'tensor_scalar', [cAb, i8fb], [oh4b], out=oh4[:, k, :], in0=tvals[:, 0:32], scalar1=i8f[:, k:k + 1], scalar2=None, op0=ALU.is_equal)
                I('dve', 'tensor_tensor', [oh4b], [maskb], out=mask[:], in0=oh4[:, 0, :], in1=oh4[:, 1, :], op=ALU.add)
                I('dve', 'tensor_tensor', [oh4b, maskb], [maskb], out=mask[:], in0=mask[:], in1=oh4[:, 2, :], op=ALU.add)
                I('dve', 'tensor_tensor', [oh4b, maskb], [maskb], out=mask[:], in0=mask[:], in1=oh4[:, 3, :], op=ALU.add)
                I('dve', 'tensor_copy', [maskb], [maskbfb], out=maskbf[:], in_=mask[:])

            def T5(i=i):
                I('pe', 'matmul', [cBb, maskbfb], [psb[7]], ps[7][:, 64:96], lhsT=tristrict, rhs=maskbf[:], start=True, stop=False)
                I('pe', 'matmul', [cBb, cmbfb], [psb[7]], ps[7][:, 64:96], lhsT=ones_bf, rhs=cmbf[:], start=False, stop=True)
                I('dve', 'tensor_scalar', [m8b], [smb], out=sm[:, 0:1], in0=m8[:, 0:1], scalar1=-1.0, scalar2=None, op0=ALU.mult)
                I('act', 'activation', [lgb, smb], [exb], out=ex[:], in_=lg[:], func=AF.Exp, bias=sm[:, 0:1], scale=1.0)
                I('dve', 'tensor_tensor', [exb, maskb], [exb], out=ex[:], in0=ex[:], in1=mask[:], op=ALU.mult)
                I('dve', 'tensor_reduce', [exb], [smb], out=sm[:, 1:2], in_=ex[:], axis=AX.X, op=ALU.add)
                I('dve', 'reciprocal', [smb], [smb], out=sm[:, 2:3], in_=sm[:, 1:2])
                I('dve', 'tensor_scalar', [exb, smb], [gatesb], out=gates[:], in0=ex[:], scalar1=sm[:, 2:3], scalar2=None, op0=ALU.mult)
                I('dve', 'tensor_tensor', [psb[7], cAb], [slotfb], out=slotf[:], in0=ps[7][:, 64:96], in1=eoff, op=ALU.add)
                I('dve', 'tensor_tensor', [cmb, maskb], [cmb], out=cm[:], in0=cm[:], in1=mask[:], op=ALU.add)
                I('dve', 'tensor_copy', [cmb], [cmbfb], out=cmbf[:], in_=cm[:])
                for k in range(4):
                    I('dve', 'tensor_tensor', [oh4b, slotfb], [jk32b], out=jk32[:], in0=oh4[:, k, :], in1=slotf[:], op=ALU.mult)
                    I('dve', 'tensor_reduce', [jk32b], [slot4fb], out=slot4f[:, k:k + 1], in_=jk32[:], axis=AX.X, op=ALU.add)
                    I('dve', 'tensor_tensor', [oh4b, gatesb], [jk32b], out=jk32[:], in0=oh4[:, k, :], in1=gates[:], op=ALU.mult)
                    I('dve', 'tensor_reduce', [jk32b], [gate4b], out=gate4[:, i, k:k + 1], in_=jk32[:], axis=AX.X, op=ALU.add)
                I('dve', 'tensor_copy', [slot4fb], [slots_ib], out=slots_i[:, i, :], in_=slot4f[:])

            if s + 1 < NSB:
                load_norm_A(4 * (s + 1) + j, j, dma=False)
                pending_tail.append(lambda j=j: load_norm_B(j, lnb, lnbb))
            pending_tail.extend([T1, T2, T3, T4, T5])
            if not DEFER_TAIL or (j == 3 and s + 1 == NSB):
                while pending_tail:
                    pending_tail.pop(0)()
            elif j == 3:
                pending_tail.pop(0)()
    P.barrier()
    esB.close()
    esAB.close()

    esD = ExitStack()
    NK = NB * 4
    cnt, cntb_ = sb("cnt", [128, 32], F32, stack=esD)
    cnti, cntib = sb("cnti", [128, 32], I32, stack=esD)
    ntl, ntlb = sb("ntl", [128, 32], F32, stack=esD)
    cinc, cincb = sb("cinc", [128, 32], F32, stack=esD)
    offs, offsb = sb("offs", [128, 32], F32, stack=esD)
    s_e, s_eb = sb("s_e", [128, NK], I32, stack=esD)
    s_p, s_pb = sb("s_p", [128, NK], I32, stack=esD)
    f_e, f_eb = sb("f_e", [128, NK], F32, stack=esD)
    f_p, f_pb = sb("f_p", [128, NK], F32, stack=esD)
    f_t, f_tb = sb("f_t", [128, NK], F32, stack=esD)
    xml = [sb("xml%d" % i, [128, D], BF16, stack=esD) for i in range(4)]
    dsxl = [DSem(P) for _ in range(4)]
    I('pe', 'matmul', [cBb, cmbfb], [psb[7]], ps[7][:, 0:32], lhsT=ones_bf, rhs=cmbf[:], start=True, stop=True)
    I('dve', 'tensor_scalar', [psb[7]], [cntb_], out=cnt[:], in0=ps[7][:, 0:32], scalar1=511.0, scalar2=None, op0=ALU.add)
    I('dve', 'tensor_copy', [cntb_], [cntib], out=cnti[:], in_=cnt[:])
    I('dve', 'tensor_single_scalar', [cntib], [cntib], out=cnti[:], in_=cnti[:], scalar=9, op=ALU.arith_shift_right)
    I('dve', 'tensor_copy', [cntib], [ntlb], out=ntl[:], in_=cnti[:])
    I('dve', 'tensor_tensor_scan', [ntlb, cAb], [cincb], out=cinc[:], data0=c_ones[:, 0:32], data1=ntl[:], initial=0.0, op0=ALU.mult, op1=ALU.add)
    I('dve', 'tensor_tensor', [cincb, ntlb], [offsb], out=offs[:], in0=cinc[:], in1=ntl[:], op=ALU.subtract)
    I('dve', 'tensor_scalar', [offsb], [offsb], out=offs[:], in0=offs[:], scalar1=512.0, scalar2=None, op0=ALU.mult)
    sl2 = slots_i[:].rearrange("p a b -> p (a b)")
    I('dve', 'tensor_single_scalar', [slots_ib], [s_eb], out=s_e[:], in_=sl2, scalar=LOGCAP, op=ALU.arith_shift_right)
    I('dve', 'tensor_single_scalar', [slots_ib], [s_pb], out=s_p[:], in_=sl2, scalar=CAP - 1, op=ALU.bitwise_and)
    I('dve', 'tensor_copy', [s_eb], [f_eb], out=f_e[:], in_=s_e[:])
    I('dve', 'tensor_copy', [s_pb], [f_pb], out=f_p[:], in_=s_p[:])
    for e_ in range(32):
        I('dve', 'tensor_scalar', [f_eb, offsb], [f_tb], out=f_t[:], in0=f_e[:], scalar1=float(e_), scalar2=offs[:, e_:e_ + 1], op0=ALU.is_equal, op1=ALU.mult)
        I('dve', 'tensor_tensor', [f_tb, f_pb], [f_pb], out=f_p[:], in0=f_p[:], in1=f_t[:], op=ALU.add)
    I('dve', 'tensor_copy', [f_pb], [slots_ib], out=sl2, in_=f_p[:])
    for i in range(NB):
        xl_t, xl_b = xml[i % 4]
        P.dma('sp', xl_t[:], xm_d[i * 128:(i + 1) * 128, :], writes=[xl_b], dsem=dsxl[i % 4])
        for k in range(4):
            P.op('pool', lambda e, k=k, i=i, xl_t=xl_t: e.indirect_dma_start(
                out=xs_d, out_offset=bass.IndirectOffsetOnAxis(ap=slots_i[:, i, k:k + 1], axis=0), in_=xl_t[:], in_offset=None),
                reads=[xl_b, slots_ib], writes=[], dsem=dsxl[i % 4], is_dma=True)
    P.barrier()
    if stop_after <= 2:
        if dbg:
            dump("slots", slots_i[:], slots_ib, [128, NB, 4], I32)
            dump("gate4", gate4[:], gate4b, [128, NB, 4])
            dump("cm", cm[:], cmb, [128, 32])
        P.emit(final_waits=[d for d in P.dsems if d.count > 0])
        es.close()
        return nc

    esE = ExitStack()
    cmp_, cmpb = sb("cmp", [128, 32], F32, stack=esE)
    ET, ETb = sb("ET", [128, NT], F32, stack=esE)
    CEX, CEXb = sb("CEX", [128, NT], F32, stack=esE)
    JT, JTb = sb("JT", [128, NT], F32, stack=esE)
    EC, ECb = sb("EC", [128, NT], F32, stack=esE)
    BASE, BASEb = sb("BASE", [128, NT], F32, stack=esE)
    idxf, idxfb = sb("idxf", [128, NT, 12], F32, stack=esE)
    idxi, idxib = sb("idxi", [128, NT, 12], I32, stack=esE)
    OH, OHb = sb("OH", [32, NT], F32, stack=esE)
    ones512, ones512b = sb("ones512", [32, 512], F32, stack=esE)
    ohb, ohbb = sb("ohb", [32, 512], BF16, stack=esE)
    b1T = [sb("b1T%d" % i, [128, 16], F32, stack=esE) for i in range(2)]
    dsB1 = [DSem(P) for _ in range(2)]
    xsT2 = [sb("xsT%d" % i, [128, 8, 512], BF16, stack=esE) for i in range(2)]
    b2n, b2nb = sb("b2n", [32, D], BF16, stack=esE)
    W1t = [sb("W1t%d" % i, [128, 8, 2048], BF16, stack=esE) for i in range(2)]
    W2t = [sb("W2t%d" % i, [128, 8 * D + 16], BF16, stack=esE) for i in range(2)]
    xst = [sb("xst%d" % i, [128, 4, D], BF16, stack=esE) for i in range(2)]
    dsW1 = [DSem(P) for _ in range(2)]
    dsW2 = [DSem(P) for _ in range(2)]
    dsXs = [DSem(P) for _ in range(2)]
    actT, actTb = sb("actT", [128, 8, 512], BF16, stack=esE)
    gbuf = [sb("gb%d" % i, [128, 512], F32, stack=esE) for i in range(2)]
    sgbuf = [sb("sgb%d" % i, [128, 512], F32, stack=esE) for i in range(2)]
    lbuf = [sb("lb%d" % i, [128, 512], F32, stack=esE) for i in range(2)]
    yst = [sb("yst%d" % i, [128, D], F32, stack=esE) for i in range(2)]
    dsYs = [DSem(P) for _ in range(2)]
    ds_e = DSem(P)
    P.dma('pool', b2n[:], b2_d, writes=[b2nb], dsem=ds_e)
    I('dve', 'memset', [], [ones512b], ones512[:], 1.0)
    for i2 in range(2):
        I('dve', 'memset', [], [b1T[i2][1]], b1T[i2][0][:], 0.0)
    for t in range(NT):
        I('dve', 'tensor_scalar', [cincb], [cmpb], out=cmp_[:], in0=cinc[:], scalar1=float(t), scalar2=None, op0=ALU.is_le)
        I('dve', 'tensor_reduce', [cmpb], [ETb], out=ET[:, t:t + 1], in_=cmp_[:], axis=AX.X, op=ALU.add)
    I('dve', 'tensor_scalar', [ETb], [ECb], out=EC[:], in0=ET[:], scalar1=31.0, scalar2=128.0, op0=ALU.min, op1=ALU.mult)
    for t in range(NT):
        I('dve', 'tensor_scalar', [cAb, ECb], [idxfb], out=idxf[:, t, 4:12], in0=pidx.to_broadcast([128, 8]), scalar1=EC[:, t:t + 1], scalar2=None, op0=ALU.add)
        I('dve', 'tensor_scalar', [cAb, ECb], [idxfb], out=idxf[:, t, 0:4], in0=pc4, scalar1=0.0, scalar2=None, op0=ALU.add)
    I('dve', 'tensor_copy', [idxfb], [idxib], out=idxi[:], in_=idxf[:])
    I('dve', 'tensor_scalar', [ETb, cAb], [OHb], out=OH[:], in0=ET[0:32, :], scalar1=pidx[0:32, 0:1], scalar2=None, op0=ALU.is_equal)
    if dbg:
        dump("ET", ET[:], ETb, [128, NT])
        dump("idxi", idxi[:], idxib, [128, NT, 12], I32)
        dump("ntl", ntl[:], ntlb, [128, 32])

    def issue_gathers(t):
        k = t % 2
        P.dma('sp', xst[k][0][:], xs_d[t * 512:(t + 1) * 512, :].rearrange("(c p) d -> p c d", p=128), writes=[xst[k][1]], dsem=dsXs[k])
        P.op('pool', lambda e, t=t, k=k: e.indirect_dma_start(
            out=W1t[k][0][:].rearrange("p a b -> p (a b)"), out_offset=None, in_=w1_d, in_offset=bass.IndirectOffsetOnAxis(ap=idxi[:, t, 4:5], axis=0)),
            reads=[idxib], writes=[W1t[k][1]], dsem=dsW1[k], is_dma=True)
        P.op('pool', lambda e, t=t, k=k: e.indirect_dma_start(
            out=W2t[k][0][:], out_offset=None, in_=w2_d, in_offset=bass.IndirectOffsetOnAxis(ap=idxi[:, t, 4:5], axis=0)),
            reads=[idxib], writes=[W2t[k][1]], dsem=dsW2[k], is_dma=True)

    def do_transposes(t):
        k = t % 2
        xs_t, xs_b = xst[k]
        xsT, xsTb = xsT2[k]
        for c in range(4):
            bk = 6 + (c % 2)
            pT = psbf(bk)
            for kc in range(8):
                I('pe', 'transpose', [xs_b, cBb], [psb[bk]], out=pT[:, kc * 128:(kc + 1) * 128], in_=xs_t[:, c, kc:D:8], identity=ident)
            if c % 2 == 0:
                I('act', 'activation', [psb[bk]], [xsTb], out=xsT[:, :, c * 128:(c + 1) * 128], in_=pT.rearrange("p (a b) -> p a b", a=8), func=AF.Copy)
            else:
                I('dve', 'tensor_copy', [psb[bk]], [xsTb], out=xsT[:, :, c * 128:(c + 1) * 128], in_=pT.rearrange("p (a b) -> p a b", a=8))

    issue_gathers(0)
    do_transposes(0)
    ysn = 0
    for t in range(NT):
        k = t % 2
        if t + 1 < NT:
            issue_gathers(t + 1)
        w1_t, w1_b = W1t[k]
        w2_flat, w2_b = W2t[k]
        w2_t = w2_flat[:, 0:8 * D].rearrange("p (a b) -> p a b", a=8)
        b1_t, b1_b = b1T[k]
        I('dve', 'tensor_copy', [w2_b], [b1_b], out=b1_t[:], in_=w2_flat[:, 8 * D:8 * D + 16])
        xsT, xsTb = xsT2[k]
        I('dve', 'tensor_scalar', [ones512b, OHb], [ohbb], out=ohb[:], in0=ones512[:], scalar1=OH[:, t:t + 1], scalar2=None, op0=ALU.mult)
        I('dve', 'tensor_scalar', [b1_b], [b1_b], out=b1_t[:, 1:16:2], in0=b1_t[:, 1:16:2], scalar1=1.0, scalar2=None, op0=ALU.add)
        for fj in range(8):
            bA = (fj % 2) * 2
            bB = bA + 1
            for (bk, off) in ((bA, 0), (bB, 1)):
                for kc in range(8):
                    I('pe', 'matmul', [w1_b, xsTb], [psb[bk]], ps[bk][:, :], lhsT=w1_t[:, kc, 2 * fj + off:2048:16], rhs=xsT[:, kc, :],
                      start=(kc == 0), stop=(kc == 7))
            g_t, g_b = gbuf[fj % 2]
            s_t, s_b = sgbuf[fj % 2]
            l_t, l_b = lbuf[fj % 2]
            I('dve', 'tensor_scalar', [psb[bA], b1_b], [g_b], out=g_t[:], in0=ps[bA][:, :], scalar1=b1_t[:, 2 * fj:2 * fj + 1], scalar2=7.0, op0=ALU.add, op1=ALU.min)
            I('act', 'activation', [g_b], [s_b], out=s_t[:], in_=g_t[:], func=AF.Sigmoid, scale=1.702)
            I('act', 'activation', [psb[bB], b1_b], [l_b], out=l_t[:], in_=ps[bB][:, :], func=AF.Identity, bias=b1_t[:, 2 * fj + 1:2 * fj + 2], scale=1.0)
            I('dve', 'tensor_scalar', [l_b], [l_b], out=l_t[:], in0=l_t[:], scalar1=8.0, scalar2=-6.0, op0=ALU.min, op1=ALU.max)
            I('dve', 'tensor_tensor', [l_b, g_b], [l_b], out=l_t[:], in0=l_t[:], in1=g_t[:], op=ALU.mult)
            I('dve', 'tensor_tensor', [l_b, s_b], [actTb], out=actT[:, fj, :], in0=l_t[:], in1=s_t[:], op=ALU.mult)
        if t + 1 < NT:
            do_transposes(t + 1)
        for c in range(4):
            ys_t, ys_b = yst[ysn % 2]
            dsy = dsYs[ysn % 2]
            ysn += 1
            for hf in range(2):
                bk = 4 + hf
                for fj in range(8):
                    I('pe', 'matmul', [actTb, w2_b], [psb[bk]], ps[bk][:, :], lhsT=actT[:, fj, c * 128:(c + 1) * 128], rhs=w2_t[:, fj, hf * 512:(hf + 1) * 512],
                      start=(fj == 0), stop=False)
                I('pe', 'matmul', [ohbb, b2nb], [psb[bk]], ps[bk][:, :], lhsT=ohb[0:32, 0:128], rhs=b2n[0:32, hf * 512:(hf + 1) * 512], start=False, stop=True)
                if hf == 0:
                    I('act', 'activation', [psb[bk]], [ys_b], out=ys_t[:, 0:512], in_=ps[bk][:, :], func=AF.Copy)
                else:
                    I('dve', 'tensor_copy', [psb[bk]], [ys_b], out=ys_t[:, 512:1024], in_=ps[bk][:, :])
            P.dma('sp', ys_d[t * 512 + c * 128:t * 512 + (c + 1) * 128, :], ys_t[:], reads=[ys_b], dsem=dsy)
    P.barrier()
    esE.close()
    esD.close()
    if stop_after <= 3 and False:
        pass

    esC = ExitStack()
    Wpg, Wpgb = sb("Wpg", [128, 8, D], BF16, stack=esC)
    Wpp, Wppb = sb("Wpp", [128, 2, D], BF16, stack=esC)
    lnfin, lnfinb = sb("lnfin", [128, D], F32, stack=esC)
    lnbp, lnbpb = sb("lnbp", [128, 8, 128], F32, stack=esC)
    ds_p = DSem(P)
    P.dma('pool', Wpg[:], w_pg_d.rearrange("(kc p) c -> p kc c", p=128), writes=[Wpgb], dsem=ds_p)
    P.dma('pool', Wpp[:], w_pp_d.rearrange("(kc p) c -> p kc c", p=128), writes=[Wppb], dsem=ds_p)
    P.dma('sp', lnfin[:], lnfin_d, writes=[lnfinb], dsem=ds_p)
    for kc in range(8):
        I('dve', 'tensor_scalar', [cAb, lnpleTb], [lnbpb], out=lnbp[:, kc, :], in0=c_ones, scalar1=lnpleT[:, kc:kc + 1], scalar2=None, op0=ALU.mult)
    NP3 = 3
    x1l = [sb("x1l%d" % i, [128, D], F32, stack=esC) for i in range(NP3)]
    dsx1l = [DSem(P) for _ in range(NP3)]
    yk = [[sb("yk%d_%d" % (i, k), [128, D], F32, stack=esC) for k in range(4)] for i in range(NP3)]
    dsyk = [DSem(P) for _ in range(NP3)]
    pb32 = [sb("pb32_%d" % i, [128, 256], F32, stack=esC) for i in range(NP3)]
    dspb = [DSem(P) for _ in range(NP3)]
    pbl = [sb("pbl%d" % i, [128, 256], BF16, stack=esC) for i in range(2)]
    xp, xpb = sb("xp", [128, D], BF16, stack=esC)
    xpTs = [sb("xpT%d" % i, [128, 8, 128], BF16, stack=esC) for i in range(2)]
    pTs = [sb("pTt%d" % i, [128, 2, 128], BF16, stack=esC) for i in range(2)]
    sgp, sgpb = sb("sgp", [128, 512], F32, stack=esC)
    x3, x3b = sb("x3", [128, D], F32, stack=esC)
    ot = [sb("ot%d" % i, [128, D], F32, stack=esC) for i in range(2)]
    dso = [DSem(P) for _ in range(2)]

    nhalf, nhalfb = sb("nhalf", [128, 1], F32, stack=esC)
    I('dve', 'memset', [], [nhalfb], nhalf[:], -0.5)

    def rms_rstd_pow(src_ap, srcb, n, col):
        I('act', 'activation', [srcb], [junkb], out=junk[:, 0:n], in_=src_ap, func=AF.Square)
        I('dve', 'tensor_reduce', [junkb], [ssb], out=ss[:, col:col + 1], in_=junk[:, 0:n], axis=AX.X, op=ALU.add)
        I('dve', 'tensor_scalar', [ssb], [ssb], out=ss[:, col + 1:col + 2], in0=ss[:, col:col + 1], scalar1=1.0 / n, scalar2=EPS, op0=ALU.mult, op1=ALU.add)
        I('pool', 'tensor_tensor', [ssb, nhalfb], [ssb], out=ss[:, col + 2:col + 3], in0=ss[:, col + 1:col + 2], in1=nhalf[:], op=ALU.pow)
        return ss[:, col + 2:col + 3]

    def stageL(i):
        x1_t, x1_b = x1l[i % NP3]
        P.dma('sp', x1_t[:], x1_d[i * 128:(i + 1) * 128, :], writes=[x1_b], dsem=dsx1l[i % NP3])
        P.dma('sp', pb32[i % NP3][0][:], p_d[i * 128:(i + 1) * 128, :], writes=[pb32[i % NP3][1]], dsem=dspb[i % NP3])
        for k in range(4):
            P.op('pool', lambda e, k=k, i=i: e.indirect_dma_start(
                out=yk[i % NP3][k][0][:], out_offset=None, in_=ys_d, in_offset=bass.IndirectOffsetOnAxis(ap=slots_i[:, i, k:k + 1], axis=0)),
                reads=[slots_ib], writes=[yk[i % NP3][k][1]], dsem=dsyk[i % NP3], is_dma=True)

    def stageA(i):
        x1_t, x1_b = x1l[i % NP3]
        pb_t, pb_b = pbl[i % 2]
        I('act', 'activation', [pb32[i % NP3][1]], [pb_b], out=pb_t[:], in_=pb32[i % NP3][0][:], func=AF.Copy)
        for k in range(4):
            y_t, y_b = yk[i % NP3][k]
            I('dve', 'scalar_tensor_tensor', [y_b, gate4b, x1_b], [x1_b], out=x1_t[:], in0=y_t[:], scalar=gate4[:, i, k:k + 1], in1=x1_t[:], op0=ALU.mult, op1=ALU.add)
        if dbg:
            P.dma('sp', dbg_d["x2"][i * 128:(i + 1) * 128, :], x1_t[:], reads=[x1_b], dsem=DSem(P))
        r = rms_rstd(x1_t[:], x1_b, D, 0)
        I('dve', 'tensor_scalar', [x1_b, ssb], [xpb], out=xp[:], in0=x1_t[:], scalar1=r, scalar2=None, op0=ALU.mult)
        xpT, xpTb = xpTs[i % 2]
        pT_, pTb_ = pTs[i % 2]
        pT = psbf(2)
        for kc in range(8):
            I('pe', 'transpose', [xpb, cBb], [psb[2]], out=pT[:, kc * 128:(kc + 1) * 128], in_=xp[:, kc * 128:(kc + 1) * 128], identity=ident)
        I('dve', 'tensor_tensor', [psb[2], lnbpb], [xpTb], out=xpT[:], in0=pT.rearrange("p (a b) -> p a b", a=8), in1=lnbp[:], op=ALU.mult)
        pT5 = psbf(5)
        for kc in range(2):
            I('pe', 'transpose', [pb_b, cBb], [psb[5]], out=pT5[:, kc * 128:(kc + 1) * 128], in_=pb_t[:, kc * 128:(kc + 1) * 128], identity=ident)
        I('act', 'activation', [psb[5]], [pTb_], out=pT_[:], in_=pT5[:, 0:256].rearrange("p (a b) -> p a b", a=2), func=AF.Copy)

    def stageB(i):
        x1_t, x1_b = x1l[i % NP3]
        xpT, xpTb = xpTs[i % 2]
        pT_, pTb_ = pTs[i % 2]
        for hf in range(2):
            for kc in range(8):
                I('pe', 'matmul', [xpTb, Wpgb], [psb[hf]], ps[hf][:, :], lhsT=xpT[:, kc, :], rhs=Wpg[:, kc, hf * 512:(hf + 1) * 512], start=(kc == 0), stop=(kc == 7))
            for kc in range(2):
                I('pe', 'matmul', [pTb_, Wppb], [psb[3 + hf]], ps[3 + hf][:, :], lhsT=pT_[:, kc, :], rhs=Wpp[:, kc, hf * 512:(hf + 1) * 512], start=(kc == 0), stop=(kc == 1))
            I('act', 'activation', [psb[hf]], [sgpb], out=sgp[:], in_=ps[hf][:, :], func=AF.Sigmoid)
            I('dve', 'tensor_tensor', [sgpb, psb[3 + hf]], [sgpb], out=sgp[:], in0=sgp[:], in1=ps[3 + hf][:, :], op=ALU.mult)
            I('dve', 'tensor_tensor', [sgpb, x1_b], [x3b], out=x3[:, hf * 512:(hf + 1) * 512], in0=sgp[:], in1=x1_t[:, hf * 512:(hf + 1) * 512], op=ALU.add)
        r = rms_rstd(x3[:], x3b, D, 4)
        o_t, o_b = ot[i % 2]
        I('dve', 'scalar_tensor_tensor', [x3b, ssb, lnfinb], [o_b], out=o_t[:], in0=x3[:], scalar=r, in1=lnfin[:], op0=ALU.mult, op1=ALU.mult)
        P.dma('sp', out_d[i * 128:(i + 1) * 128, :], o_t[:], reads=[o_b], dsem=dso[i % 2])

    stageL(0)
    if NB > 1:
        stageL(1)
    stageA(0)
    for i in range(NB):
        if i + 2 < NB:
            stageL(i + 2)
        if i + 1 < NB:
            stageA(i + 1)
        stageB(i)
    P.emit(final_waits=[d for d in P.dsems if d.count > 0])
    esC.close()
    es.close()
    return nc


def make_consts(T):
    CAP = T
    s = np.arange(128)[:, None]
    t = np.arange(128)[None, :]
    ident = (s == t).astype(np.float32)
    triS = np.where(s <= t, -1.0 / 16.0, 0.0).astype(np.float32)
    tristrict = (s < t).astype(np.float32)
    ones = np.ones((128, 128), np.float32)
    maskA = (s <= t).astype(np.float32)
    maskA4 = np.tile(maskA, (1, 4))
    eoff = np.tile((np.arange(32) * CAP).astype(np.float32)[None, :], (128, 1))
    pk8 = (np.arange(8)[None, :] * 128 + np.arange(128)[:, None]).astype(np.float32)
    pc4 = (np.arange(4)[None, :] * 128 + np.arange(128)[:, None]).astype(np.float32)
    pidx = np.arange(128, dtype=np.float32)[:, None]
    tv = np.tile(np.arange(64, dtype=np.float32)[None, :], (128, 1))
    cA = np.concatenate([ident, triS, tristrict, ones, ident, ones, maskA4, eoff, pk8, pc4, pidx, tv], axis=1)
    maskadd = np.zeros((128, 5, 128), np.float32)
    maskadd[0:64, 0, 64:128] = NEG
    maskadd[64:128, 4, 0:64] = NEG
    return np.ascontiguousarray(cA.astype(np.float32)), maskadd


def make_shared(inputs, T):
    f = lambda a: np.ascontiguousarray(np.asarray(a, dtype=np.float32))
    cA, maskadd = make_consts(T)
    rel_bias = np.asarray(inputs["rel_bias"][0], np.float32)
    k = np.arange(128)[:, None, None]
    kb = np.arange(5)[None, :, None]
    q = np.arange(128)[None, None, :]
    rel = np.clip((512 + q) - (kb * 128 + k), -256, 256) + 256
    biasT = np.transpose(rel_bias[:, rel], (1, 0, 2, 3))
    sh = {
        "w_in": f(inputs["w_in"][0]),
        "wgk_aug": f(np.concatenate([inputs["w_gk"][0], inputs["b_gk"][0][None, :]], axis=0)),
        "biasT": f(biasT),
        "w_out": f(inputs["w_out"][0]),
        "w_router": f(inputs["w_router"][0]),
        "w1": f(np.asarray(inputs["w1"][0]).reshape(32 * 128, 8 * 2048)),
        "b1": f(np.asarray(inputs["b1"][0]).reshape(32 * 128, 16)),
        "w2": f(np.concatenate([np.asarray(inputs["w2"][0], np.float32).reshape(32 * 128, 8 * D), np.asarray(inputs["b1"][0], np.float32).reshape(32 * 128, 16)], axis=1)),
        "b2": f(inputs["b2"][0]),
        "w_pg": f(inputs["w_ple_gate"][0]),
        "w_pp": f(inputs["w_ple_proj"][0]),
        "lnmixT": f(np.asarray(inputs["ln_mix"][0]).reshape(8, 128).T),
        "lnpleT": f(np.asarray(inputs["ln_ple"][0]).reshape(8, 128).T),
        "lnmoe_b": f(np.broadcast_to(np.asarray(inputs["ln_moe"][0])[None, :], (128, D))),
        "lnfin_b": f(np.broadcast_to(np.asarray(inputs["ln_final"])[None, :], (128, D))),
        "gnorm_b": f(np.broadcast_to(np.asarray(inputs["gla_norm"][0])[None, :], (128, 256))),
        "brout_b": f(np.broadcast_to(np.asarray(inputs["b_router"][0])[None, :], (128, 32))),
        "cA": cA,
        "maskadd": maskadd,
    }
    return sh


def kernel(**inputs):
    x = np.asarray(inputs["x"], np.float32)
    p = np.asarray(inputs["p"], np.float32)
    Bn, T, _ = x.shape
    nc = build_program(T)
    sh = make_shared(inputs, T)
    in_maps = []
    for c in range(Bn):
        m = dict(sh)
        m["x"] = np.ascontiguousarray(x[c])
        m["p"] = np.ascontiguousarray(p[0, c])
        in_maps.append(m)
    res = run_bass_kernel_spmd(nc, in_maps, core_ids=list(range(Bn)))
    return np.stack([np.asarray(r["out"], np.float32) for r in res.results], axis=0)
```
